# Optimizing a Trainium2 kernel written in Bass

```python
import math
import jax
import jax.numpy as jnp
from jax import lax
import numpy as np

D_MODEL = 2048
BATCH = 4
SEQ = 8192
DEPTH = 2

GRID_W = 64
CTX_LEN = 256
N_DIR = 2

ML_HEADS = 4
ML_DH = 256
ML_W = ML_HEADS * ML_DH
ML_CHUNK = 64
ML_NORM_EPS = 1e-6

RW_HEADS = 16
RW_DH = 64
RW_W = RW_HEADS * RW_DH
RW_DECAY_RANK = 64
RW_A_RANK = 64
RW_G_RANK = 128
RW_DECAY_OFFSET = 0.5
RW_NORM_EPS = 64e-5

S5_W = 1024
S5_GROUP = 16
S5_GROUPS = S5_W // S5_GROUP
S5_STATE = 64

N_BRANCH = 3

N_GROUPS = 4
EXPERTS_PER_GROUP = 4
N_EXPERTS = N_GROUPS * EXPERTS_PER_GROUP
TOP_K = 2
D_EXPERT = 1024

DEEPNORM_ALPHA = (2.0 * DEPTH) ** 0.25
DEEPNORM_BETA = (8.0 * DEPTH) ** -0.25
LN_EPS = 1e-5
N_MOD = 6

IN_WIDTHS = (ML_W, ML_W, ML_W, ML_W, N_DIR * ML_HEADS, N_DIR * ML_HEADS,
             RW_W, RW_W, RW_W, N_DIR * RW_DECAY_RANK, N_DIR * RW_A_RANK, RW_G_RANK,
             S5_W, N_BRANCH * D_MODEL)
D_IN = sum(IN_WIDTHS)
RW_IN_WIDTHS = (RW_W, RW_W, RW_W, N_DIR * RW_DECAY_RANK, N_DIR * RW_A_RANK, RW_G_RANK)
RW_IN_W = sum(RW_IN_WIDTHS)

kernel_name = 'hybrid_mlstm_rwkv7_s5_moe_deepnorm'


def layer_norm(x, eps=LN_EPS):
    xf = x.astype(jnp.float32)
    mu = jnp.mean(xf, axis=-1, keepdims=True)
    var = jnp.mean(jnp.square(xf - mu), axis=-1, keepdims=True)
    return (xf - mu) * lax.rsqrt(var + eps)


def modulate(x, shift, scale):
    return (layer_norm(x) * (1.0 + scale) + shift).astype(x.dtype)


def post_norm(z, gain, bias):
    return (layer_norm(z) * gain + bias).astype(z.dtype)


def head_norm(h, gain, bias, eps):
    y = layer_norm(h, eps)
    return y.reshape(h.shape[:-2] + (-1,)) * gain + bias


def split_cols(z, widths):
    return jnp.split(z, np.cumsum(widths)[:-1].tolist(), axis=-1)


def depthwise_conv3x3(z, w, b):
    ch = z.shape[-1]
    y = lax.conv_general_dilated(z, w[:, :, None, :].astype(z.dtype), window_strides=(1, 1), padding='SAME',
                                 dimension_numbers=('NHWC', 'HWIO', 'NHWC'), feature_group_count=ch)
    return y + b.astype(z.dtype)


def centred_shift(z):
    zp = jnp.pad(z, ((0, 0), (1, 1), (0, 0)))
    return 0.5 * (zp[:, :-2] + zp[:, 2:])


def dir_seq(c, x):
    fwd = jnp.concatenate([c[0], x[0]], axis=1)
    bwd = jnp.concatenate([jnp.flip(c[-1], 1), jnp.flip(x[-1], 1)], axis=1)
    return jnp.stack([fwd, bwd])


def dir_unsum(y, n_ctx):
    yc = y[0, :, :n_ctx] + jnp.flip(y[1, :, :n_ctx], 1)
    yx = y[0, :, n_ctx:] + jnp.flip(y[1, :, n_ctx:], 1)
    return yc, yx


def mlstm_chunkwise(q, k, v, ig, fg):
    P, B, T, H, dk = q.shape
    dv = v.shape[-1]
    L = ML_CHUNK
    n_chunks = T // L

    def to_chunks(a):
        a = a.astype(jnp.float32).reshape((P, B, n_chunks, L) + a.shape[3:])
        return jnp.swapaxes(jnp.moveaxis(a, 2, 0), 3, 4)

    xs = (to_chunks(q), to_chunks(k * dk ** -0.5), to_chunks(v), to_chunks(ig),
          to_chunks(jax.nn.log_sigmoid(fg.astype(jnp.float32))))
    tri = jnp.tril(jnp.ones((L, L), dtype=bool))

    def step(carry, inp):
        cmat, nvec, m = carry
        qj, kj, vj, li, lf = inp
        bcum = jnp.cumsum(lf, axis=-1)
        log_d = jnp.where(tri, bcum[..., :, None] - bcum[..., None, :] + li[..., None, :], -jnp.inf)
        inter = bcum + m[..., None]
        m_j = jnp.maximum(jnp.max(log_d, axis=-1), inter)
        scores = jnp.einsum('pbhjd,pbhsd->pbhjs', qj, kj) * jnp.exp(log_d - m_j[..., None])
        s_inter = jnp.exp(inter - m_j)
        num = (jnp.einsum('pbhjs,pbhsv->pbhjv', scores, vj)
               + s_inter[..., None] * jnp.einsum('pbhvd,pbhjd->pbhjv', cmat, qj))
        den = jnp.sum(scores, axis=-1) + s_inter * jnp.einsum('pbhd,pbhjd->pbhj', nvec, qj)
        h = num / jnp.maximum(jnp.abs(den), jnp.exp(-m_j))[..., None]
        b_last = bcum[..., -1]
        log_w = b_last[..., None] - bcum + li
        m_new = jnp.maximum(b_last + m, jnp.max(log_w, axis=-1))
        wts = jnp.exp(log_w - m_new[..., None])
        carry_decay = jnp.exp(b_last + m - m_new)
        cmat = carry_decay[..., None, None] * cmat + jnp.einsum('pbhs,pbhsv,pbhsd->pbhvd', wts, vj, kj)
        nvec = carry_decay[..., None] * nvec + jnp.einsum('pbhs,pbhsd->pbhd', wts, kj)
        return (cmat, nvec, m_new), h

    init = (jnp.zeros((P, B, H, dv, dk), jnp.float32), jnp.zeros((P, B, H, dk), jnp.float32),
            jnp.zeros((P, B, H), jnp.float32))
    _, h = lax.scan(step, init, xs)
    h = jnp.moveaxis(jnp.swapaxes(h, 3, 4), 0, 2)
    return h.reshape(P, B, T, H, dv)


def rwkv7_scan(r, w, k, v, kk, a):
    P, B, T, H, n = r.shape

    def to_time(z):
        return jnp.moveaxis(z.astype(jnp.float32), 2, 0)

    def step(s, inp):
        rt, wt, kt, vt, kkt, at = inp
        s_kk = jnp.einsum('pbhvk,pbhk->pbhv', s, kkt)
        s = s * wt[..., None, :] - s_kk[..., :, None] * (kkt * at)[..., None, :] + vt[..., :, None] * kt[..., None, :]
        return s, jnp.einsum('pbhvk,pbhk->pbhv', s, rt)

    s0 = jnp.zeros((P, B, H, n, n), jnp.float32)
    _, y = lax.scan(step, s0, tuple(to_time(z) for z in (r, w, k, v, kk, a)))
    return jnp.moveaxis(y, 0, 2)


def _diag_linear_combine(e1, e2):
    a1, b1 = e1
    a2, b2 = e2
    return a1 * a2, a2 * b1 + b2


def mlstm_branch(seg_c, seg_x, hw_c, hw_x, conv_w, conv_b, ig_b, fg_b, norm_g, norm_b):
    def heads(z):
        return z.reshape(z.shape[:-1] + (ML_HEADS, ML_DH))[None]

    prepped = []
    for (q, k, v, o, ig, fg), (rows, cols) in ((seg_c, hw_c), (seg_x, hw_x)):
        bsz, length, _ = q.shape
        qk = jnp.concatenate([q, k], axis=-1).reshape(bsz, rows, cols, 2 * ML_W)
        qk = jax.nn.silu(depthwise_conv3x3(qk, conv_w, conv_b)).reshape(bsz, length, 2 * ML_W)
        q, k = jnp.split(qk, 2, axis=-1)
        ig = jnp.moveaxis(ig.reshape(bsz, length, N_DIR, ML_HEADS) + ig_b, 2, 0)
        fg = jnp.moveaxis(fg.reshape(bsz, length, N_DIR, ML_HEADS) + fg_b, 2, 0)
        prepped.append((heads(q), heads(k), heads(v), ig, fg))
    (qc, kc, vc, ic, fc), (qx, kx, vx, ix, fx) = prepped
    h = mlstm_chunkwise(dir_seq(qc, qx), dir_seq(kc, kx), dir_seq(vc, vx), dir_seq(ic, ix), dir_seq(fc, fx))
    hc, hx = dir_unsum(h, qc.shape[2])
    oc, ox = seg_c[3], seg_x[3]
    yc = (jax.nn.sigmoid(oc) * head_norm(hc, norm_g, norm_b, ML_NORM_EPS)).astype(oc.dtype)
    yx = (jax.nn.sigmoid(ox) * head_norm(hx, norm_g, norm_b, ML_NORM_EPS)).astype(ox.dtype)
    return yc, yx


def rwkv7_branch(seg_c, seg_x, mu, w0, w_up, a0, a_up, g_up, k_k, k_a, r_k, norm_g, norm_b):
    def hd(z):
        return z.reshape(z.shape[:-1] + (RW_HEADS, RW_DH))

    prepped = []
    for seg in (seg_c, seg_x):
        z = jnp.concatenate(seg, axis=-1)
        z = z + mu * (centred_shift(z) - z)
        r, k, v, wd, ad, gd = split_cols(z, RW_IN_WIDTHS)
        bsz, length, _ = r.shape
        wd = wd.reshape(bsz, length, N_DIR, RW_DECAY_RANK)
        ad = ad.reshape(bsz, length, N_DIR, RW_A_RANK)
        w_pre = (w0 + jnp.einsum('bldr,drc->bldc', jnp.tanh(wd), w_up)).astype(jnp.float32)
        decay = jnp.exp(-jnp.exp(-jax.nn.softplus(-w_pre) - RW_DECAY_OFFSET))
        a = jax.nn.sigmoid(a0 + jnp.einsum('bldr,drc->bldc', ad, a_up))
        g = jax.nn.sigmoid(gd) @ g_up
        kk = hd(k * k_k).astype(jnp.float32)
        kk = kk / jnp.maximum(jnp.sqrt(jnp.sum(jnp.square(kk), axis=-1, keepdims=True)), 1e-12)
        k_dir = k[:, :, None, :] * (1.0 + (a - 1.0) * k_a)
        bonus = jnp.sum(hd(r[:, :, None, :] * k_dir * r_k.reshape(-1)), axis=(2, 4))
        bonus = bonus[..., None] * hd(v)
        prepped.append((hd(r)[None], jnp.moveaxis(hd(decay), 2, 0), jnp.moveaxis(hd(k_dir), 2, 0),
                        hd(v)[None], kk[None], jnp.moveaxis(hd(a), 2, 0), bonus, g))
    (rc, dc, kc, vc, kkc, ac, bc, gc), (rx, dx, kx, vx, kkx, ax, bx, gx) = prepped
    y = rwkv7_scan(dir_seq(rc, rx), dir_seq(dc, dx), dir_seq(kc, kx), dir_seq(vc, vx),
                   dir_seq(kkc, kkx), dir_seq(ac, ax))
    yc, yx = dir_unsum(y, rc.shape[2])

    def finish(yy, bonus, g):
        out = (head_norm(yy, norm_g, norm_b, RW_NORM_EPS) + bonus.reshape(bonus.shape[:-2] + (-1,))) * g
        return out.astype(g.dtype)

    return finish(yc, bc, gc), finish(yx, bx, gx)


def s5_branch(uc, ux, lam_re, lam_im, log_dt, b_re, b_im, c_re, c_im, d_skip):
    n_ctx = uc.shape[1]
    useq = dir_seq(uc[None], ux[None]).astype(jnp.float32)
    _, bsz, t_len, _ = useq.shape
    yc = d_skip * uc.astype(jnp.float32)
    yx = d_skip * ux.astype(jnp.float32)
    for d in range(N_DIR):
        lam = lax.complex(lam_re[d].astype(jnp.float32), lam_im[d].astype(jnp.float32))
        lam_bar = jnp.exp(lam * jnp.exp(log_dt[d].astype(jnp.float32))[:, None])
        b_bar = ((lam_bar - 1.0) / lam)[..., None] * lax.complex(b_re[d].astype(jnp.float32),
                                                                  b_im[d].astype(jnp.float32))
        u = useq[d].reshape(bsz, t_len, S5_GROUPS, S5_GROUP).astype(jnp.complex64)
        bu = jnp.einsum('gnc,btgc->btgn', b_bar, u)
        a_el = jnp.broadcast_to(lam_bar, (1, t_len) + lam_bar.shape)
        _, state = lax.associative_scan(_diag_linear_combine, (a_el, bu), axis=1)
        c_mat = lax.complex(c_re[d].astype(jnp.float32), c_im[d].astype(jnp.float32))
        y = jnp.real(jnp.einsum('gcn,btgn->btgc', c_mat, state)).reshape(bsz, t_len, S5_W)
        if d == 0:
            yc = yc + y[:, :n_ctx]
            yx = yx + y[:, n_ctx:]
        else:
            yc = yc + jnp.flip(y[:, :n_ctx], 1)
            yx = yx + jnp.flip(y[:, n_ctx:], 1)
    return jax.nn.gelu(yc).astype(uc.dtype), jax.nn.gelu(yx).astype(ux.dtype)


def token_mixer(uc, ux, need_ctx, w_in,
                ml_conv_w, ml_conv_b, ml_ig_b, ml_fg_b, ml_norm_g, ml_norm_b, ml_proj,
                rw_mu, rw_w0, rw_w_up, rw_a0, rw_a_up, rw_g_up, rw_k_k, rw_k_a, rw_r_k,
                rw_norm_g, rw_norm_b, rw_proj,
                s5_lam_re, s5_lam_im, s5_log_dt, s5_b_re, s5_b_im, s5_c_re, s5_c_im, s5_d,
                s5_w_val, s5_w_gate, w_out):
    hw_c = (1, uc.shape[1])
    hw_x = (ux.shape[1] // GRID_W, GRID_W)
    pc = split_cols(uc @ w_in, IN_WIDTHS)
    px = split_cols(ux @ w_in, IN_WIDTHS)
    ml_c, ml_x = mlstm_branch(pc[0:6], px[0:6], hw_c, hw_x, ml_conv_w, ml_conv_b, ml_ig_b, ml_fg_b,
                              ml_norm_g, ml_norm_b)
    rw_c, rw_x = rwkv7_branch(pc[6:12], px[6:12], rw_mu, rw_w0, rw_w_up, rw_a0, rw_a_up, rw_g_up,
                              rw_k_k, rw_k_a, rw_r_k, rw_norm_g, rw_norm_b)
    s5_c, s5_x = s5_branch(pc[12], px[12], s5_lam_re, s5_lam_im, s5_log_dt, s5_b_re, s5_b_im,
                           s5_c_re, s5_c_im, s5_d)

    def merge(ml, rw, s5, gate_pre):
        g = jax.nn.sigmoid(gate_pre).reshape(gate_pre.shape[:-1] + (N_BRANCH, D_MODEL))
        z = (g[..., 0, :] * (ml @ ml_proj) + g[..., 1, :] * (rw @ rw_proj)
             + g[..., 2, :] * ((s5 @ s5_w_val) * jax.nn.sigmoid(s5 @ s5_w_gate)))
        return z @ w_out

    out_x = merge(ml_x, rw_x, s5_x, px[13])
    out_c = merge(ml_c, rw_c, s5_c, pc[13]) if need_ctx else None
    return out_c, out_x


def moe_ffn(u, router_w, router_b, w_gate, w_up, w_down):
    n_tok = u.shape[0]
    affinity = jax.nn.sigmoid((u @ router_w).astype(jnp.float32))
    grouped = (affinity + router_b.astype(jnp.float32)).reshape(n_tok, N_GROUPS, EXPERTS_PER_GROUP)
    group_score = jnp.sum(lax.top_k(grouped, TOP_K)[0], axis=-1)
    best = jnp.argmax(group_score, axis=-1)
    keep = (best[:, None] == jnp.arange(N_GROUPS))[..., None]
    masked = jnp.where(keep, grouped, -jnp.inf).reshape(n_tok, N_EXPERTS)
    _, idx = lax.top_k(masked, TOP_K)
    wsel = jnp.take_along_axis(affinity, idx, axis=-1)
    wsel = wsel / jnp.sum(wsel, axis=-1, keepdims=True)
    gates = jnp.sum((idx[..., None] == jnp.arange(N_EXPERTS)).astype(jnp.float32) * wsel[..., None], axis=1)
    gates = gates.astype(u.dtype)
    out = jnp.zeros_like(u)
    for e in range(N_EXPERTS):
        h = jax.nn.silu(u @ w_gate[e]) * (u @ w_up[e])
        out = out + gates[:, e, None] * (h @ w_down[e])
    return out


def setup_inputs(seed: int = 0) -> dict:
    key = jax.random.key(seed)
    ks = iter(jax.random.split(key, 64))

    def nrm(shape, scale):
        return scale * jax.random.normal(next(ks), shape, jnp.float32)

    def unif(shape, lo, hi):
        return jax.random.uniform(next(ks), shape, jnp.float32, minval=lo, maxval=hi)

    L = DEPTH
    D = D_MODEL
    return {
        'x': nrm((BATCH, SEQ, D), 1.0),
        'c': nrm((BATCH, D), 1.0),
        'ctx': nrm((BATCH, CTX_LEN, D), 1.0),
        'c_ctx': nrm((D,), 1.0),
        'ada_w': nrm((L, D, N_MOD * D), D ** -0.5),
        'ada_b': nrm((L, N_MOD * D), 0.02),
        'w_in': nrm((L, D, D_IN), D ** -0.5),
        'ml_conv_w': nrm((L, 3, 3, 2 * ML_W), 1.0 / 3.0),
        'ml_conv_b': nrm((L, 2 * ML_W), 0.02),
        'ml_ig_b': nrm((L, N_DIR, ML_HEADS), 0.5),
        'ml_fg_b': jnp.linspace(3.0, 6.0, ML_HEADS) + nrm((L, N_DIR, ML_HEADS), 0.1),
        'ml_norm_g': 1.0 + nrm((L, ML_W), 0.02),
        'ml_norm_b': nrm((L, ML_W), 0.02),
        'ml_proj': nrm((L, ML_W, D), DEEPNORM_BETA * ML_W ** -0.5),
        'rw_mu': unif((L, RW_IN_W), 0.0, 1.0),
        'rw_w0': jnp.linspace(-6.0, -1.0, RW_W) + nrm((L, N_DIR, RW_W), 0.1),
        'rw_w_up': nrm((L, N_DIR, RW_DECAY_RANK, RW_W), 0.5 * RW_DECAY_RANK ** -0.5),
        'rw_a0': nrm((L, N_DIR, RW_W), 0.1),
        'rw_a_up': nrm((L, N_DIR, RW_A_RANK, RW_W), 0.5 * RW_A_RANK ** -0.5),
        'rw_g_up': nrm((L, RW_G_RANK, RW_W), RW_G_RANK ** -0.5),
        'rw_k_k': 0.85 + nrm((L, RW_W), 0.02),
        'rw_k_a': 1.0 + nrm((L, RW_W), 0.02),
        'rw_r_k': nrm((L, RW_HEADS, RW_DH), 0.1),
        'rw_norm_g': 1.0 + nrm((L, RW_W), 0.02),
        'rw_norm_b': nrm((L, RW_W), 0.02),
        'rw_proj': nrm((L, RW_W, D), DEEPNORM_BETA * RW_W ** -0.5),
        's5_lam_re': -0.5 + nrm((L, N_DIR, S5_GROUPS, S5_STATE), 0.01),
        's5_lam_im': math.pi * jnp.arange(S5_STATE, dtype=jnp.float32) + nrm((L, N_DIR, S5_GROUPS, S5_STATE), 0.01),
        's5_log_dt': unif((L, N_DIR, S5_GROUPS), math.log(1e-3), math.log(1e-1)),
        's5_b_re': nrm((L, N_DIR, S5_GROUPS, S5_STATE, S5_GROUP), (2.0 * S5_GROUP) ** -0.5),
        's5_b_im': nrm((L, N_DIR, S5_GROUPS, S5_STATE, S5_GROUP), (2.0 * S5_GROUP) ** -0.5),
        's5_c_re': nrm((L, N_DIR, S5_GROUPS, S5_GROUP, S5_STATE), 1.0),
        's5_c_im': nrm((L, N_DIR, S5_GROUPS, S5_GROUP, S5_STATE), 1.0),
        's5_d': nrm((L, S5_W), 0.5),
        's5_w_val': nrm((L, S5_W, D), DEEPNORM_BETA * S5_W ** -0.5),
        's5_w_gate': nrm((L, S5_W, D), S5_W ** -0.5),
        'w_out': nrm((L, D, D), DEEPNORM_BETA * D ** -0.5),
        'ln1_g': 1.0 + nrm((L, D), 0.02),
        'ln1_b': nrm((L, D), 0.02),
        'ln2_g': 1.0 + nrm((L, D), 0.02),
        'ln2_b': nrm((L, D), 0.02),
        'router_w': nrm((D, N_EXPERTS), D ** -0.5),
        'router_b': nrm((N_EXPERTS,), 0.01),
        'exp_w_gate': nrm((L, N_EXPERTS, D, D_EXPERT), D ** -0.5),
        'exp_w_up': nrm((L, N_EXPERTS, D, D_EXPERT), D ** -0.5),
        'exp_w_down': nrm((L, N_EXPERTS, D_EXPERT, D), DEEPNORM_BETA * D_EXPERT ** -0.5),
    }


def reference(x, c, ctx, c_ctx, ada_w, ada_b, w_in, ml_conv_w, ml_conv_b, ml_ig_b, ml_fg_b, ml_norm_g,
              ml_norm_b, ml_proj, rw_mu, rw_w0, rw_w_up, rw_a0, rw_a_up, rw_g_up, rw_k_k, rw_k_a, rw_r_k,
              rw_norm_g, rw_norm_b, rw_proj, s5_lam_re, s5_lam_im, s5_log_dt, s5_b_re, s5_b_im, s5_c_re,
              s5_c_im, s5_d, s5_w_val, s5_w_gate, w_out, ln1_g, ln1_b, ln2_g, ln2_b, router_w, router_b,
              exp_w_gate, exp_w_up, exp_w_down):
    silu_c = jax.nn.silu(c)[:, None, :]
    silu_cc = jax.nn.silu(c_ctx)[None, None, :]
    xc = ctx
    for i in range(DEPTH):
        need_ctx = i < DEPTH - 1
        mx = jnp.split(silu_c @ ada_w[i] + ada_b[i], N_MOD, axis=-1)
        mc = jnp.split(silu_cc @ ada_w[i] + ada_b[i], N_MOD, axis=-1)
        mix_c, mix_x = token_mixer(
            modulate(xc, mc[0], mc[1]), modulate(x, mx[0], mx[1]), need_ctx, w_in[i],
            ml_conv_w[i], ml_conv_b[i], ml_ig_b[i], ml_fg_b[i], ml_norm_g[i], ml_norm_b[i], ml_proj[i],
            rw_mu[i], rw_w0[i], rw_w_up[i], rw_a0[i], rw_a_up[i], rw_g_up[i], rw_k_k[i], rw_k_a[i], rw_r_k[i],
            rw_norm_g[i], rw_norm_b[i], rw_proj[i],
            s5_lam_re[i], s5_lam_im[i], s5_log_dt[i], s5_b_re[i], s5_b_im[i], s5_c_re[i], s5_c_im[i], s5_d[i],
            s5_w_val[i], s5_w_gate[i], w_out[i])
        x = post_norm(DEEPNORM_ALPHA * x + mx[2] * mix_x, ln1_g[i], ln1_b[i])
        tokens = [modulate(x, mx[3], mx[4]).reshape(-1, D_MODEL)]
        if need_ctx:
            xc = post_norm(DEEPNORM_ALPHA * xc + mc[2] * mix_c, ln1_g[i], ln1_b[i])
            tokens.append(modulate(xc, mc[3], mc[4]).reshape(-1, D_MODEL))
        ffn = moe_ffn(jnp.concatenate(tokens, axis=0), router_w, router_b, exp_w_gate[i], exp_w_up[i],
                      exp_w_down[i])
        n_lat = x.shape[0] * x.shape[1]
        x = post_norm(DEEPNORM_ALPHA * x + mx[5] * ffn[:n_lat].reshape(x.shape), ln2_g[i], ln2_b[i])
        if need_ctx:
            xc = post_norm(DEEPNORM_ALPHA * xc + mc[5] * ffn[n_lat:].reshape(xc.shape), ln2_g[i], ln2_b[i])
    return x
```

```python
import contextlib
import numpy as np
import concourse.bass as bass
import concourse.mybir as mybir
from concourse.bass_utils import run_bass_kernel_spmd
import math

F32 = mybir.dt.float32
BF16 = mybir.dt.bfloat16
ALU = mybir.AluOpType
AF = mybir.ActivationFunctionType
AX = mybir.AxisListType

class Res:
    __slots__ = ("name", "writer", "readers", "excl")
    def __init__(self, name, excl=False):
        self.name = name
        self.excl = excl
        self.writer = None
        self.readers = []

class KB:
    ENGS = ("pe", "act", "dve", "pool", "sp")
    def __init__(self, nc, n_dma_sems=10):
        self.nc = nc
        self.stack = contextlib.ExitStack()
        self.prog = {e: [] for e in self.ENGS}
        self.sem = {e: self.stack.enter_context(nc.semaphore("s_" + e)) for e in self.ENGS}
        self.cnt = {e: 0 for e in self.ENGS}
        self.dsem = {}
        self.dcnt = {}
        self.dnext = {}
        for q in ("sp", "act", "pool"):
            self.dsem[q] = [self.stack.enter_context(nc.semaphore(f"d_{q}{i}")) for i in range(n_dma_sems)]
            self.dcnt[q] = [0] * n_dma_sems
            self.dnext[q] = 0
        self.known = {e: {} for e in self.ENGS}
        self.nres = 0
        self.nalloc = 0

    def res(self, name=None, excl=False):
        self.nres += 1
        return Res(name or f"r{self.nres}", excl)
    def pres(self):
        return self.res(excl=True)

    def sbuf(self, name, shape, dtype, st=None):
        self.nalloc += 1
        t = (st or self.stack).enter_context(self.nc.sbuf_tensor(f"{name}_{self.nalloc}", list(shape), dtype))
        return t
    def psum(self, name, shape, dtype, st=None):
        assert dtype == F32
        self.nalloc += 1
        t = (st or self.stack).enter_context(self.nc.psum_tensor(f"{name}_{self.nalloc}", [128, 512], F32))
        n = 1
        for d in shape[1:]:
            n *= d
        assert n <= 512
        v = t[:shape[0], :n]
        if len(shape) == 3:
            v = v.rearrange("p (a b) -> p a b", b=shape[2])
        return v

    def _collect(self, e, reads, writes):
        waits = {}
        def add(sv):
            if sv is None:
                return
            s, v = sv
            k = id(s)
            if k not in waits or waits[k][1] < v:
                waits[k] = (s, v)
        for r in reads:
            add(r.writer)
        for w in writes:
            add(w.writer)
            for rd in w.readers:
                add(rd)
        out = []
        for k, (s, v) in waits.items():
            if e == "pe" and s is self.sem["pe"]:
                continue
            if self.known[e].get(k, 0) >= v:
                continue
            self.known[e][k] = v
            out.append((s, v))
        return out

    def op(self, e, fn, reads=(), writes=()):
        writes = list(writes) + [r for r in reads if r.excl]
        reads = [r for r in reads if not r.excl]
        waits = self._collect(e, reads, writes)
        self.cnt[e] += 1
        n = self.cnt[e]
        s = self.sem[e]
        self.prog[e].append((waits, fn, (s, 1)))
        for r in reads:
            r.readers.append((s, n))
            if len(r.readers) > 8:
                r.readers = self._compact(r.readers)
        for w in writes:
            w.writer = (s, n)
            w.readers = []
        return n

    @staticmethod
    def _compact(lst):
        best = {}
        for s, v in lst:
            k = id(s)
            if k not in best or best[k][1] < v:
                best[k] = (s, v)
        return list(best.values())

    def dma(self, q, out, in_, reads=(), writes=(), **kw):
        waits = self._collect(q, reads, writes)
        i = self.dnext[q]
        self.dnext[q] = (i + 1) % len(self.dsem[q])
        S = self.dsem[q][i]
        prev = self.dcnt[q][i]
        if prev > 0 and self.known[q].get(id(S), 0) < prev:
            self.known[q][id(S)] = prev
            waits.append((S, prev))
        val = prev + 16
        self.dcnt[q][i] = val
        self.prog[q].append((waits, lambda eng: eng.dma_start(out=out, in_=in_, **kw), (S, 16)))
        for r in reads:
            r.readers.append((S, val))
            if len(r.readers) > 8:
                r.readers = self._compact(r.readers)
        for w in writes:
            w.writer = (S, val)
            w.readers = []
        return (S, val)

    def barrier(self):
        targets = []
        for e in self.ENGS:
            if self.cnt[e] > 0:
                targets.append((self.sem[e], self.cnt[e]))
        for q in self.dsem:
            for S, v in zip(self.dsem[q], self.dcnt[q]):
                if v > 0:
                    targets.append((S, v))
        for e in self.ENGS:
            waits = []
            for s, v in targets:
                if s is self.sem[e]:
                    continue
                if self.known[e].get(id(s), 0) >= v:
                    continue
                self.known[e][id(s)] = v
                waits.append((s, v))
            if waits:
                self.prog[e].append((waits, None, None))

    def emit(self):
        self.barrier()
        nc = self.nc
        engmap = {"pe": "tensor", "act": "scalar", "dve": "vector", "pool": "gpsimd", "sp": "sync"}
        with nc.Block() as block:
            for e in self.ENGS:
                prog = self.prog[e]
                def body(eng, prog=prog):
                    for waits, fn, inc in prog:
                        for s, v in waits:
                            eng.wait_ge(s, v)
                        if fn is not None:
                            ins = fn(eng)
                            ins.then_inc(inc[0], inc[1])
                getattr(block, engmap[e])(body)
        self.stack.close()


D = 2048; KC = 16; NMOD = 6
LN_EPS = 1e-5

class SplitRows:
    def __init__(self, parts):
        self.parts = parts
    def pieces(self, r0, r1):
        out = []
        for (a, b, ap) in self.parts:
            lo, hi = max(a, r0), min(b, r1)
            if lo < hi:
                out.append((ap[lo - a:hi - a, :], lo - r0, hi - r0))
        return out

def phase_adaln(k, ada_w, ada_b_pc, c2, mod, rmod, ones_unused=None):
    nc = k.nc
    with contextlib.ExitStack() as st:
        ct = k.sbuf("ad_c", [128, KC, 2], F32, st); rc = k.res()
        cs = k.sbuf("ad_cs", [128, KC, 2], F32, st); rcs = k.res()
        bt = k.sbuf("ad_b", [128, 96], F32, st); rb = k.res()
        NB = 3
        wts = [k.sbuf(f"ad_w{i}", [128, KC, 128], F32, st) for i in range(NB)]
        rws = [k.res() for _ in range(NB)]
        pss = [k.psum(f"ad_ps{i}", [128, 2], F32, st) for i in range(2)]
        rps = [k.pres() for _ in range(2)]
        k.dma("sp", ct[:], c2, writes=[rc])
        k.dma("sp", bt[:], ada_b_pc, writes=[rb])
        k.op("act", lambda e: e.activation(out=cs[:], in_=ct[:], func=AF.Silu), reads=[rc], writes=[rcs])
        for j in range(96):
            wt, rw = wts[j % NB], rws[j % NB]
            ps, rp = pss[j % 2], rps[j % 2]
            k.dma(("sp", "pool")[j % 2], wt[:], ada_w[j], writes=[rw])
            for kc in range(KC):
                k.op("pe", lambda e, wt=wt, ps=ps, kc=kc: e.matmul(ps[:], lhsT=wt[:, kc, :], rhs=cs[:, kc, :],
                                                                    start=(kc == 0), stop=(kc == KC - 1)),
                     reads=[rw, rcs], writes=[rp])
            k.op("dve", lambda e, ps=ps, j=j: e.tensor_scalar(out=mod[:, j, :], in0=ps[:], scalar1=bt[:, j:j + 1],
                                                              scalar2=None, op0=ALU.add),
                 reads=[rp, rb], writes=[rmod])
        k.barrier()

def phase_ln_gemm(k, xT, w, outT, n_cols, T, blocks, mod, rmod, shift_idx, scale_idx, ones, rones, name="g1"):
    nc = k.nc
    TB = 512
    ncc = (n_cols + 127) // 128
    with contextlib.ExitStack() as st:
        xt = k.sbuf(name + "x", [128, KC, TB], F32, st); rx = k.res()
        sq = k.sbuf(name + "sq", [128, KC, TB], F32, st); rsq = k.res()
        u = k.sbuf(name + "u", [128, KC, TB], BF16, st); ru = k.res()
        mean = k.sbuf(name + "mean", [128, TB], F32, st); rmean = k.res()
        rstd = k.sbuf(name + "rstd", [128, TB], F32, st); rrstd = k.res()
        tmp = k.sbuf(name + "tmp", [128, KC, TB], F32, st); rtmp = k.res()
        sc1 = k.sbuf(name + "sc1", [128, KC, 2], F32, st); rsc1 = k.res()
        psm = k.psum(name + "psm", [128, TB], F32, st); rpsm = k.pres()
        pse = k.psum(name + "pse", [128, TB], F32, st); rpse = k.pres()
        NW = 3
        wf = [k.sbuf(f"{name}wf{i}", [128, KC, 128], F32, st) for i in range(NW)]; rwf = [k.res() for _ in range(NW)]
        wb = [k.sbuf(f"{name}wb{i}", [128, KC, 128], BF16, st) for i in range(NW)]; rwb = [k.res() for _ in range(NW)]
        NP = 3
        pso = [k.psum(f"{name}pso{i}", [128, TB], F32, st) for i in range(NP)]; rpso = [k.pres() for _ in range(NP)]
        ot = [k.sbuf(f"{name}ot{i}", [128, TB], F32, st) for i in range(NP)]; rot = [k.res() for _ in range(NP)]
        epst = k.sbuf(name + "eps", [128, 1], F32, st); reps = k.res()
        k.op("pool", lambda e: e.memset(epst[:], LN_EPS), writes=[reps])
        k.op("dve", lambda e: e.tensor_scalar(out=sc1[:], in0=mod[:, scale_idx * KC:(scale_idx + 1) * KC, :], scalar1=1.0,
                                              scalar2=None, op0=ALU.add), reads=[rmod], writes=[rsc1])
        xv = xT.rearrange("(kc p) t -> p kc t", p=128)
        it = 0
        for (t0, n, which) in blocks:
            k.dma("sp", xt[:, :, :n], xv[:, :, t0:t0 + n], writes=[rx])
            k.op("act", lambda e, n=n: e.activation(out=sq[:, :, :n], in_=xt[:, :, :n], func=AF.Square), reads=[rx], writes=[rsq])
            for kc in range(KC):
                k.op("pe", lambda e, kc=kc, n=n: e.matmul(psm[:, :n], lhsT=ones[:], rhs=xt[:, kc, :n], start=(kc == 0), stop=(kc == KC - 1)),
                     reads=[rx, rones], writes=[rpsm])
            for kc in range(KC):
                k.op("pe", lambda e, kc=kc, n=n: e.matmul(pse[:, :n], lhsT=ones[:], rhs=sq[:, kc, :n], start=(kc == 0), stop=(kc == KC - 1)),
                     reads=[rsq, rones], writes=[rpse])
            k.op("dve", lambda e, n=n: e.tensor_copy(out=mean[:, :n], in_=psm[:, :n]), reads=[rpsm], writes=[rmean])
            k.op("dve", lambda e, n=n: e.tensor_tensor(out=rstd[:, :n], in0=mean[:, :n], in1=mean[:, :n], op=ALU.mult), reads=[rmean], writes=[rrstd])
            k.op("dve", lambda e, n=n: e.tensor_tensor(out=rstd[:, :n], in0=pse[:, :n], in1=rstd[:, :n], op=ALU.subtract), reads=[rpse, rrstd], writes=[rrstd])
            k.op("act", lambda e, n=n: e.activation(out=rstd[:, :n], in_=rstd[:, :n], func=AF.Sqrt, bias=epst[:, 0:1]),
                 reads=[rrstd, reps], writes=[rrstd])
            k.op("dve", lambda e, n=n: e.reciprocal(out=rstd[:, :n], in_=rstd[:, :n]), reads=[rrstd], writes=[rrstd])
            k.op("dve", lambda e, n=n: e.tensor_tensor(out=tmp[:, :, :n], in0=xt[:, :, :n],
                                                       in1=mean[:, :n].unsqueeze(1).broadcast_to([128, KC, n]), op=ALU.subtract),
                 reads=[rx, rmean], writes=[rtmp])
            k.op("pool", lambda e, n=n: e.tensor_tensor(out=tmp[:, :, :n], in0=tmp[:, :, :n],
                                                        in1=rstd[:, :n].unsqueeze(1).broadcast_to([128, KC, n]), op=ALU.mult),
                 reads=[rtmp, rrstd], writes=[rtmp])
            for kc in range(KC):
                k.op(("dve", "act")[0], lambda e, kc=kc, n=n, which=which: e.tensor_scalar(
                    out=u[:, kc, :n], in0=tmp[:, kc, :n], scalar1=sc1[:, kc, which:which + 1],
                    scalar2=mod[:, shift_idx * KC + kc, which:which + 1], op0=ALU.mult, op1=ALU.add),
                    reads=[rtmp, rsc1, rmod], writes=[ru])
            for j in range(ncc):
                cw = min(128, n_cols - j * 128)
                b = it % NW
                k.dma(("act", "pool")[it % 2], wf[b][:, :, :cw], w[j][:, :, :cw], writes=[rwf[b]])
                k.op(("act", "pool")[it % 2], (lambda e, b=b, cw=cw: e.activation(out=wb[b][:, :, :cw], in_=wf[b][:, :, :cw], func=AF.Copy)) if it % 2 == 0
                     else (lambda e, b=b, cw=cw: e.tensor_copy(out=wb[b][:, :, :cw], in_=wf[b][:, :, :cw])),
                     reads=[rwf[b]], writes=[rwb[b]])
                p = it % NP
                for kc in range(KC):
                    k.op("pe", lambda e, kc=kc, b=b, p=p, cw=cw, n=n: e.matmul(pso[p][:cw, :n], lhsT=wb[b][:, kc, :cw], rhs=u[:, kc, :n],
                                                                            start=(kc == 0), stop=(kc == KC - 1)),
                         reads=[rwb[b], ru], writes=[rpso[p]])
                k.op("dve", lambda e, p=p, cw=cw, n=n: e.tensor_copy(out=ot[p][:cw, :n], in_=pso[p][:cw, :n]), reads=[rpso[p]], writes=[rot[p]])
                for (oap, a, b_) in outT.pieces(j * 128, j * 128 + cw):
                    k.dma("sp", oap[:, t0:t0 + n], ot[p][a:b_, :n], reads=[rot[p]])
                it += 1
        k.barrier()


GRID_W = 64; CTXL = 256

def phase_conv(k, PT, QK, cw_pc, cb_pc, T, name="cv"):
    R = (T - CTXL) // GRID_W
    with contextlib.ExitStack() as st:
        cw = k.sbuf(name + "w", [128, 16, 9], F32, st); rcw = k.res()
        cb = k.sbuf(name + "b", [128, 16], F32, st); rcb = k.res()
        k.dma("sp", cw[:], cw_pc, writes=[rcw]); k.dma("sp", cb[:], cb_pc, writes=[rcb])
        NB = 2
        zs = [k.sbuf(f"{name}z{i}", [128, T], F32, st) for i in range(NB)]; rz = [k.res() for _ in range(NB)]
        accs = [k.sbuf(f"{name}a{i}", [128, T], F32, st) for i in range(NB)]; ra = [k.res() for _ in range(NB)]
        for j in range(16):
            z, a = zs[j % NB], accs[j % NB]; rzj, raj = rz[j % NB], ra[j % NB]
            k.dma(("sp", "pool")[j % 2], z[:], PT[j * 128:(j + 1) * 128, :], writes=[rzj])
            k.op("act", lambda e, z=z, a=a, j=j: e.activation(out=a[:], in_=z[:], func=AF.Identity, scale=cw[:, j, 4:5], bias=cb[:, j:j + 1]),
                 reads=[rzj, rcw, rcb], writes=[raj])
            zl = z[:, CTXL:].rearrange("p (r c) -> p r c", c=GRID_W); al = a[:, CTXL:].rearrange("p (r c) -> p r c", c=GRID_W)
            for dy in (-1, 0, 1):
                for dx in (-1, 0, 1):
                    if dy == 0 and dx == 0:
                        continue
                    tap = (dy + 1) * 3 + (dx + 1)
                    r0, r1 = max(0, -dy), R - max(0, dy)
                    c0, c1 = max(0, -dx), GRID_W - max(0, dx)
                    if r1 > r0:
                        k.op("dve", lambda e, al=al, zl=zl, r0=r0, r1=r1, c0=c0, c1=c1, dy=dy, dx=dx, j=j, tap=tap: e.scalar_tensor_tensor(
                            out=al[:, r0:r1, c0:c1], in0=zl[:, r0 + dy:r1 + dy, c0 + dx:c1 + dx], scalar=cw[:, j, tap:tap + 1],
                            in1=al[:, r0:r1, c0:c1], op0=ALU.mult, op1=ALU.add), reads=[rzj, raj, rcw], writes=[raj])
                    if dy == 0:
                        k.op("dve", lambda e, a=a, z=z, c0=c0, dx=dx, j=j, tap=tap: e.scalar_tensor_tensor(
                            out=a[:, c0:CTXL - max(0, dx)], in0=z[:, c0 + dx:CTXL - max(0, dx) + dx], scalar=cw[:, j, tap:tap + 1],
                            in1=a[:, c0:CTXL - max(0, dx)], op0=ALU.mult, op1=ALU.add), reads=[rzj, raj, rcw], writes=[raj])
            k.op("act", lambda e, a=a: e.activation(out=a[:], in_=a[:], func=AF.Silu), reads=[raj], writes=[raj])
            k.dma(("sp", "pool")[(j + 1) % 2], QK[j * 128:(j + 1) * 128, :], a[:], reads=[raj])
        k.barrier()


L = 64; ML_H = 4; ML_DH = 256

def make_ml_consts():
    t = np.arange(64)
    tri_f = (t[:, None] <= t[None, :]).astype(np.float32)
    tri_b = (t[:, None] >= t[None, :]).astype(np.float32)
    neg_f = np.where(t[:, None] <= t[None, :], 0.0, -30000.0).astype(np.float32)
    neg_b = np.where(t[:, None] >= t[None, :], 0.0, -30000.0).astype(np.float32)
    c = np.zeros((128, 4, 64), np.float32)
    c[:64, 0] = tri_f; c[:64, 1] = tri_b; c[:64, 2] = neg_f; c[:64, 3] = neg_b
    return c

def phase_mlstm(k, PT, QK, HD, igb_bc, fgb_bc, mlc, ident, T, name="mls_"):
    NCH = T // L
    order = {0: list(range(NCH)), 1: [3, 2, 1, 0] + list(range(NCH - 1, 3, -1))}
    with contextlib.ExitStack() as st:
        cst = k.sbuf(name + "c", [128, 4, 64], F32, st); rcst = k.res()
        idt = k.sbuf(name + "id", [128, 128], F32, st); rid = k.res()
        ones = k.sbuf(name + "ones", [64, 128], F32, st); rones = k.res()
        igb = k.sbuf(name + "igb", [64, 8], F32, st); fgb = k.sbuf(name + "fgb", [64, 8], F32, st); rgb = k.res()
        gT = k.sbuf(name + "gT", [16, T], F32, st); rgT = k.res()
        LI = k.sbuf(name + "LI", [64, NCH, 8], F32, st); LF = k.sbuf(name + "LF", [64, NCH, 8], F32, st); rLI = k.res(); rLF = k.res()
        one1 = k.sbuf(name + "one1", [64, 1], F32, st); rone1 = k.res()
        k.dma("sp", cst[:], mlc, writes=[rcst]); k.dma("sp", idt[:], ident, writes=[rid])
        k.dma("sp", igb[:], igb_bc, writes=[rgb]); k.dma("sp", fgb[:], fgb_bc, writes=[rgb])
        k.dma("sp", gT[:], PT[4096:4112, :], writes=[rgT])
        k.op("pool", lambda e: e.memset(ones[:], 1.0), writes=[rones])
        k.op("pool", lambda e: e.memset(one1[:], 1.0), writes=[rone1])
        pst = k.psum(name + "pst", [64, 512], F32, st); rpst = k.pres()
        for c0 in range(0, NCH, 32):
            nb = min(32, NCH - c0)
            for c in range(c0, c0 + nb):
                k.op("pe", lambda e, c=c, c0=c0: e.transpose(out=pst[:, (c - c0) * 16:(c - c0 + 1) * 16], in_=gT[:, c * L:(c + 1) * L], identity=idt[:16, :16]),
                     reads=[rgT, rid], writes=[rpst])
            pv = pst[:, :nb * 16].rearrange("p (c g) -> p c g", g=16)
            k.op("dve", lambda e, pv=pv, c0=c0, nb=nb: e.tensor_tensor(out=LI[:, c0:c0 + nb, :], in0=pv[:, :, 0:8],
                                                                       in1=igb[:].unsqueeze(1).broadcast_to([64, nb, 8]), op=ALU.add),
                 reads=[rpst, rgb], writes=[rLI])
            k.op("dve", lambda e, pv=pv, c0=c0, nb=nb: e.tensor_tensor(out=LF[:, c0:c0 + nb, :], in0=pv[:, :, 8:16],
                                                                       in1=fgb[:].unsqueeze(1).broadcast_to([64, nb, 8]), op=ALU.add),
                 reads=[rpst, rgb], writes=[rLF])
        k.op("act", lambda e: e.activation(out=LF[:], in_=LF[:], func=AF.Exp, scale=-1.0), reads=[rLF], writes=[rLF])
        k.op("act", lambda e: e.activation(out=LF[:], in_=LF[:], func=AF.Ln, bias=one1[:, 0:1]), reads=[rLF, rone1], writes=[rLF])
        k.op("dve", lambda e: e.tensor_scalar(out=LF[:], in0=LF[:], scalar1=-1.0, scalar2=None, op0=ALU.mult), reads=[rLF], writes=[rLF])

        STOP = 99
        if STOP == 0:
            k.barrier(); return
        BLK = 8
        NB = 2
        qb = [k.sbuf(f"{name}q{i}", [128, 2, BLK * L], F32, st) for i in range(NB)]
        kb_ = [k.sbuf(f"{name}k{i}", [128, 2, BLK * L], F32, st) for i in range(NB)]
        vb = [k.sbuf(f"{name}v{i}", [128, 2, BLK * L], F32, st) for i in range(NB)]
        hb = [k.sbuf(f"{name}h{i}", [128, 2, BLK * L], F32, st) for i in range(NB)]
        rq = [k.res() for _ in range(NB)]; rk = [k.res() for _ in range(NB)]; rv = [k.res() for _ in range(NB)]; rh = [k.res() for _ in range(NB)]
        CT = k.sbuf(name + "CT", [128, 2, 257], F32, st); rCT = k.res()
        ktm = k.sbuf(name + "ktm", [64, 256], F32, st); rktm = k.res()
        vtm = k.sbuf(name + "vtm", [64, 257], F32, st); rvtm = k.res()
        vw = k.sbuf(name + "vw", [64, 257], F32, st); rvw = k.res()
        col = k.sbuf(name + "col", [64, 1], F32, st); rcol = k.res()
        wts = k.sbuf(name + "wts", [64, 1], F32, st); rwts = k.res()
        bl = k.sbuf(name + "bl", [128, 1], F32, st); rbl = k.res()
        ebl = k.sbuf(name + "ebl", [128, 1], F32, st); rebl = k.res()
        tmp = k.sbuf(name + "tmp", [64, 64], F32, st); rtmp = k.res()
        DT = k.sbuf(name + "DT", [64, 64], F32, st); rDT = k.res()
        ScT = k.sbuf(name + "ScT", [64, 64], F32, st); rScT = k.res()
        ebc = k.sbuf(name + "ebc", [128, 64], F32, st); rebc = k.res()
        qs = k.sbuf(name + "qs", [128, 2, 64], F32, st); rqs = k.res()
        rden = k.sbuf(name + "rden", [128, 64], F32, st); rrden = k.res()
        p_bc = k.psum(name + "pbc", [128, 64], F32, st); rpbc = k.pres()
        p_col = k.psum(name + "pcol", [64, 1], F32, st); rpcol = k.pres()
        p_sc = k.psum(name + "psc", [64, 64], F32, st); rpsc = k.pres()
        p_nm = k.psum(name + "pnm", [128, 3, 64], F32, st); rpnm = k.pres()
        p_dc = [k.psum(f"{name}pdc{i}", [128, 257], F32, st) for i in range(2)]; rpdc = [k.pres() for _ in range(2)]
        p_tr = k.psum(name + "ptr", [64, 512], F32, st); rptr = k.pres()
        k.op("pool", lambda e: e.memset(vtm[:, 256:257], 1.0), writes=[rvtm])
        it = 0
        for h in range(ML_H):
            for d in range(2):
                tri = cst[:64, d, :]; neg = cst[:64, 2 + d, :]
                last = L - 1 if d == 0 else 0
                g = d * 4 + h
                k.op("pool", lambda e: e.memset(CT[:], 0.0), writes=[rCT])
                chunks = order[d]
                groups = []
                i = 0
                while i < len(chunks):
                    grp = [chunks[i]]
                    while len(grp) < BLK and i + len(grp) < len(chunks) and abs(chunks[i + len(grp)] - grp[-1]) == 1 \
                            and (chunks[i + len(grp)] // BLK == grp[0] // BLK):
                        grp.append(chunks[i + len(grp)])
                    groups.append(grp); i += len(grp)
                for gi, grp in enumerate(groups):
                    if STOP == 1 and (h, d, gi) != (0, 0, 0): continue
                    lo = min(grp); n = len(grp)
                    b = it % NB; it += 1
                    t0 = lo * L; tn = n * L
                    qv = QK[h * 256:(h + 1) * 256, t0:t0 + tn].rearrange("(dc p) t -> p dc t", p=128)
                    kv = QK[1024 + h * 256:1024 + (h + 1) * 256, t0:t0 + tn].rearrange("(dc p) t -> p dc t", p=128)
                    vv = PT[2048 + h * 256:2048 + (h + 1) * 256, t0:t0 + tn].rearrange("(dc p) t -> p dc t", p=128)
                    k.dma("sp", qb[b][:, :, :tn], qv, writes=[rq[b]])
                    k.dma("act", kb_[b][:, :, :tn], kv, writes=[rk[b]])
                    k.dma("pool", vb[b][:, :, :tn], vv, writes=[rv[b]])
                    k.op("act", lambda e, b=b, tn=tn: e.mul(out=kb_[b][:, :, :tn], in_=kb_[b][:, :, :tn], mul=1.0 / 16.0), reads=[rk[b]], writes=[rk[b]])
                    for c in grp:
                        if STOP < 50:
                            break
                        o = (c - lo) * L
                        lfc = LF[:, c, g:g + 1]; lic = LI[:, c, g:g + 1]
                        for dc in range(2):
                            k.op("pe", lambda e, b=b, dc=dc, o=o: e.transpose(out=p_tr[:, dc * 128:(dc + 1) * 128], in_=kb_[b][:, dc, o:o + L], identity=idt[:]),
                                 reads=[rk[b], rid], writes=[rptr])
                            k.op("pe", lambda e, b=b, dc=dc, o=o: e.transpose(out=p_tr[:, 256 + dc * 128:256 + (dc + 1) * 128], in_=vb[b][:, dc, o:o + L], identity=idt[:]),
                                 reads=[rv[b], rid], writes=[rptr])
                        k.op("dve", lambda e: e.tensor_copy(out=ktm[:], in_=p_tr[:, 0:256]), reads=[rptr], writes=[rktm])
                        k.op("act", lambda e: e.copy(out=vtm[:, 0:256], in_=p_tr[:, 256:512]), reads=[rptr], writes=[rvtm])
                        if STOP <= 51: continue
                        k.op("pe", lambda e, lfc=lfc, tri=tri: e.matmul(p_bc[:], lhsT=lfc.broadcast_to([64, 128]), rhs=tri, start=True, stop=True),
                             reads=[rLF, rcst], writes=[rpbc])
                        k.op("pe", lambda e, lfc=lfc, tri=tri: e.matmul(p_col[:], lhsT=tri, rhs=lfc, start=True, stop=True),
                             reads=[rLF, rcst], writes=[rpcol])
                        k.op("dve", lambda e, lic=lic: e.tensor_tensor(out=col[:], in0=lic, in1=p_col[:], op=ALU.subtract), reads=[rLI, rpcol], writes=[rcol])
                        k.op("dve", lambda e, neg=neg: e.tensor_tensor(out=tmp[:], in0=p_bc[:64, :], in1=neg, op=ALU.add), reads=[rpbc, rcst], writes=[rtmp])
                        k.op("act", lambda e: e.activation(out=DT[:], in_=tmp[:], func=AF.Exp, bias=col[:, 0:1]), reads=[rtmp, rcol], writes=[rDT])
                        k.op("act", lambda e: e.activation(out=ebc[:], in_=p_bc[:], func=AF.Exp), reads=[rpbc], writes=[rebc])
                        k.op("dve", lambda e, last=last: e.tensor_copy(out=bl[:], in_=p_bc[:, last:last + 1]), reads=[rpbc], writes=[rbl])
                        if STOP <= 53: continue
                        for dc in range(2):
                            k.op("pe", lambda e, b=b, dc=dc, o=o: e.matmul(p_sc[:], lhsT=kb_[b][:, dc, o:o + L], rhs=qb[b][:, dc, o:o + L], start=(dc == 0), stop=(dc == 1)),
                                 reads=[rk[b], rq[b]], writes=[rpsc])
                        k.op("dve", lambda e: e.tensor_tensor(out=ScT[:], in0=p_sc[:], in1=DT[:], op=ALU.mult), reads=[rpsc, rDT], writes=[rScT])
                        k.op("pool", lambda e, b=b, o=o: e.tensor_tensor(out=qs[:], in0=qb[b][:, :, o:o + L], in1=ebc[:].unsqueeze(1).broadcast_to([128, 2, 64]), op=ALU.mult),
                             reads=[rq[b], rebc], writes=[rqs])
                        if STOP <= 54: continue
                        for m in range(3):
                            lhs_i = vtm[:, m * 128:(m + 1) * 128] if m < 2 else vtm[:, 256:257].broadcast_to([64, 128])
                            k.op("pe", lambda e, m=m, lhs_i=lhs_i: e.matmul(p_nm[:, m, :], lhsT=lhs_i, rhs=ScT[:], start=True, stop=False),
                                 reads=[rvtm, rScT], writes=[rpnm])
                            for dc in range(2):
                                lhs_c = CT[:, dc, m * 128:(m + 1) * 128] if m < 2 else CT[:, dc, 256:257].broadcast_to([128, 128])
                                k.op("pe", lambda e, m=m, dc=dc, lhs_c=lhs_c: e.matmul(p_nm[:, m, :], lhsT=lhs_c, rhs=qs[:, dc, :], start=False, stop=(dc == 1)),
                                     reads=[rCT, rqs], writes=[rpnm])
                        if STOP <= 55: continue
                        k.op("act", lambda e: e.activation(out=rden[:], in_=p_nm[:, 2, :], func=AF.Abs), reads=[rpnm], writes=[rrden])
                        k.op("dve", lambda e: e.tensor_scalar(out=rden[:], in0=rden[:], scalar1=1.0, scalar2=None, op0=ALU.max), reads=[rrden], writes=[rrden])
                        k.op("dve", lambda e: e.reciprocal(out=rden[:], in_=rden[:]), reads=[rrden], writes=[rrden])
                        k.op("dve", lambda e, b=b, o=o: e.tensor_tensor(out=hb[b][:, :, o:o + L], in0=p_nm[:, 0:2, :],
                                                                          in1=rden[:].unsqueeze(1).broadcast_to([128, 2, 64]), op=ALU.mult),
                             reads=[rpnm, rrden], writes=[rh[b]])
                        if STOP <= 56: continue
                        k.op("act", lambda e: e.activation(out=wts[:], in_=col[:], func=AF.Exp, bias=bl[:64, 0:1]), reads=[rcol, rbl], writes=[rwts])
                        k.op("act", lambda e: e.activation(out=ebl[:], in_=bl[:], func=AF.Exp), reads=[rbl], writes=[rebl])
                        k.op("dve", lambda e: e.tensor_scalar(out=vw[:], in0=vtm[:], scalar1=wts[:, 0:1], scalar2=None, op0=ALU.mult), reads=[rvtm, rwts], writes=[rvw])
                        for dc in range(2):
                            k.op("pe", lambda e, dc=dc: e.matmul(p_dc[dc][:], lhsT=ktm[:, dc * 128:(dc + 1) * 128], rhs=vw[:], start=True, stop=True),
                                 reads=[rktm, rvw], writes=[rpdc[dc]])
                            k.op("dve", lambda e, dc=dc: e.scalar_tensor_tensor(out=CT[:, dc, :], in0=CT[:, dc, :], scalar=ebl[:, 0:1], in1=p_dc[dc][:],
                                                                                 op0=ALU.mult, op1=ALU.add), reads=[rCT, rebl, rpdc[dc]], writes=[rCT])
                    hv = HD[d, h * 256:(h + 1) * 256, t0:t0 + tn].rearrange("(dc p) t -> p dc t", p=128)
                    k.dma("sp", hv, hb[b][:, :, :tn], reads=[rh[b]])
        k.barrier()


L = 64; RW_H = 16

def make_rw_consts():
    t = np.arange(64)
    c = np.zeros((64, 2, 384), np.float32)
    for d in range(2):
        before = (t[:, None] < t[None, :]) if d == 0 else (t[:, None] > t[None, :])
        beq = (t[:, None] <= t[None, :]) if d == 0 else (t[:, None] >= t[None, :])
        c[:, d, 0:64] = before; c[:, d, 64:128] = beq; c[:, d, 128:192] = before; c[:, d, 192:256] = beq
        c[:, d, 256:320] = before.T
        c[:, d, 320:384] = beq
    return c

def phase_rwkv_scan(k, R, KK, V, LW, KD, A, YD, rwc, ident, T, name="rws_"):
    NCH = T // L
    order = {0: list(range(NCH)), 1: [3, 2, 1, 0] + list(range(NCH - 1, 3, -1))}
    H16 = RW_H
    with contextlib.ExitStack() as st:
        cst = k.sbuf(name + "c", [64, 2, 384], F32, st); rcst = k.res()
        idt = k.sbuf(name + "id", [64, 64], F32, st); rid = k.res()
        k.dma("sp", cst[:], rwc, writes=[rcst]); k.dma("sp", idt[:], ident[0:64, 0:64], writes=[rid])
        NB = 2
        def tl(nm, shape):
            return [k.sbuf(f"{name}{nm}{i}", shape, F32, st) for i in range(NB)], [k.res() for _ in range(NB)]
        lw_, rlw = tl("lw", [64, H16, L]); kd_, rkd = tl("kd", [64, H16, L]); a_, ra = tl("a", [64, H16, L])
        r_, rr = tl("r", [64, H16, L]); kk_, rkk = tl("kk", [64, H16, L]); v_, rv = tl("v", [64, H16, L])
        KR, rKR = tl("KR", [64, H16, 2, L]); KBt, rKB = tl("KB", [64, H16, 2, L])
        Wt, rW = tl("W", [64, H16, L]); Wi, rWi = tl("Wi", [64, H16, L]); Wp, rWp = tl("Wp", [64, H16, L])
        lwtm, rlwtm = tl("lwtm", [64, H16 * 64]); khtm, rkhtm = tl("khtm", [64, H16 * 64])
        nbtm, rnbtm = tl("nbtm", [64, H16 * 64]); vtm, rvtm = tl("vtm", [64, H16 * 64])
        yb, ryb = tl("yb", [64, H16, L])
        HS = k.sbuf(name + "HS", [64, 2, H16, 64], F32, st); rHS = [[k.res() for _ in range(H16)] for _ in range(2)]
        k.op("pool", lambda e: e.memset(HS[:], 0.0), writes=[rHS[d][h] for d in range(2) for h in range(H16)])
        NH = 2
        GT = [k.sbuf(f"{name}GT{i}", [64, 320], F32, st) for i in range(NH)]; rGT = [k.res() for _ in range(NH)]
        GTr = [k.sbuf(f"{name}GTr{i}", [64, 320], F32, st) for i in range(NH)]; rGTr = [k.res() for _ in range(NH)]
        MN = [k.sbuf(f"{name}MN{i}", [64, 128], F32, st) for i in range(NH)]; rMN = [k.res() for _ in range(NH)]
        MN2 = [k.sbuf(f"{name}MNb{i}", [64, 128], F32, st) for i in range(NH)]; rMN2 = [k.res() for _ in range(NH)]
        Pm = [k.sbuf(f"{name}P{i}", [64, 64], F32, st) for i in range(NH)]; rPm = [k.res() for _ in range(NH)]
        RHS = [k.sbuf(f"{name}RHS{i}", [64, 64], F32, st) for i in range(NH)]; rRHS = [k.res() for _ in range(NH)]
        Ut = [k.sbuf(f"{name}U{i}", [64, 64], F32, st) for i in range(NH)]; rUt = [k.res() for _ in range(NH)]
        Htmp = [k.sbuf(f"{name}Ht{i}", [64, 64], F32, st) for i in range(NH)]; rHt = [k.res() for _ in range(NH)]
        ptA = k.psum(name + "ptA", [64, 512], F32, st); rptA = k.pres()
        ptB = k.psum(name + "ptB", [64, 512], F32, st); rptB = k.pres()
        pG = [k.psum(f"{name}pG{i}", [64, 320], F32, st) for i in range(2)]; rpG = [k.pres() for _ in range(2)]
        pI = [k.psum(f"{name}pI{i}", [64, 192], F32, st) for i in range(2)]; rpI = [k.pres() for _ in range(2)]
        pS = [k.psum(f"{name}pS{i}", [64, 256], F32, st) for i in range(2)]; rpS = [k.pres() for _ in range(2)]
        pt = [ptA, ptB]; rpt = [rptA, rptB]

        def transpose16(src_fn, dst, rsrc, rdst):
            for half in range(2):
                p, rp = pt[half], rpt[half]
                for hh in range(8):
                    h = half * 8 + hh
                    k.op("pe", lambda e, p=p, hh=hh, h=h: e.transpose(out=p[:, hh * 64:(hh + 1) * 64], in_=src_fn(h), identity=idt[:]),
                         reads=[rsrc, rid], writes=[rp])
                k.op(("act", "dve")[half], (lambda e, p=p, half=half: e.copy(out=dst[:, half * 512:(half + 1) * 512], in_=p[:])) if half == 0 else
                     (lambda e, p=p, half=half: e.tensor_copy(out=dst[:, half * 512:(half + 1) * 512], in_=p[:])), reads=[rp], writes=[rdst])

        it = 0
        for s in range(NCH):
            for d in range(2):
                c = order[d][s]; t0 = c * L
                b = it % NB; it += 1
                last = L - 1 if d == 0 else 0
                def ld(q, dst, src, rdst):
                    k.dma(q, dst[:], src.rearrange("(h p) t -> p h t", p=64)[:, :, t0:t0 + L], writes=[rdst])
                ld("sp", lw_[b], LW[d], rlw[b]); ld("act", kd_[b], KD[d], rkd[b]); ld("pool", a_[b], A[d], ra[b])
                ld("sp", r_[b], R, rr[b]); ld("act", kk_[b], KK, rkk[b]); ld("pool", v_[b], V, rv[b])
                transpose16(lambda h, b=b: lw_[b][:, h, :], lwtm[b], rlw[b], rlwtm[b])
                tri = cst[:, d, 320:384]
                for half in range(2):
                    p, rp = pt[half], rpt[half]
                    for hh in range(8):
                        h = half * 8 + hh
                        k.op("pe", lambda e, p=p, hh=hh, h=h, b=b, tri=tri: e.matmul(p[:, hh * 64:(hh + 1) * 64], lhsT=lwtm[b][:, h * 64:(h + 1) * 64], rhs=tri, start=True, stop=True),
                             reads=[rlwtm[b], rcst], writes=[rp])
                    hs = slice(half * 8, half * 8 + 8)
                    pv = p.rearrange("p (h t) -> p h t", t=L)
                    k.op("act", lambda e, pv=pv, hs=hs, b=b: e.activation(out=Wt[b][:, hs, :], in_=pv, func=AF.Exp), reads=[rp], writes=[rW[b]])
                    k.op("act", lambda e, pv=pv, hs=hs, b=b: e.activation(out=Wi[b][:, hs, :], in_=pv, func=AF.Exp, scale=-1.0), reads=[rp], writes=[rWi[b]])
                    k.op("dve", lambda e, pv=pv, hs=hs, b=b: e.tensor_tensor(out=Wp[b][:, hs, :], in0=pv, in1=lw_[b][:, hs, :], op=ALU.subtract), reads=[rp, rlw[b]], writes=[rWp[b]])
                k.op("act", lambda e, b=b: e.activation(out=Wp[b][:], in_=Wp[b][:], func=AF.Exp), reads=[rWp[b]], writes=[rWp[b]])
                k.op("pool", lambda e, b=b: e.tensor_tensor(out=KR[b][:, :, 0, :], in0=kk_[b][:], in1=Wp[b][:], op=ALU.mult), reads=[rkk[b], rWp[b]], writes=[rKR[b]])
                k.op("pool", lambda e, b=b: e.tensor_tensor(out=KR[b][:, :, 1, :], in0=r_[b][:], in1=Wt[b][:], op=ALU.mult), reads=[rr[b], rW[b]], writes=[rKR[b]])
                k.op("dve", lambda e, b=b: e.tensor_tensor(out=KBt[b][:, :, 0, :], in0=kd_[b][:], in1=Wi[b][:], op=ALU.mult), reads=[rkd[b], rWi[b]], writes=[rKB[b]])
                k.op("pool", lambda e, b=b: e.tensor_tensor(out=a_[b][:], in0=a_[b][:], in1=kk_[b][:], op=ALU.mult), reads=[ra[b], rkk[b]], writes=[ra[b]])
                k.op("dve", lambda e, b=b: e.scalar_tensor_tensor(out=KBt[b][:, :, 1, :], in0=a_[b][:], scalar=-1.0, in1=Wi[b][:], op0=ALU.mult, op1=ALU.mult),
                     reads=[ra[b], rWi[b]], writes=[rKB[b]])
                transpose16(lambda h, b=b: KBt[b][:, h, 0, :], khtm[b], rKB[b], rkhtm[b])
                transpose16(lambda h, b=b: KBt[b][:, h, 1, :], nbtm[b], rKB[b], rnbtm[b])
                transpose16(lambda h, b=b: v_[b][:, h, :], vtm[b], rv[b], rvtm[b])
                for h in range(H16):
                    q = h % NH
                    hcs = slice(h * 64, (h + 1) * 64)
                    Hst = HS[:, d, h, :]; rH = rHS[d][h]
                    krf = KR[b][:, h, :, :].rearrange("p a t -> p (a t)")
                    g = h % 2
                    k.op("pe", lambda e, g=g, b=b, h=h, krf=krf: e.matmul(pG[g][:, 0:128], lhsT=KBt[b][:, h, 0, :], rhs=krf, start=True, stop=True),
                         reads=[rKB[b], rKR[b]], writes=[rpG[g]])
                    k.op("pe", lambda e, g=g, b=b, h=h, krf=krf: e.matmul(pG[g][:, 128:256], lhsT=KBt[b][:, h, 1, :], rhs=krf, start=True, stop=True),
                         reads=[rKB[b], rKR[b]], writes=[rpG[g]])
                    k.op("pe", lambda e, g=g, b=b, h=h: e.matmul(pG[g][:, 256:320], lhsT=KR[b][:, h, 0, :], rhs=KBt[b][:, h, 1, :], start=True, stop=True),
                         reads=[rKB[b], rKR[b]], writes=[rpG[g]])
                    k.op("act", lambda e, g=g, q=q: e.copy(out=GTr[q][:], in_=pG[g][:]), reads=[rpG[g]], writes=[rGTr[q]])
                    k.op("pool", lambda e, q=q, d=d: e.tensor_tensor(out=GT[q][:], in0=GTr[q][:], in1=cst[:, d, 0:320], op=ALU.mult), reads=[rGTr[q], rcst], writes=[rGT[q]])
                    k.op("pool", lambda e, q=q: e.tensor_tensor(out=Pm[q][:], in0=GT[q][:, 128:192], in1=idt[:], op=ALU.add), reads=[rGT[q], rid], writes=[rPm[q]])
                    Mc, Nc, rMc = GT[q][:, 128:192], GT[q][:, 256:320], rGT[q]
                    for lev in range(5):
                        dstt, rdst = (MN, rMN) if lev % 2 == 0 else (MN2, rMN2)
                        pi = pI[lev % 2]; rpi = rpI[lev % 2]
                        k.op("pe", lambda e, pi=pi, Mc=Mc, Nc=Nc: e.matmul(pi[:, 0:64], lhsT=Nc, rhs=Mc, start=True, stop=True), reads=[rMc], writes=[rpi])
                        k.op("pe", lambda e, pi=pi, Mc=Mc, Nc=Nc: e.matmul(pi[:, 64:128], lhsT=Mc, rhs=Nc, start=True, stop=True), reads=[rMc], writes=[rpi])
                        k.op("act", lambda e, pi=pi, dstt=dstt, q=q: e.copy(out=dstt[q][:], in_=pi[:, 0:128]), reads=[rpi], writes=[rdst[q]])
                        Mc, Nc, rMc = dstt[q][:, 0:64], dstt[q][:, 64:128], rdst[q]
                        k.op("pe", lambda e, pi=pi, Nc=Nc, q=q: e.matmul(pi[:, 128:192], lhsT=Nc, rhs=Pm[q][:], start=True, stop=True), reads=[rMc, rPm[q]], writes=[rpi])
                        k.op("dve", lambda e, pi=pi, q=q: e.tensor_tensor(out=Pm[q][:], in0=Pm[q][:], in1=pi[:, 128:192], op=ALU.add), reads=[rpi, rPm[q]], writes=[rPm[q]])
                    ps = pS[h % 2]; rps = rpS[h % 2]
                    k.op("pe", lambda e, ps=ps, b=b, h=h, Hst=Hst: e.matmul(ps[:, 0:64], lhsT=KR[b][:, h, 0, :], rhs=Hst, start=True, stop=False), reads=[rKR[b], rH], writes=[rps])
                    k.op("pe", lambda e, ps=ps, b=b, q=q, hcs=hcs: e.matmul(ps[:, 0:64], lhsT=GT[q][:, 0:64], rhs=vtm[b][:, hcs], start=False, stop=True), reads=[rGT[q], rvtm[b]], writes=[rps])
                    k.op("act", lambda e, ps=ps, q=q: e.copy(out=RHS[q][:], in_=ps[:, 0:64]), reads=[rps], writes=[rRHS[q]])
                    k.op("pe", lambda e, ps=ps, q=q: e.matmul(ps[:, 64:128], lhsT=Pm[q][:], rhs=RHS[q][:], start=True, stop=True), reads=[rPm[q], rRHS[q]], writes=[rps])
                    k.op("dve", lambda e, ps=ps, q=q: e.tensor_copy(out=Ut[q][:], in_=ps[:, 64:128]), reads=[rps], writes=[rUt[q]])
                    k.op("pe", lambda e, ps=ps, b=b, h=h, Hst=Hst: e.matmul(ps[:, 128:192], lhsT=Hst, rhs=KR[b][:, h, 1, :], start=True, stop=False), reads=[rH, rKR[b]], writes=[rps])
                    k.op("pe", lambda e, ps=ps, b=b, q=q, hcs=hcs: e.matmul(ps[:, 128:192], lhsT=vtm[b][:, hcs], rhs=GT[q][:, 64:128], start=False, stop=False), reads=[rvtm[b], rGT[q]], writes=[rps])
                    k.op("pe", lambda e, ps=ps, q=q: e.matmul(ps[:, 128:192], lhsT=Ut[q][:], rhs=GT[q][:, 192:256], start=False, stop=True), reads=[rUt[q], rGT[q]], writes=[rps])
                    k.op("act", lambda e, ps=ps, b=b, h=h: e.copy(out=yb[b][:, h, :], in_=ps[:, 128:192]), reads=[rps], writes=[ryb[b]])
                    k.op("pe", lambda e, ps=ps, b=b, hcs=hcs: e.matmul(ps[:, 192:256], lhsT=khtm[b][:, hcs], rhs=vtm[b][:, hcs], start=True, stop=False), reads=[rkhtm[b], rvtm[b]], writes=[rps])
                    k.op("pe", lambda e, ps=ps, b=b, q=q, hcs=hcs: e.matmul(ps[:, 192:256], lhsT=nbtm[b][:, hcs], rhs=Ut[q][:], start=False, stop=True), reads=[rnbtm[b], rUt[q]], writes=[rps])
                    k.op("dve", lambda e, ps=ps, q=q, Hst=Hst: e.tensor_tensor(out=Htmp[q][:], in0=Hst, in1=ps[:, 192:256], op=ALU.add), reads=[rps, rH], writes=[rHt[q]])
                    k.op("pool", lambda e, q=q, Hst=Hst, b=b, h=h, last=last: e.tensor_scalar(out=Hst, in0=Htmp[q][:], scalar1=Wt[b][:, h, last:last + 1], scalar2=None, op0=ALU.mult),
                         reads=[rHt[q], rW[b]], writes=[rH])
                k.dma("sp", YD[d].rearrange("(h p) t -> p h t", p=64)[:, :, t0:t0 + L], yb[b][:], reads=[ryb[b]])
        k.barrier()


CTXL = 256
RW0 = 4112

def _blocks(T, TB=512):
    return [(0, CTXL)] + [(t, min(TB, T - t)) for t in range(CTXL, T, TB)]

def head_ln(k, x, rx, n, nch, onesb, rones, eps_ap, reps, psm, rpsm, pse, rpse, sq, rsq, mean, rmean, rstd, rrstd, groups):
    k.op("act", lambda e: e.activation(out=sq[:, :nch, :n], in_=x[:, :nch, :n], func=AF.Square), reads=[rx], writes=[rsq])
    for grp in groups:
        for i, ch in enumerate(grp):
            k.op("pe", lambda e, ch=ch, i=i, grp=grp: e.matmul(psm[:, :n], lhsT=onesb[:], rhs=x[:, ch, :n], start=(i == 0), stop=(i == len(grp) - 1)),
                 reads=[rx, rones], writes=[rpsm])
        for i, ch in enumerate(grp):
            k.op("pe", lambda e, ch=ch, i=i, grp=grp: e.matmul(pse[:, :n], lhsT=onesb[:], rhs=sq[:, ch, :n], start=(i == 0), stop=(i == len(grp) - 1)),
                 reads=[rsq, rones], writes=[rpse])
        k.op("dve", lambda e: e.tensor_copy(out=mean[:, :n], in_=psm[:, :n]), reads=[rpsm], writes=[rmean])
        k.op("dve", lambda e: e.tensor_tensor(out=rstd[:, :n], in0=mean[:, :n], in1=mean[:, :n], op=ALU.mult), reads=[rmean], writes=[rrstd])
        k.op("dve", lambda e: e.tensor_tensor(out=rstd[:, :n], in0=pse[:, :n], in1=rstd[:, :n], op=ALU.subtract), reads=[rpse, rrstd], writes=[rrstd])
        k.op("act", lambda e: e.activation(out=rstd[:, :n], in_=rstd[:, :n], func=AF.Sqrt, bias=eps_ap), reads=[rrstd, reps], writes=[rrstd])
        k.op("dve", lambda e: e.reciprocal(out=rstd[:, :n], in_=rstd[:, :n]), reads=[rrstd], writes=[rrstd])
        for ch in grp:
            k.op("dve", lambda e, ch=ch: e.tensor_tensor(out=x[:, ch, :n], in0=x[:, ch, :n], in1=mean[:, :n], op=ALU.subtract), reads=[rx, rmean], writes=[rx])
            k.op("pool", lambda e, ch=ch: e.tensor_tensor(out=x[:, ch, :n], in0=x[:, ch, :n], in1=rstd[:, :n], op=ALU.mult), reads=[rx, rrstd], writes=[rx])

def phase_ml_finish(k, HD, PT, YML, ng_pc, nb_pc, T, name="mlf_"):
    with contextlib.ExitStack() as st:
        TB = 512
        onesb = k.sbuf(name + "ones", [128, 128], F32, st); rones = k.res()
        k.op("pool", lambda e: e.memset(onesb[:], 1.0 / 256.0), writes=[rones])
        eps = k.sbuf(name + "eps", [128, 1], F32, st); reps = k.res()
        k.op("pool", lambda e: e.memset(eps[:], 1e-6), writes=[reps])
        ng = k.sbuf(name + "ng", [128, 8], F32, st); nb = k.sbuf(name + "nb", [128, 8], F32, st); rpar = k.res()
        k.dma("sp", ng[:], ng_pc, writes=[rpar]); k.dma("sp", nb[:], nb_pc, writes=[rpar])
        x = k.sbuf(name + "x", [128, 8, TB], F32, st); rx = k.res()
        x2 = k.sbuf(name + "x2", [128, 8, TB], F32, st); rx2 = k.res()
        o = k.sbuf(name + "o", [128, 8, TB], F32, st); ro = k.res()
        sq = k.sbuf(name + "sq", [128, 8, TB], F32, st); rsq = k.res()
        mean = k.sbuf(name + "mean", [128, TB], F32, st); rmean = k.res()
        rstd = k.sbuf(name + "rstd", [128, TB], F32, st); rrstd = k.res()
        psm = k.psum(name + "psm", [128, TB], F32, st); rpsm = k.pres()
        pse = k.psum(name + "pse", [128, TB], F32, st); rpse = k.pres()
        for (t0, n) in _blocks(T):
            k.dma("sp", x[:, :, :n], HD[0, :, t0:t0 + n].rearrange("(c p) t -> p c t", p=128), writes=[rx])
            k.dma("act", x2[:, :, :n], HD[1, :, t0:t0 + n].rearrange("(c p) t -> p c t", p=128), writes=[rx2])
            k.dma("pool", o[:, :, :n], PT[3072:4096, t0:t0 + n].rearrange("(c p) t -> p c t", p=128), writes=[ro])
            k.op("dve", lambda e, n=n: e.tensor_tensor(out=x[:, :, :n], in0=x[:, :, :n], in1=x2[:, :, :n], op=ALU.add), reads=[rx2], writes=[rx])
            head_ln(k, x, rx, n, 8, onesb, rones, eps[:, 0:1], reps, psm, rpsm, pse, rpse, sq, rsq, mean, rmean, rstd, rrstd,
                    [[0, 1], [2, 3], [4, 5], [6, 7]])
            k.op("act", lambda e, n=n: e.activation(out=o[:, :, :n], in_=o[:, :, :n], func=AF.Sigmoid), reads=[ro], writes=[ro])
            for ch in range(8):
                k.op("dve", lambda e, ch=ch, n=n: e.tensor_scalar(out=x[:, ch, :n], in0=x[:, ch, :n], scalar1=ng[:, ch:ch + 1], scalar2=nb[:, ch:ch + 1],
                                                               op0=ALU.mult, op1=ALU.add), reads=[rx, rpar], writes=[rx])
            k.op("pool", lambda e, n=n: e.tensor_tensor(out=x[:, :, :n], in0=x[:, :, :n], in1=o[:, :, :n], op=ALU.mult), reads=[rx, ro], writes=[rx])
            k.dma("sp", YML[:, t0:t0 + n].rearrange("(c p) t -> p c t", p=128), x[:, :, :n], reads=[rx])
        k.barrier()

def phase_rw_prep(k, PT, outs, prm, T, name="rwp_"):
    with contextlib.ExitStack() as st:
        TB = 512
        def ld(nm, shape, src):
            t = k.sbuf(name + nm, shape, F32, st); r = k.res(); k.dma("sp", t[:], src, writes=[r]); return t, r
        mu, rmu = ld("mu", [128, 27], prm["mu_pc"]); w0, rw0 = ld("w0", [128, 2, 8], prm["w0_pc"]); a0, ra0 = ld("a0", [128, 2, 8], prm["a0_pc"])
        wup, rwup = ld("wup", [128, 1024], prm["w_up"]); aup, raup = ld("aup", [128, 1024], prm["a_up"]); gup, rgup = ld("gup", [128, 1024], prm["g_up"])
        kkp, rkkp = ld("kkp", [128, 8], prm["kk_pc"]); kap, rkap = ld("kap", [128, 8], prm["ka_pc"]); rkp, rrkp = ld("rkp", [128, 8], prm["rk_pc"])
        mu1 = k.sbuf(name + "mu1", [128, 27], F32, st); muh = k.sbuf(name + "muh", [128, 27], F32, st); rmu2 = k.res()
        ka1 = k.sbuf(name + "ka1", [128, 8], F32, st); rka1 = k.res()
        k.op("dve", lambda e: e.tensor_scalar(out=mu1[:], in0=mu[:], scalar1=-1.0, scalar2=1.0, op0=ALU.mult, op1=ALU.add), reads=[rmu], writes=[rmu2])
        k.op("dve", lambda e: e.tensor_scalar(out=muh[:], in0=mu[:], scalar1=0.5, scalar2=None, op0=ALU.mult), reads=[rmu], writes=[rmu2])
        k.op("dve", lambda e: e.tensor_scalar(out=ka1[:], in0=kap[:], scalar1=-1.0, scalar2=1.0, op0=ALU.mult, op1=ALU.add), reads=[rkap], writes=[rka1])
        bones = k.sbuf(name + "bones", [128, 128], F32, st); rbones = k.res()
        k.op("pool", lambda e: e.memset(bones[:], 0.0), writes=[rbones])
        k.op("pool", lambda e: e.memset(bones[0:64, 0:64], 1.0), writes=[rbones])
        k.op("pool", lambda e: e.memset(bones[64:128, 64:128], 1.0), writes=[rbones])
        tiny = k.sbuf(name + "tiny", [128, 1], F32, st); rtiny = k.res()
        k.op("pool", lambda e: e.memset(tiny[:], 0.0), writes=[rtiny])
        z = [k.sbuf(f"{name}z{i}", [128, TB + 2], F32, st) for i in range(3)]; rz = [k.res() for _ in range(3)]
        s_ = [k.sbuf(f"{name}s{i}", [128, TB], F32, st) for i in range(2)]; rs = [k.res() for _ in range(2)]
        ZS = k.sbuf(name + "ZS", [128, 27, TB], F32, st); rZS = [k.res() for _ in range(27)]
        Ab = k.sbuf(name + "Ab", [128, 2, 8, TB], F32, st); rAb = [[k.res() for _ in range(8)] for _ in range(2)]
        KDb = k.sbuf(name + "KDb", [128, 2, TB], F32, st); rKDb = k.res()
        t1 = [k.sbuf(f"{name}t1{i}", [128, TB], F32, st) for i in range(3)]; rt1 = [k.res() for _ in range(3)]
        t2 = [k.sbuf(f"{name}t2{i}", [128, TB], F32, st) for i in range(3)]; rt2 = [k.res() for _ in range(3)]
        ps = [k.psum(f"{name}ps{i}", [128, TB], F32, st) for i in range(4)]; rps = [k.pres() for _ in range(4)]
        qi = 0; pi = 0; ti = 0
        for (t0, n) in _blocks(T):
            seg_lo, seg_hi = (0, CTXL) if t0 < CTXL else (CTXL, T)
            for c in range(27):
                zz, rzz = z[c % 3], rz[c % 3]
                rows = PT[RW0 + c * 128:RW0 + (c + 1) * 128, :]
                lo = max(seg_lo, t0 - 1); hi = min(seg_hi, t0 + n + 1)
                if lo > t0 - 1:
                    k.op("pool", lambda e, zz=zz: e.memset(zz[:, 0:1], 0.0), writes=[rzz])
                if hi < t0 + n + 1:
                    k.op("pool", lambda e, zz=zz, n=n: e.memset(zz[:, n + 1:n + 2], 0.0), writes=[rzz])
                k.dma(("sp", "act", "pool")[c % 3], zz[:, lo - (t0 - 1):hi - (t0 - 1)], rows[:, lo:hi], writes=[rzz])
                ss, rss = s_[c % 2], rs[c % 2]
                k.op("pool", lambda e, zz=zz, ss=ss, n=n: e.tensor_tensor(out=ss[:, :n], in0=zz[:, 0:n], in1=zz[:, 2:n + 2], op=ALU.add), reads=[rzz], writes=[rss])
                k.op("dve", lambda e, ss=ss, c=c, n=n: e.tensor_scalar(out=ss[:, :n], in0=ss[:, :n], scalar1=muh[:, c:c + 1], scalar2=None, op0=ALU.mult), reads=[rss, rmu2], writes=[rss])
                k.op("dve", lambda e, zz=zz, ss=ss, c=c, n=n: e.scalar_tensor_tensor(out=ZS[:, c, :n], in0=zz[:, 1:n + 1], scalar=mu1[:, c:c + 1], in1=ss[:, :n], op0=ALU.mult, op1=ALU.add),
                     reads=[rzz, rss, rmu2], writes=[rZS[c]])
            k.dma("sp", outs["R"][:, t0:t0 + n].rearrange("(c p) t -> p c t", p=128), ZS[:, 0:8, :n], reads=rZS[0:8])
            k.dma("act", outs["V"][:, t0:t0 + n].rearrange("(c p) t -> p c t", p=128), ZS[:, 16:24, :n], reads=rZS[16:24])
            k.op("act", lambda e, n=n: e.activation(out=ZS[:, 24, :n], in_=ZS[:, 24, :n], func=AF.Tanh), reads=[rZS[24]], writes=[rZS[24]])
            k.op("act", lambda e, n=n: e.activation(out=ZS[:, 26, :n], in_=ZS[:, 26, :n], func=AF.Sigmoid), reads=[rZS[26]], writes=[rZS[26]])
            for d in range(2):
                for j in range(8):
                    p, rp = ps[pi % 4], rps[pi % 4]; pi += 1
                    tt, rtt = t1[ti % 3], rt1[ti % 3]; ti += 1
                    k.op("pe", lambda e, p=p, d=d, j=j, n=n: e.matmul(p[:, :n], lhsT=wup[d * 64:(d + 1) * 64, j * 128:(j + 1) * 128], rhs=ZS[d * 64:(d + 1) * 64, 24, :n], start=True, stop=True),
                         reads=[rwup, rZS[24]], writes=[rp])
                    k.op("act", lambda e, p=p, tt=tt, d=d, j=j, n=n: e.activation(out=tt[:, :n], in_=p[:, :n], func=AF.Sigmoid, bias=w0[:, d, j:j + 1]), reads=[rp, rw0], writes=[rtt])
                    k.op("pool", lambda e, tt=tt, n=n: e.tensor_scalar(out=tt[:, :n], in0=tt[:, :n], scalar1=-0.6065306597126334, scalar2=None, op0=ALU.mult), reads=[rtt], writes=[rtt])
                    k.dma("sp", outs["LW"][d, j * 128:(j + 1) * 128, t0:t0 + n], tt[:, :n], reads=[rtt])
                    p, rp = ps[pi % 4], rps[pi % 4]; pi += 1
                    k.op("pe", lambda e, p=p, d=d, j=j, n=n: e.matmul(p[:, :n], lhsT=aup[d * 64:(d + 1) * 64, j * 128:(j + 1) * 128], rhs=ZS[d * 64:(d + 1) * 64, 25, :n], start=True, stop=True),
                         reads=[raup, rZS[25]], writes=[rp])
                    k.op("act", lambda e, p=p, d=d, j=j, n=n: e.activation(out=Ab[:, d, j, :n], in_=p[:, :n], func=AF.Sigmoid, bias=a0[:, d, j:j + 1]), reads=[rp, ra0], writes=[rAb[d][j]])
                    k.dma("act", outs["A"][d, j * 128:(j + 1) * 128, t0:t0 + n], Ab[:, d, j, :n], reads=[rAb[d][j]])
            for j in range(8):
                p, rp = ps[pi % 4], rps[pi % 4]; pi += 1
                tt, rtt = t1[ti % 3], rt1[ti % 3]; ti += 1
                k.op("pe", lambda e, p=p, j=j, n=n: e.matmul(p[:, :n], lhsT=gup[:, j * 128:(j + 1) * 128], rhs=ZS[:, 26, :n], start=True, stop=True), reads=[rgup, rZS[26]], writes=[rp])
                k.op("act", lambda e, p=p, tt=tt, n=n: e.copy(out=tt[:, :n], in_=p[:, :n]), reads=[rp], writes=[rtt])
                k.dma("pool", outs["G"][j * 128:(j + 1) * 128, t0:t0 + n], tt[:, :n], reads=[rtt])
            for j in range(8):
                kc = 8 + j; rc = j; vc = 16 + j
                tt, rtt = t1[ti % 3], rt1[ti % 3]; tu, rtu = t2[ti % 3], rt2[ti % 3]; ti += 1
                p, rp = ps[pi % 4], rps[pi % 4]; pi += 1
                k.op("act", lambda e, tt=tt, kc=kc, j=j, n=n: e.activation(out=tt[:, :n], in_=ZS[:, kc, :n], func=AF.Square, scale=kkp[:, j:j + 1]), reads=[rZS[kc], rkkp], writes=[rtt])
                k.op("pe", lambda e, p=p, tt=tt, n=n: e.matmul(p[:, :n], lhsT=bones[:], rhs=tt[:, :n], start=True, stop=True), reads=[rtt, rbones], writes=[rp])
                k.op("act", lambda e, p=p, tt=tt, n=n: e.activation(out=tt[:, :n], in_=p[:, :n], func=AF.Sqrt, bias=tiny[:, 0:1]), reads=[rp, rtiny], writes=[rtt])
                k.op("dve", lambda e, tt=tt, n=n: e.tensor_scalar(out=tt[:, :n], in0=tt[:, :n], scalar1=1e-12, scalar2=None, op0=ALU.max), reads=[rtt], writes=[rtt])
                k.op("dve", lambda e, tt=tt, n=n: e.reciprocal(out=tt[:, :n], in_=tt[:, :n]), reads=[rtt], writes=[rtt])
                k.op("dve", lambda e, tt=tt, kc=kc, j=j, n=n: e.scalar_tensor_tensor(out=tt[:, :n], in0=ZS[:, kc, :n], scalar=kkp[:, j:j + 1], in1=tt[:, :n], op0=ALU.mult, op1=ALU.mult),
                     reads=[rZS[kc], rkkp, rtt], writes=[rtt])
                k.dma("sp", outs["KK"][j * 128:(j + 1) * 128, t0:t0 + n], tt[:, :n], reads=[rtt])
                for d in range(2):
                    k.op("dve", lambda e, d=d, j=j, n=n: e.tensor_scalar(out=KDb[:, d, :n], in0=Ab[:, d, j, :n], scalar1=kap[:, j:j + 1], scalar2=ka1[:, j:j + 1], op0=ALU.mult, op1=ALU.add),
                         reads=[rAb[d][j], rkap, rka1], writes=[rKDb])
                    k.op("pool", lambda e, d=d, kc=kc, n=n: e.tensor_tensor(out=KDb[:, d, :n], in0=KDb[:, d, :n], in1=ZS[:, kc, :n], op=ALU.mult), reads=[rKDb, rZS[kc]], writes=[rKDb])
                    k.dma(("act", "pool")[d], outs["KD"][d, j * 128:(j + 1) * 128, t0:t0 + n], KDb[:, d, :n], reads=[rKDb])
                p, rp = ps[pi % 4], rps[pi % 4]; pi += 1
                k.op("pool", lambda e, tu=tu, n=n: e.tensor_tensor(out=tu[:, :n], in0=KDb[:, 0, :n], in1=KDb[:, 1, :n], op=ALU.add), reads=[rKDb], writes=[rtu])
                k.op("dve", lambda e, tu=tu, rc=rc, j=j, n=n: e.scalar_tensor_tensor(out=tu[:, :n], in0=ZS[:, rc, :n], scalar=rkp[:, j:j + 1], in1=tu[:, :n], op0=ALU.mult, op1=ALU.mult),
                     reads=[rZS[rc], rrkp, rtu], writes=[rtu])
                k.op("pe", lambda e, p=p, tu=tu, n=n: e.matmul(p[:, :n], lhsT=bones[:], rhs=tu[:, :n], start=True, stop=True), reads=[rtu, rbones], writes=[rp])
                k.op("dve", lambda e, p=p, tu=tu, vc=vc, n=n: e.tensor_tensor(out=tu[:, :n], in0=p[:, :n], in1=ZS[:, vc, :n], op=ALU.mult), reads=[rp, rZS[vc]], writes=[rtu])
                k.dma("sp", outs["BV"][j * 128:(j + 1) * 128, t0:t0 + n], tu[:, :n], reads=[rtu])
        k.barrier()

def phase_rw_finish(k, YD, BV, G, YRW, ng_pc, nb_pc, T, name="rwf_"):
    with contextlib.ExitStack() as st:
        TB = 512
        onesb = k.sbuf(name + "ones", [128, 128], F32, st); rones = k.res()
        k.op("pool", lambda e: e.memset(onesb[:], 0.0), writes=[rones])
        k.op("pool", lambda e: e.memset(onesb[0:64, 0:64], 1.0 / 64.0), writes=[rones])
        k.op("pool", lambda e: e.memset(onesb[64:128, 64:128], 1.0 / 64.0), writes=[rones])
        eps = k.sbuf(name + "eps", [128, 1], F32, st); reps = k.res()
        k.op("pool", lambda e: e.memset(eps[:], 64e-5), writes=[reps])
        ng = k.sbuf(name + "ng", [128, 8], F32, st); nb = k.sbuf(name + "nb", [128, 8], F32, st); rpar = k.res()
        k.dma("sp", ng[:], ng_pc, writes=[rpar]); k.dma("sp", nb[:], nb_pc, writes=[rpar])
        x = k.sbuf(name + "x", [128, 8, TB], F32, st); rx = k.res()
        x2 = k.sbuf(name + "x2", [128, 8, TB], F32, st); rx2 = k.res()
        bv = k.sbuf(name + "bv", [128, 8, TB], F32, st); rbv = k.res()
        g = k.sbuf(name + "g", [128, 8, TB], F32, st); rg = k.res()
        sq = k.sbuf(name + "sq", [128, 8, TB], F32, st); rsq = k.res()
        mean = k.sbuf(name + "mean", [128, TB], F32, st); rmean = k.res()
        rstd = k.sbuf(name + "rstd", [128, TB], F32, st); rrstd = k.res()
        psm = k.psum(name + "psm", [128, TB], F32, st); rpsm = k.pres()
        pse = k.psum(name + "pse", [128, TB], F32, st); rpse = k.pres()
        for (t0, n) in _blocks(T):
            v3 = lambda ap: ap[:, t0:t0 + n].rearrange("(c p) t -> p c t", p=128)
            k.dma("sp", x[:, :, :n], v3(YD[0]), writes=[rx]); k.dma("act", x2[:, :, :n], v3(YD[1]), writes=[rx2])
            k.dma("pool", bv[:, :, :n], v3(BV), writes=[rbv]); k.dma("sp", g[:, :, :n], v3(G), writes=[rg])
            k.op("dve", lambda e, n=n: e.tensor_tensor(out=x[:, :, :n], in0=x[:, :, :n], in1=x2[:, :, :n], op=ALU.add), reads=[rx2], writes=[rx])
            head_ln(k, x, rx, n, 8, onesb, rones, eps[:, 0:1], reps, psm, rpsm, pse, rpse, sq, rsq, mean, rmean, rstd, rrstd, [[c] for c in range(8)])
            for ch in range(8):
                k.op("dve", lambda e, ch=ch, n=n: e.tensor_scalar(out=x[:, ch, :n], in0=x[:, ch, :n], scalar1=ng[:, ch:ch + 1], scalar2=nb[:, ch:ch + 1],
                                                               op0=ALU.mult, op1=ALU.add), reads=[rx, rpar], writes=[rx])
            k.op("pool", lambda e, n=n: e.tensor_tensor(out=x[:, :, :n], in0=x[:, :, :n], in1=bv[:, :, :n], op=ALU.add), reads=[rx, rbv], writes=[rx])
            k.op("dve", lambda e, n=n: e.tensor_tensor(out=x[:, :, :n], in0=x[:, :, :n], in1=g[:, :, :n], op=ALU.mult), reads=[rx, rg], writes=[rx])
            k.dma("sp", v3(YRW), x[:, :, :n], reads=[rx])
        k.barrier()


L = 64; S5G = 64; S5N = 64; S5C = 16; S5_ROW0 = 0
GB = 8

def s5_host_layout(lam_re, lam_im, log_dt, b_re, b_im, c_re, c_im):
    lamN = np.stack([np.transpose(lam_re, (2, 0, 1)), np.transpose(lam_im, (2, 0, 1)),
                     np.broadcast_to(log_dt[None], (64, 2, 64))], 1).astype(np.float32)
    lamC = np.stack([lam_re, lam_im, np.broadcast_to(log_dt[..., None], (2, 64, 64))], 0)
    lamC = np.broadcast_to(lamC[None], (16, 3, 2, 64, 64)).astype(np.float32)
    Bc = np.stack([np.transpose(b_re, (3, 0, 1, 2)), np.transpose(b_im, (3, 0, 1, 2))], 1).astype(np.float32)
    Cn = np.stack([np.transpose(c_re, (3, 0, 1, 2)), np.transpose(c_im, (3, 0, 1, 2))], 1).astype(np.float32)
    return {"lamN": np.ascontiguousarray(lamN), "lamC": np.ascontiguousarray(lamC), "Bc": np.ascontiguousarray(Bc), "Cn": np.ascontiguousarray(Cn)}

def make_s5_consts():
    c = np.zeros((64, 2, 512), np.float32)
    c[:, 0, :64] = np.arange(1, 65, dtype=np.float32)[None]
    rst = np.ones((GB, 64), np.float32); rst[:, 0] = 0.0
    c[:, 1, :] = rst.reshape(-1)[None]
    return c

def cmul(k, eng2, outr, outi, ar, ai, br, bi, tmp, rds, wr, rtmp):
    e1, e2 = eng2
    k.op(e1, lambda e: e.tensor_tensor(out=outr, in0=ar, in1=br, op=ALU.mult), reads=rds, writes=[wr])
    k.op(e2, lambda e: e.tensor_tensor(out=tmp, in0=ai, in1=bi, op=ALU.mult), reads=rds, writes=[rtmp])
    k.op(e1, lambda e: e.tensor_tensor(out=outr, in0=outr, in1=tmp, op=ALU.subtract), reads=[wr, rtmp], writes=[wr])
    k.op(e2, lambda e: e.tensor_tensor(out=outi, in0=ar, in1=bi, op=ALU.mult), reads=rds, writes=[wr])
    k.op(e1, lambda e: e.tensor_tensor(out=tmp, in0=ai, in1=br, op=ALU.mult), reads=rds + [wr], writes=[rtmp])
    k.op(e2, lambda e: e.tensor_tensor(out=outi, in0=outi, in1=tmp, op=ALU.add), reads=[wr, rtmp], writes=[wr])

def phase_s5(k, PT, YSD, lamN_d, lamC_d, Bc_d, Cn_d, s5c_d, T, name="s5_", dbg=None):
    NCH = T // L
    order = {0: list(range(NCH)), 1: [3, 2, 1, 0] + list(range(NCH - 1, 3, -1))}
    with contextlib.ExitStack() as st:
        def sb(nm, shape): return k.sbuf(name + nm, shape, F32, st)
        cst = sb("cst", [64, 2, 512]); rcst = k.res(); k.dma("sp", cst[:], s5c_d, writes=[rcst])
        lamN = sb("lamN", [64, 3, 2, 64]); rlamN = k.res(); k.dma("sp", lamN[:], lamN_d, writes=[rlamN])
        Cn = sb("Cn", [64, 2, 2, 64, 16]); rCn = k.res(); k.dma("act", Cn[:], Cn_d, writes=[rCn])
        halfpi = sb("hpi", [64, 1]); rhp = k.res(); k.op("pool", lambda e: e.memset(halfpi[:], math.pi / 2), writes=[rhp])
        k.op("pool", lambda e: e.tensor_scalar(out=Cn[:, 1], in0=Cn[:, 1], scalar1=-1.0, scalar2=None, op0=ALU.mult), reads=[rCn], writes=[rCn])
        dtN = sb("dtN", [64, 2, 64]); aN = sb("aN", [64, 2, 64]); thN = sb("thN", [64, 2, 64]); rN = k.res()
        c1 = sb("c1", [64, 2, 64]); s1 = sb("s1", [64, 2, 64]); tN = sb("tN", [64, 2, 64]); tN2 = sb("tN2", [64, 2, 64]); rcs = k.res(); rtN = k.res()
        k.op("act", lambda e: e.activation(out=dtN[:], in_=lamN[:, 2], func=AF.Exp), reads=[rlamN], writes=[rN])
        k.op("dve", lambda e: e.tensor_tensor(out=aN[:], in0=lamN[:, 0], in1=dtN[:], op=ALU.mult), reads=[rlamN, rN], writes=[rN])
        k.op("dve", lambda e: e.tensor_tensor(out=thN[:], in0=lamN[:, 1], in1=dtN[:], op=ALU.mult), reads=[rlamN, rN], writes=[rN])
        def unit_phasor(th, cc, ss, t1_, t2_, shape_p, rth, rout, rt, hp):
            k.op("act", lambda e: e.activation(out=ss, in_=th, func=AF.Sin, scale=1.0 / 16.0), reads=[rth], writes=[rout])
            k.op("act", lambda e: e.activation(out=cc, in_=th, func=AF.Sin, scale=1.0 / 16.0, bias=hp), reads=[rth, rhp], writes=[rout])
            for _ in range(4):
                k.op("dve", lambda e: e.tensor_tensor(out=t1_, in0=cc, in1=cc, op=ALU.mult), reads=[rout], writes=[rt])
                k.op("dve", lambda e: e.tensor_tensor(out=t2_, in0=ss, in1=ss, op=ALU.mult), reads=[rout], writes=[rt])
                k.op("dve", lambda e: e.scalar_tensor_tensor(out=ss, in0=ss, scalar=2.0, in1=cc, op0=ALU.mult, op1=ALU.mult), reads=[rout], writes=[rout])
                k.op("dve", lambda e: e.tensor_tensor(out=cc, in0=t1_, in1=t2_, op=ALU.subtract), reads=[rt], writes=[rout])
        unit_phasor(thN[:], c1[:], s1[:], tN[:], tN2[:], None, rN, rcs, rtN, halfpi[:, 0:1])
        Bbr = sb("Bbr", [16, 2, 64, 64]); Bbi = sb("Bbi", [16, 2, 64, 64]); rBb = k.res()
        with contextlib.ExitStack() as st2:
            def s2(nm): return k.sbuf(name + "b_" + nm, [16, 16, 64], F32, st2)
            lre, lim, ldt, br_, bi_ = s2("lre"), s2("lim"), s2("ldt"), s2("br"), s2("bi")
            ea, cc, ss, u1, u2, fr, fi = s2("ea"), s2("cc"), s2("ss"), s2("u1"), s2("u2"), s2("fr"), s2("fi")
            rl = k.res(); rw = k.res(); rt = k.res(); rf = k.res()
            def bbar_block(d, qq):
                qs = slice(qq * 16, (qq + 1) * 16)
                k.dma("sp", lre[:], lamC_d[:, 0, d, qs], writes=[rl]); k.dma("act", lim[:], lamC_d[:, 1, d, qs], writes=[rl]); k.dma("pool", ldt[:], lamC_d[:, 2, d, qs], writes=[rl])
                k.dma("sp", br_[:], Bc_d[:, 0, d, qs], writes=[rl]); k.dma("act", bi_[:], Bc_d[:, 1, d, qs], writes=[rl])
                k.op("act", lambda e: e.activation(out=ldt[:], in_=ldt[:], func=AF.Exp), reads=[rl], writes=[rl])
                k.op("dve", lambda e: e.tensor_tensor(out=u1[:], in0=lre[:], in1=ldt[:], op=ALU.mult), reads=[rl], writes=[rw])
                k.op("dve", lambda e: e.tensor_tensor(out=u2[:], in0=lim[:], in1=ldt[:], op=ALU.mult), reads=[rl], writes=[rw])
                k.op("act", lambda e: e.activation(out=ea[:], in_=u1[:], func=AF.Exp), reads=[rw], writes=[rw])
                unit_phasor(u2[:], cc[:], ss[:], fr[:], fi[:], None, rw, rw, rf, halfpi[0:16, 0:1])
                k.op("dve", lambda e: e.tensor_tensor(out=cc[:], in0=cc[:], in1=ea[:], op=ALU.mult), reads=[rw], writes=[rw])
                k.op("dve", lambda e: e.tensor_scalar(out=cc[:], in0=cc[:], scalar1=-1.0, scalar2=None, op0=ALU.add), reads=[rw], writes=[rw])
                k.op("dve", lambda e: e.tensor_tensor(out=ss[:], in0=ss[:], in1=ea[:], op=ALU.mult), reads=[rw], writes=[rw])
                k.op("dve", lambda e: e.tensor_tensor(out=u1[:], in0=lre[:], in1=lre[:], op=ALU.mult), reads=[rl, rw], writes=[rw])
                k.op("dve", lambda e: e.tensor_tensor(out=u2[:], in0=lim[:], in1=lim[:], op=ALU.mult), reads=[rl, rw], writes=[rw])
                k.op("dve", lambda e: e.tensor_tensor(out=ea[:], in0=u1[:], in1=u2[:], op=ALU.add), reads=[rw], writes=[rw])
                k.op("dve", lambda e: e.reciprocal(out=ea[:], in_=ea[:]), reads=[rw], writes=[rw])
                k.op("dve", lambda e: e.tensor_tensor(out=fr[:], in0=cc[:], in1=lre[:], op=ALU.mult), reads=[rw, rl], writes=[rf])
                k.op("dve", lambda e: e.tensor_tensor(out=u1[:], in0=ss[:], in1=lim[:], op=ALU.mult), reads=[rw, rl], writes=[rw])
                k.op("dve", lambda e: e.tensor_tensor(out=fr[:], in0=fr[:], in1=u1[:], op=ALU.add), reads=[rw, rf], writes=[rf])
                k.op("dve", lambda e: e.tensor_tensor(out=fr[:], in0=fr[:], in1=ea[:], op=ALU.mult), reads=[rw, rf], writes=[rf])
                k.op("dve", lambda e: e.tensor_tensor(out=fi[:], in0=ss[:], in1=lre[:], op=ALU.mult), reads=[rw, rl], writes=[rf])
                k.op("dve", lambda e: e.tensor_tensor(out=u1[:], in0=cc[:], in1=lim[:], op=ALU.mult), reads=[rw, rl], writes=[rw])
                k.op("dve", lambda e: e.tensor_tensor(out=fi[:], in0=fi[:], in1=u1[:], op=ALU.subtract), reads=[rw, rf], writes=[rf])
                k.op("dve", lambda e: e.tensor_tensor(out=fi[:], in0=fi[:], in1=ea[:], op=ALU.mult), reads=[rw, rf], writes=[rf])
                cmul(k, ("dve", "pool"), Bbr[:, d, qs], Bbi[:, d, qs], fr[:], fi[:], br_[:], bi_[:], u1[:], [rf, rl], rBb, rw)
            for d in range(2):
                for qq in range(4):
                    bbar_block(d, qq)
            k.barrier()
        if dbg is not None:
            k.dma("sp", dbg["c1"], c1[:], reads=[rcs]); k.dma("sp", dbg["s1"], s1[:], reads=[rcs]); k.dma("sp", dbg["aN"], aN[:], reads=[rN])
            k.dma("sp", dbg["Bbr"], Bbr[:], reads=[rBb]); k.dma("sp", dbg["Bbi"], Bbi[:], reads=[rBb])
        u_ = [sb(f"u{i}", [16, GB, L]) for i in range(2)]; ru = [k.res() for _ in range(2)]
        yb = [sb(f"yb{i}", [16, GB, L]) for i in range(2)]; ryb = [k.res() for _ in range(2)]
        shp = [64, GB, L]
        EMp, EMm, CM, SM = sb("EMp", shp), sb("EMm", shp), sb("CM", shp), sb("SM", shp); rtab0 = k.res()
        Pr, Pi, Qr, Qi = sb("Pr", shp), sb("Pi", shp), sb("Qr", shp), sb("Qi", shp); rtab = k.res()
        Zr, Zi, Sr, Si, Xr, Xi, tm = sb("Zr", shp), sb("Zi", shp), sb("Sr", shp), sb("Si", shp), sb("Xr", shp), sb("Xi", shp), sb("tm", shp)
        rZ, rS, rX, rtm = k.res(), k.res(), k.res(), k.res()
        xsr, xsi, l65r, l65i, c0r, c0i, ts = sb("xsr", [64, GB, 1]), sb("xsi", [64, GB, 1]), sb("l65r", [64, GB, 1]), sb("l65i", [64, GB, 1]), sb("c0r", [64, GB, 1]), sb("c0i", [64, GB, 1]), sb("ts", [64, GB, 1])
        rxs, rl65, rc0, rts = k.res(), k.res(), k.res(), k.res()
        pBr = k.psum(name + "pBr", [64, GB, L], F32, st); rpBr = k.pres()
        pBi = k.psum(name + "pBi", [64, GB, L], F32, st); rpBi = k.pres()
        pY = [k.psum(f"{name}pY{i}", [16, GB, L], F32, st) for i in range(2)]; rpY = [k.pres() for _ in range(2)]
        mr = cst[:, 0, 0:64]; rst = cst[:, 1, :].rearrange("p (g t) -> p g t", t=L)
        itc = [0]
        def do_block(d, gb):
                gs = slice(gb * GB, (gb + 1) * GB)
                a_bc = aN[:, d, gs].unsqueeze(2).broadcast_to(shp); mr_bc = mr.unsqueeze(1).broadcast_to(shp)
                k.op("dve", lambda e: e.tensor_tensor(out=EMp[:], in0=a_bc, in1=mr_bc, op=ALU.mult), reads=[rN, rcst, rtab], writes=[rtab0])
                k.op("act", lambda e: e.activation(out=EMm[:], in_=EMp[:], func=AF.Exp, scale=-1.0), reads=[rtab0], writes=[rtab0])
                k.op("act", lambda e: e.activation(out=EMp[:], in_=EMp[:], func=AF.Exp), reads=[rtab0], writes=[rtab0])
                k.op("dve", lambda e: e.tensor_copy(out=CM[:, :, 0:1], in_=c1[:, d, gs].unsqueeze(2)), reads=[rcs], writes=[rtab0])
                k.op("dve", lambda e: e.tensor_copy(out=SM[:, :, 0:1], in_=s1[:, d, gs].unsqueeze(2)), reads=[rcs], writes=[rtab0])
                kk_ = 1
                while kk_ < L:
                    ck = CM[:, :, kk_ - 1:kk_].broadcast_to([64, GB, kk_]); sk = SM[:, :, kk_ - 1:kk_].broadcast_to([64, GB, kk_])
                    cmul(k, ("dve", "pool"), CM[:, :, kk_:2 * kk_], SM[:, :, kk_:2 * kk_], CM[:, :, 0:kk_], SM[:, :, 0:kk_], ck, sk, tm[:, :, 0:kk_], [rtab0], rtab0, rtm)
                    kk_ *= 2
                k.op("dve", lambda e: e.tensor_tensor(out=Pr[:], in0=EMm[:], in1=CM[:], op=ALU.mult), reads=[rtab0], writes=[rtab])
                k.op("dve", lambda e: e.scalar_tensor_tensor(out=Pi[:], in0=EMm[:], scalar=-1.0, in1=SM[:], op0=ALU.mult, op1=ALU.mult), reads=[rtab0], writes=[rtab])
                k.op("pool", lambda e: e.tensor_tensor(out=Qr[:], in0=EMp[:], in1=CM[:], op=ALU.mult), reads=[rtab0], writes=[rtab])
                k.op("pool", lambda e: e.tensor_tensor(out=Qi[:], in0=EMp[:], in1=SM[:], op=ALU.mult), reads=[rtab0], writes=[rtab])
                if d == 1:
                    cmul(k, ("dve", "pool"), l65r[:], l65i[:], Qr[:, :, 63:64], Qi[:, :, 63:64], Qr[:, :, 0:1], Qi[:, :, 0:1], ts[:], [rtab], rl65, rts)
                k.op("pool", lambda e: e.memset(xsr[:], 0.0), writes=[rxs]); k.op("pool", lambda e: e.memset(xsi[:], 0.0), writes=[rxs])
                Tin = (Pr, Pi) if d == 0 else (Qr, Qi); Tout = (Qr, Qi) if d == 0 else (Pr, Pi)
                last = L - 1 if d == 0 else 0
                for c in order[d]:
                    do_chunk(d, gb, c, Tin, Tout, last)
        def do_chunk(d, gb, c, Tin, Tout, last):
                    b = itc[0] % 2; itc[0] += 1
                    t0 = c * L
                    k.dma("sp", u_[b][:], PT[S5_ROW0 + gb * GB * 16:S5_ROW0 + (gb + 1) * GB * 16, t0:t0 + L].rearrange("(g c) t -> c g t", c=16), writes=[ru[b]])
                    for g in range(GB):
                        k.op("pe", lambda e, g=g, b=b: e.matmul(pBr[:, g, :], lhsT=Bbr[:, d, gb * GB + g, :], rhs=u_[b][:, g, :], start=True, stop=True), reads=[rBb, ru[b]], writes=[rpBr])
                        k.op("pe", lambda e, g=g, b=b: e.matmul(pBi[:, g, :], lhsT=Bbi[:, d, gb * GB + g, :], rhs=u_[b][:, g, :], start=True, stop=True), reads=[rBb, ru[b]], writes=[rpBi])
                    k.op("dve", lambda e: e.tensor_tensor(out=Zr[:], in0=pBr[:], in1=Tin[0][:], op=ALU.mult), reads=[rpBr, rtab], writes=[rZ])
                    k.op("dve", lambda e: e.tensor_tensor(out=tm[:], in0=pBi[:], in1=Tin[1][:], op=ALU.mult), reads=[rpBi, rtab], writes=[rtm])
                    k.op("pool", lambda e: e.tensor_tensor(out=Zr[:], in0=Zr[:], in1=tm[:], op=ALU.subtract), reads=[rtm, rZ], writes=[rZ])
                    k.op("dve", lambda e: e.tensor_tensor(out=Zi[:], in0=pBr[:], in1=Tin[1][:], op=ALU.mult), reads=[rpBr, rtab], writes=[rZ])
                    k.op("dve", lambda e: e.tensor_tensor(out=tm[:], in0=pBi[:], in1=Tin[0][:], op=ALU.mult), reads=[rpBi, rtab, rZ], writes=[rtm])
                    k.op("pool", lambda e: e.tensor_tensor(out=Zi[:], in0=Zi[:], in1=tm[:], op=ALU.add), reads=[rtm, rZ], writes=[rZ])
                    fl = lambda ap: ap.rearrange("p g t -> p (g t)")
                    k.op("dve", lambda e: e.tensor_tensor_scan(out=fl(Sr[:]), data0=fl(rst), data1=fl(Zr[:]), initial=0.0, op0=ALU.mult, op1=ALU.add), reads=[rZ, rcst], writes=[rS])
                    k.op("dve", lambda e: e.tensor_tensor_scan(out=fl(Si[:]), data0=fl(rst), data1=fl(Zi[:]), initial=0.0, op0=ALU.mult, op1=ALU.add), reads=[rZ, rcst], writes=[rS])
                    if d == 0:
                        k.op("pool", lambda e: e.tensor_tensor(out=Sr[:], in0=Sr[:], in1=xsr[:].broadcast_to(shp), op=ALU.add), reads=[rS, rxs], writes=[rS])
                        k.op("pool", lambda e: e.tensor_tensor(out=Si[:], in0=Si[:], in1=xsi[:].broadcast_to(shp), op=ALU.add), reads=[rS, rxs], writes=[rS])
                    else:
                        cmul(k, ("dve", "pool"), c0r[:], c0i[:], l65r[:], l65i[:], xsr[:], xsi[:], ts[:], [rl65, rxs], rc0, rts)
                        k.op("dve", lambda e: e.tensor_tensor(out=c0r[:], in0=c0r[:], in1=Sr[:, :, 63:64], op=ALU.add), reads=[rS, rc0], writes=[rc0])
                        k.op("dve", lambda e: e.tensor_tensor(out=c0i[:], in0=c0i[:], in1=Si[:, :, 63:64], op=ALU.add), reads=[rS, rc0], writes=[rc0])
                        k.op("pool", lambda e: e.tensor_tensor(out=Sr[:], in0=Zr[:], in1=Sr[:], op=ALU.subtract), reads=[rS, rZ, rc0], writes=[rS])
                        k.op("pool", lambda e: e.tensor_tensor(out=Si[:], in0=Zi[:], in1=Si[:], op=ALU.subtract), reads=[rS, rZ, rc0], writes=[rS])
                        k.op("pool", lambda e: e.tensor_tensor(out=Sr[:], in0=Sr[:], in1=c0r[:].broadcast_to(shp), op=ALU.add), reads=[rS, rc0], writes=[rS])
                        k.op("pool", lambda e: e.tensor_tensor(out=Si[:], in0=Si[:], in1=c0i[:].broadcast_to(shp), op=ALU.add), reads=[rS, rc0], writes=[rS])
                    cmul(k, ("dve", "pool"), Xr[:], Xi[:], Sr[:], Si[:], Tout[0][:], Tout[1][:], tm[:], [rS, rtab], rX, rtm)
                    if dbg is not None and d == 0 and gb == 0 and c == 0:
                        for nm, tl_, rr_ in (("Zi", CM, rtab0), ("Si", EMp, rtab0), ("Pr", Pr, rtab), ("Pi", Pi, rtab), ("Qr", Qr, rtab), ("Qi", Qi, rtab), ("Zr", Zr, rZ), ("Sr", Sr, rS), ("Xr", Xr, rX), ("Xi", Xi, rX)):
                            k.dma("sp", dbg[nm], tl_[:], reads=[rr_])
                    py, rpy = pY[b], rpY[b]
                    for g in range(GB):
                        k.op("pe", lambda e, g=g, py=py: e.matmul(py[:, g, :], lhsT=Cn[:, 0, d, gb * GB + g, :], rhs=Xr[:, g, :], start=True, stop=False), reads=[rCn, rX], writes=[rpy])
                        k.op("pe", lambda e, g=g, py=py: e.matmul(py[:, g, :], lhsT=Cn[:, 1, d, gb * GB + g, :], rhs=Xi[:, g, :], start=False, stop=True), reads=[rCn, rX], writes=[rpy])
                    k.op("act", lambda e, py=py, b=b: e.copy(out=yb[b][:], in_=py[:]), reads=[rpy], writes=[ryb[b]])
                    k.dma("act", YSD[d, gb * GB * 16:(gb + 1) * GB * 16, t0:t0 + L].rearrange("(g c) t -> c g t", c=16), yb[b][:], reads=[ryb[b]])
                    k.op("dve", lambda e: e.tensor_copy(out=xsr[:], in_=Xr[:, :, last:last + 1]), reads=[rX], writes=[rxs])
                    k.op("dve", lambda e: e.tensor_copy(out=xsi[:], in_=Xi[:, :, last:last + 1]), reads=[rX], writes=[rxs])
        for d in range(2):
            for gb in range(S5G // GB):
                do_block(d, gb)
        k.barrier()

def phase_s5_finish(k, PT, YSD, YS5, d_pc, T, name="s5f_"):
    with contextlib.ExitStack() as st:
        TB = 512
        dsk = k.sbuf(name + "d", [128, 8], F32, st); rd = k.res(); k.dma("sp", dsk[:], d_pc, writes=[rd])
        x = k.sbuf(name + "x", [128, 8, TB], F32, st); rx = k.res()
        y0 = k.sbuf(name + "y0", [128, 8, TB], F32, st); ry0 = k.res()
        y1 = k.sbuf(name + "y1", [128, 8, TB], F32, st); ry1 = k.res()
        t = k.sbuf(name + "t", [128, 8, TB], F32, st); rt = k.res()
        blocks = [(t0, min(TB, T - t0)) for t0 in range(0, T, TB)]
        for (t0, n) in blocks:
            v3 = lambda ap: ap[:, t0:t0 + n].rearrange("(c p) t -> p c t", p=128)
            k.dma("sp", x[:, :, :n], v3(PT[S5_ROW0:S5_ROW0 + 1024]), writes=[rx]); k.dma("act", y0[:, :, :n], v3(YSD[0]), writes=[ry0]); k.dma("pool", y1[:, :, :n], v3(YSD[1]), writes=[ry1])
            k.op("pool", lambda e, n=n: e.tensor_tensor(out=y0[:, :, :n], in0=y0[:, :, :n], in1=y1[:, :, :n], op=ALU.add), reads=[ry1], writes=[ry0])
            for ch in range(8):
                k.op("dve", lambda e, ch=ch, n=n: e.scalar_tensor_tensor(out=x[:, ch, :n], in0=x[:, ch, :n], scalar=dsk[:, ch:ch + 1], in1=y0[:, ch, :n], op0=ALU.mult, op1=ALU.add),
                     reads=[rx, ry0, rd], writes=[rx])
            k.op("pool", lambda e, n=n: e.tensor_tensor(out=t[:, :, :n], in0=x[:, :, :n], in1=x[:, :, :n], op=ALU.mult), reads=[rx], writes=[rt])
            k.op("dve", lambda e, n=n: e.tensor_scalar(out=t[:, :, :n], in0=t[:, :, :n], scalar1=0.044715, scalar2=1.0, op0=ALU.mult, op1=ALU.add), reads=[rt], writes=[rt])
            k.op("pool", lambda e, n=n: e.tensor_tensor(out=t[:, :, :n], in0=t[:, :, :n], in1=x[:, :, :n], op=ALU.mult), reads=[rt, rx], writes=[rt])
            k.op("act", lambda e, n=n: e.activation(out=t[:, :, :n], in_=t[:, :, :n], func=AF.Tanh, scale=0.7978845608028654), reads=[rt], writes=[rt])
            k.op("dve", lambda e, n=n: e.tensor_scalar(out=t[:, :, :n], in0=t[:, :, :n], scalar1=0.5, scalar2=0.5, op0=ALU.mult, op1=ALU.add), reads=[rt], writes=[rt])
            k.op("pool", lambda e, n=n: e.tensor_tensor(out=t[:, :, :n], in0=t[:, :, :n], in1=x[:, :, :n], op=ALU.mult), reads=[rt, rx], writes=[rt])
            k.dma("sp", v3(YS5), t[:, :, :n], reads=[rt])
        k.barrier()


KC = 16; D = 2048; GATE_ROW0 = 1024
ALPHA = (2.0 * 2) ** 0.25
LN_EPS = 1e-5

def ln_block(k, x, rx, n, ones, rones, epst, reps, psm, rpsm, pse, rpse, sq, rsq, mean, rmean, rstd, rrstd):
    k.op("act", lambda e: e.activation(out=sq[:, :, :n], in_=x[:, :, :n], func=AF.Square), reads=[rx], writes=[rsq])
    for kc in range(KC):
        k.op("pe", lambda e, kc=kc: e.matmul(psm[:, :n], lhsT=ones[:], rhs=x[:, kc, :n], start=(kc == 0), stop=(kc == KC - 1)), reads=[rx, rones], writes=[rpsm])
    for kc in range(KC):
        k.op("pe", lambda e, kc=kc: e.matmul(pse[:, :n], lhsT=ones[:], rhs=sq[:, kc, :n], start=(kc == 0), stop=(kc == KC - 1)), reads=[rsq, rones], writes=[rpse])
    k.op("dve", lambda e: e.tensor_copy(out=mean[:, :n], in_=psm[:, :n]), reads=[rpsm], writes=[rmean])
    k.op("dve", lambda e: e.tensor_tensor(out=rstd[:, :n], in0=mean[:, :n], in1=mean[:, :n], op=ALU.mult), reads=[rmean], writes=[rrstd])
    k.op("dve", lambda e: e.tensor_tensor(out=rstd[:, :n], in0=pse[:, :n], in1=rstd[:, :n], op=ALU.subtract), reads=[rpse, rrstd], writes=[rrstd])
    k.op("act", lambda e: e.activation(out=rstd[:, :n], in_=rstd[:, :n], func=AF.Sqrt, bias=epst[:, 0:1]), reads=[rrstd, reps], writes=[rrstd])
    k.op("dve", lambda e: e.reciprocal(out=rstd[:, :n], in_=rstd[:, :n]), reads=[rrstd], writes=[rrstd])
    k.op("dve", lambda e: e.tensor_tensor(out=x[:, :, :n], in0=x[:, :, :n], in1=mean[:, :n].unsqueeze(1).broadcast_to([128, KC, n]), op=ALU.subtract), reads=[rx, rmean], writes=[rx])
    k.op("pool", lambda e: e.tensor_tensor(out=x[:, :, :n], in0=x[:, :, :n], in1=rstd[:, :n].unsqueeze(1).broadcast_to([128, KC, n]), op=ALU.mult), reads=[rx, rrstd], writes=[rx])

def affine_block(k, dst, rdst, src, rsrc, n, sc, sh, rpar, sc_idx=None):
    for kc in range(KC):
        k.op("dve", lambda e, kc=kc: e.tensor_scalar(out=dst[:, kc, :n], in0=src[:, kc, :n], scalar1=sc(kc), scalar2=sh(kc), op0=ALU.mult, op1=ALU.add),
             reads=[rsrc] + rpar, writes=[rdst])

class WStream:
    def __init__(self, k, st, name, kcmax, nbuf=3):
        self.k = k; self.n = nbuf; self.i = 0
        self.wf = [k.sbuf(f"{name}wf{i}", [128, kcmax, 128], F32, st) for i in range(nbuf)]; self.rwf = [k.res() for _ in range(nbuf)]
        self.wb = [k.sbuf(f"{name}wb{i}", [128, kcmax, 128], BF16, st) for i in range(nbuf)]; self.rwb = [k.res() for _ in range(nbuf)]
    def get(self, w2d, col0, kcn, ncol=128):
        k = self.k; b = self.i % self.n; it = self.i; self.i += 1
        wf, wb, rwf, rwb = self.wf[b], self.wb[b], self.rwf[b], self.rwb[b]
        src = w2d[col0 // 128]
        k.dma(("sp", "act", "pool")[it % 3], wf[:, :kcn, :ncol], src, writes=[rwf])
        if it % 2 == 0:
            k.op("act", lambda e: e.copy(out=wb[:, :kcn, :ncol], in_=wf[:, :kcn, :ncol]), reads=[rwf], writes=[rwb])
        else:
            k.op("pool", lambda e: e.tensor_copy(out=wb[:, :kcn, :ncol], in_=wf[:, :kcn, :ncol]), reads=[rwf], writes=[rwb])
        return wb, rwb

def phase_merge(k, xin, X1, PT, YML, YRW, YS5, W, mod, rmod, ln_g_pc, ln_b_pc, blocks, name="mg_"):
    with contextlib.ExitStack() as st:
        TB = 512
        def sb(nm, shape, dt=F32): return k.sbuf(name + nm, shape, dt, st)
        ones = sb("ones", [128, 128]); rones = k.res(); k.op("pool", lambda e: e.memset(ones[:], 1.0 / D), writes=[rones])
        epst = sb("eps", [128, 1]); reps = k.res(); k.op("pool", lambda e: e.memset(epst[:], LN_EPS), writes=[reps])
        lg = sb("lg", [128, KC]); lb = sb("lb", [128, KC]); rl = k.res()
        k.dma("sp", lg[:], ln_g_pc, writes=[rl]); k.dma("sp", lb[:], ln_b_pc, writes=[rl])
        yf = sb("yf", [128, 8, TB]); ryf = k.res()
        ybf = [sb(f"yb{i}", [128, 8, TB], BF16) for i in range(3)]; rybf = [k.res() for _ in range(3)]
        z = sb("z", [128, KC, TB], BF16); rz = k.res()
        xb = sb("x", [128, KC, TB]); rxb = k.res()
        pre = sb("pre", [128, KC, TB]); rpre = k.res()
        sq, rsq = xb, rxb
        gt = sb("gt", [128, 3, TB]); rgt = k.res()
        sgg = sb("sgg", [128, TB]); rsgg = k.res()
        t1 = sb("t1", [128, TB]); rt1 = k.res(); t2 = sb("t2", [128, TB]); rt2 = k.res(); t3 = sb("t3", [128, TB]); rt3 = k.res()
        mean = sb("mean", [128, TB]); rmean = k.res(); rstd = sb("rstd", [128, TB]); rrstd = k.res()
        ws = WStream(k, st, name, KC, nbuf=3)
        pp = [k.psum(f"{name}p{i}", [128, TB], F32, st) for i in range(4)]; rpp = [k.pres() for _ in range(4)]
        psm = k.psum(name + "psm", [128, TB], F32, st); rpsm = k.pres()
        pse = k.psum(name + "pse", [128, TB], F32, st); rpse = k.pres()
        po = [k.psum(f"{name}po{i}", [128, TB], F32, st) for i in range(2)]; rpo = [k.pres() for _ in range(2)]
        def do_block(t0, n, which):
            v8 = lambda ap: ap[:, t0:t0 + n].rearrange("(c p) t -> p c t", p=128)
            for i, src in enumerate((YML, YRW, YS5)):
                k.dma(("sp", "act", "pool")[i], yf[:, :, :n], v8(src), writes=[ryf])
                k.op(("dve", "act", "pool")[i], (lambda e, i=i: e.tensor_copy(out=ybf[i][:, :, :n], in_=yf[:, :, :n])) if i != 1 else
                     (lambda e, i=i: e.copy(out=ybf[i][:, :, :n], in_=yf[:, :, :n])), reads=[ryf], writes=[rybf[i]])
            k.dma("sp", xb[:, :, :n], xin[:, t0:t0 + n].rearrange("(c p) t -> p c t", p=128), writes=[rxb])
            k.op("act", lambda e: e.mul(out=xb[:, :, :n], in_=xb[:, :, :n], mul=ALPHA), reads=[rxb], writes=[rxb])
            def zchunk(j):
                for wi, (wn, src_i) in enumerate((("ml_proj", 0), ("rw_proj", 1), ("s5_w_val", 2), ("s5_w_gate", 2))):
                    wb, rwb = ws.get(W[wn], j * 128, 8)
                    for kc in range(8):
                        k.op("pe", lambda e, kc=kc, wb=wb, wi=wi, src_i=src_i: e.matmul(pp[wi][:, :n], lhsT=wb[:, kc, :], rhs=ybf[src_i][:, kc, :n], start=(kc == 0), stop=(kc == 7)),
                             reads=[rwb, rybf[src_i]], writes=[rpp[wi]])
                k.dma("sp", gt[:, :, :n], PT[GATE_ROW0:GATE_ROW0 + 3 * D, t0:t0 + n].rearrange("(b c p) t -> p b c t", b=3, p=128)[:, :, j, :], writes=[rgt])
                k.op("act", lambda e: e.activation(out=gt[:, :, :n], in_=gt[:, :, :n], func=AF.Sigmoid), reads=[rgt], writes=[rgt])
                k.op("act", lambda e: e.activation(out=sgg[:, :n], in_=pp[3][:, :n], func=AF.Sigmoid), reads=[rpp[3]], writes=[rsgg])
                k.op("dve", lambda e: e.tensor_tensor(out=t1[:, :n], in0=pp[2][:, :n], in1=sgg[:, :n], op=ALU.mult), reads=[rpp[2], rsgg], writes=[rt1])
                k.op("pool", lambda e: e.tensor_tensor(out=t1[:, :n], in0=t1[:, :n], in1=gt[:, 2, :n], op=ALU.mult), reads=[rt1, rgt], writes=[rt1])
                k.op("dve", lambda e: e.tensor_tensor(out=t2[:, :n], in0=pp[0][:, :n], in1=gt[:, 0, :n], op=ALU.mult), reads=[rpp[0], rgt], writes=[rt2])
                k.op("dve", lambda e: e.tensor_tensor(out=t3[:, :n], in0=pp[1][:, :n], in1=gt[:, 1, :n], op=ALU.mult), reads=[rpp[1], rgt], writes=[rt3])
                k.op("pool", lambda e: e.tensor_tensor(out=t2[:, :n], in0=t2[:, :n], in1=t3[:, :n], op=ALU.add), reads=[rt2, rt3], writes=[rt2])
                k.op("pool", lambda e: e.tensor_tensor(out=z[:, j, :n], in0=t2[:, :n], in1=t1[:, :n], op=ALU.add), reads=[rt2, rt1], writes=[rz])
            for j in range(KC):
                zchunk(j)
            def ochunk(j):
                wb, rwb = ws.get(W["w_out"], j * 128, KC)
                p, rp = po[j % 2], rpo[j % 2]
                for kc in range(KC):
                    k.op("pe", lambda e, kc=kc: e.matmul(p[:, :n], lhsT=wb[:, kc, :], rhs=z[:, kc, :n], start=(kc == 0), stop=(kc == KC - 1)), reads=[rwb, rz], writes=[rp])
                k.op("dve", lambda e: e.scalar_tensor_tensor(out=pre[:, j, :n], in0=p[:, :n], scalar=mod[:, 2 * KC + j, which:which + 1], in1=xb[:, j, :n], op0=ALU.mult, op1=ALU.add),
                     reads=[rp, rmod, rxb], writes=[rpre])
            for j in range(KC):
                ochunk(j)
            ln_block(k, pre, rpre, n, ones, rones, epst, reps, psm, rpsm, pse, rpse, sq, rsq, mean, rmean, rstd, rrstd)
            affine_block(k, pre, rpre, pre, rpre, n, lambda kc: lg[:, kc:kc + 1], lambda kc: lb[:, kc:kc + 1], [rl])
            k.dma("sp", X1[:, t0:t0 + n].rearrange("(c p) t -> p c t", p=128), pre[:, :, :n], reads=[rpre])
        for (t0, n, which) in blocks:
            do_block(t0, n, which)
        k.barrier()

def make_sel_const():
    s = np.zeros((16, 16, 128), np.float32)
    for e in range(16):
        s[e, e, :] = 1.0
    return s

def phase_moe(k, X1, out_fn, Wg, Wu, Wd, rw_lay, rb_bc, sel_d, ident_d, mod, rmod, ln_g_pc, ln_b_pc, blocks, name="moe_"):
    with contextlib.ExitStack() as st:
        TB = 512; NE = 16
        def sb(nm, shape, dt=F32): return k.sbuf(name + nm, shape, dt, st)
        ones = sb("ones", [128, 128]); rones = k.res(); k.op("pool", lambda e: e.memset(ones[:], 1.0 / D), writes=[rones])
        epst = sb("eps", [128, 1]); reps = k.res(); k.op("pool", lambda e: e.memset(epst[:], LN_EPS), writes=[reps])
        lg = sb("lg", [128, KC]); lb = sb("lb", [128, KC]); rl = k.res()
        k.dma("sp", lg[:], ln_g_pc, writes=[rl]); k.dma("sp", lb[:], ln_b_pc, writes=[rl])
        rwt = sb("rwt", [128, KC, NE]); rbt = sb("rbt", [128, NE]); selt = sb("sel", [16, NE, 128]); idt = sb("idt", [128, 128]); rcn = k.res()
        k.dma("sp", rwt[:], rw_lay, writes=[rcn]); k.dma("sp", rbt[:], rb_bc, writes=[rcn]); k.dma("sp", selt[:], sel_d, writes=[rcn]); k.dma("sp", idt[:], ident_d, writes=[rcn])
        sc1 = sb("sc1", [128, KC, 2]); rsc1 = k.res()
        k.op("dve", lambda e: e.tensor_scalar(out=sc1[:], in0=mod[:, 4 * KC:5 * KC, :], scalar1=1.0, scalar2=None, op0=ALU.add), reads=[rmod], writes=[rsc1])
        x1 = sb("x1", [128, KC, TB]); rx1 = k.res()
        uf = sb("uf", [128, KC, TB]); ruf = k.res()
        ub = sb("ub", [128, KC, TB], BF16); rub = k.res()
        acc = sb("acc", [128, KC, TB]); racc = k.res()
        hb = sb("h", [128, 8, TB], BF16); rhb = k.res()
        gbt = sb("gb", [128, TB]); rgb = k.res()
        s1 = sb("s1", [128, TB]); rs1 = k.res(); s2 = sb("s2", [128, TB]); rs2 = k.res()
        mean = sb("mean", [128, TB]); rmean = k.res(); rstd = sb("rstd", [128, TB]); rrstd = k.res()
        affT = sb("affT", [16, TB]); raffT = k.res(); gT = sb("gT", [16, TB]); rgT = k.res()
        NBm = TB // 128
        def rt(nm, last): return sb(nm, [128, NBm, last])
        aff = rt("aff", 16); scr = rt("scr", 16); eq = rt("eq", 16); ms = rt("ms", 16); ws_ = rt("ws", 16); rr = k.res()
        m1 = rt("m1", 4); m2 = rt("m2", 4); gs = rt("gs", 4); keep = rt("keep", 4); gmx = rt("gmx", 1); tt1 = rt("tt1", 1); tt2 = rt("tt2", 1); wsum = rt("wsum", 1)
        wst = WStream(k, st, name, KC, nbuf=3)
        psm = k.psum(name + "psm", [128, TB], F32, st); rpsm = k.pres()
        pse = k.psum(name + "pse", [128, TB], F32, st); rpse = k.pres()
        ph1 = k.psum(name + "ph1", [128, TB], F32, st); rph1 = k.pres()
        ph2 = k.psum(name + "ph2", [128, TB], F32, st); rph2 = k.pres()
        po = [k.psum(f"{name}po{i}", [128, TB], F32, st) for i in range(2)]; rpo = [k.pres() for _ in range(2)]
        pgb = k.psum(name + "pgb", [128, TB], F32, st); rpgb = k.pres()
        pms = k.psum(name + "pms", [128, TB], F32, st); rpms = k.pres()
        def do_block(t0, n, which):
            nb = n // 128
            k.dma("sp", x1[:, :, :n], X1[:, t0:t0 + n].rearrange("(c p) t -> p c t", p=128), writes=[rx1])
            k.op("pool", lambda e: e.tensor_copy(out=uf[:, :, :n], in_=x1[:, :, :n]), reads=[rx1], writes=[ruf])
            ln_block(k, uf, ruf, n, ones, rones, epst, reps, psm, rpsm, pse, rpse, acc, racc, mean, rmean, rstd, rrstd)
            affine_block(k, uf, ruf, uf, ruf, n, lambda kc: sc1[:, kc, which:which + 1], lambda kc: mod[:, 3 * KC + kc, which:which + 1], [rsc1, rmod])
            k.op("act", lambda e: e.copy(out=ub[:, :, :n], in_=uf[:, :, :n]), reads=[ruf], writes=[rub])
            for kc in range(KC):
                k.op("pe", lambda e, kc=kc: e.matmul(pms[:16, :n], lhsT=rwt[:, kc, :], rhs=uf[:, kc, :n], start=(kc == 0), stop=(kc == KC - 1)), reads=[rcn, ruf], writes=[rpms])
            k.op("act", lambda e: e.activation(out=affT[:, :n], in_=pms[:16, :n], func=AF.Sigmoid), reads=[rpms], writes=[raffT])
            for tb in range(nb):
                k.op("pe", lambda e, tb=tb: e.transpose(out=pms[:, tb * 16:(tb + 1) * 16], in_=affT[:, tb * 128:(tb + 1) * 128], identity=idt[:16, :16]), reads=[raffT, rcn], writes=[rpms])
            A3 = lambda t_: t_[:, :nb, :]
            k.op("dve", lambda e: e.tensor_copy(out=A3(aff), in_=pms[:, :nb * 16].rearrange("p (b e) -> p b e", e=16)), reads=[rpms], writes=[rr])
            k.op("dve", lambda e: e.tensor_tensor(out=A3(scr), in0=A3(aff), in1=rbt[:].unsqueeze(1).broadcast_to([128, nb, 16]), op=ALU.add), reads=[rr, rcn], writes=[rr])
            g4 = lambda t_: t_[:, :nb, :].rearrange("p b (g e) -> p (b g) e", e=4)
            f4 = lambda t_: t_[:, :nb, :].rearrange("p b g -> p (b g)")
            k.op("dve", lambda e: e.tensor_reduce(out=f4(m1), in_=g4(scr), axis=AX.X, op=ALU.max), reads=[rr], writes=[rr])
            k.op("dve", lambda e: e.tensor_tensor(out=g4(eq), in0=g4(scr), in1=f4(m1).unsqueeze(2).broadcast_to([128, nb * 4, 4]), op=ALU.is_equal), reads=[rr], writes=[rr])
            k.op("dve", lambda e: e.scalar_tensor_tensor(out=A3(ms), in0=A3(eq), scalar=-1e9, in1=A3(scr), op0=ALU.mult, op1=ALU.add), reads=[rr], writes=[rr])
            k.op("dve", lambda e: e.tensor_reduce(out=f4(m2), in_=g4(ms), axis=AX.X, op=ALU.max), reads=[rr], writes=[rr])
            k.op("dve", lambda e: e.tensor_tensor(out=A3(gs), in0=A3(m1), in1=A3(m2), op=ALU.add), reads=[rr], writes=[rr])
            k.op("dve", lambda e: e.tensor_reduce(out=gmx[:, :nb, 0], in_=A3(gs), axis=AX.X, op=ALU.max), reads=[rr], writes=[rr])
            k.op("dve", lambda e: e.tensor_tensor(out=A3(keep), in0=A3(gs), in1=gmx[:, :nb, :].broadcast_to([128, nb, 4]), op=ALU.is_equal), reads=[rr], writes=[rr])
            k.op("dve", lambda e: e.tensor_scalar(out=A3(keep), in0=A3(keep), scalar1=1e9, scalar2=-1e9, op0=ALU.mult, op1=ALU.add), reads=[rr], writes=[rr])
            k.op("dve", lambda e: e.tensor_tensor(out=g4(ms), in0=g4(scr), in1=f4(keep).unsqueeze(2).broadcast_to([128, nb * 4, 4]), op=ALU.add), reads=[rr], writes=[rr])
            k.op("dve", lambda e: e.tensor_reduce(out=tt1[:, :nb, 0], in_=A3(ms), axis=AX.X, op=ALU.max), reads=[rr], writes=[rr])
            k.op("dve", lambda e: e.tensor_tensor(out=A3(eq), in0=A3(ms), in1=tt1[:, :nb, :].broadcast_to([128, nb, 16]), op=ALU.is_equal), reads=[rr], writes=[rr])
            k.op("dve", lambda e: e.scalar_tensor_tensor(out=A3(scr), in0=A3(eq), scalar=-1e9, in1=A3(ms), op0=ALU.mult, op1=ALU.add), reads=[rr], writes=[rr])
            k.op("dve", lambda e: e.tensor_reduce(out=tt2[:, :nb, 0], in_=A3(scr), axis=AX.X, op=ALU.max), reads=[rr], writes=[rr])
            k.op("dve", lambda e: e.tensor_tensor(out=A3(eq), in0=A3(ms), in1=tt2[:, :nb, :].broadcast_to([128, nb, 16]), op=ALU.is_ge), reads=[rr], writes=[rr])
            k.op("dve", lambda e: e.tensor_tensor(out=A3(ws_), in0=A3(aff), in1=A3(eq), op=ALU.mult), reads=[rr], writes=[rr])
            k.op("dve", lambda e: e.tensor_reduce(out=wsum[:, :nb, 0], in_=A3(ws_), axis=AX.X, op=ALU.add), reads=[rr], writes=[rr])
            k.op("dve", lambda e: e.reciprocal(out=wsum[:, :nb, :], in_=wsum[:, :nb, :]), reads=[rr], writes=[rr])
            k.op("dve", lambda e: e.tensor_tensor(out=A3(ws_), in0=A3(ws_), in1=wsum[:, :nb, :].broadcast_to([128, nb, 16]), op=ALU.mult), reads=[rr], writes=[rr])
            for tb in range(nb):
                k.op("pe", lambda e, tb=tb: e.transpose(out=pms[:16, tb * 128:(tb + 1) * 128], in_=ws_[:, tb, :], identity=idt[:]), reads=[rr, rcn], writes=[rpms])
            k.op("act", lambda e: e.copy(out=gT[:, :n], in_=pms[:16, :n]), reads=[rpms], writes=[rgT])
            def expert(e_i):
                k.op("pe", lambda e: e.matmul(pgb[:, :n], lhsT=selt[:, e_i, :], rhs=gT[:, :n], start=True, stop=True), reads=[rcn, rgT], writes=[rpgb])
                k.op("act", lambda e: e.copy(out=gbt[:, :n], in_=pgb[:, :n]), reads=[rpgb], writes=[rgb])
                def hchunk(jc):
                    wb, rwb = wst.get(Wg[e_i], jc * 128, KC)
                    for kc in range(KC):
                        k.op("pe", lambda e, kc=kc: e.matmul(ph1[:, :n], lhsT=wb[:, kc, :], rhs=ub[:, kc, :n], start=(kc == 0), stop=(kc == KC - 1)), reads=[rwb, rub], writes=[rph1])
                    wb2, rwb2 = wst.get(Wu[e_i], jc * 128, KC)
                    for kc in range(KC):
                        k.op("pe", lambda e, kc=kc: e.matmul(ph2[:, :n], lhsT=wb2[:, kc, :], rhs=ub[:, kc, :n], start=(kc == 0), stop=(kc == KC - 1)), reads=[rwb2, rub], writes=[rph2])
                    k.op("act", lambda e: e.activation(out=s1[:, :n], in_=ph1[:, :n], func=AF.Silu), reads=[rph1], writes=[rs1])
                    k.op("dve", lambda e: e.tensor_tensor(out=s2[:, :n], in0=ph2[:, :n], in1=s1[:, :n], op=ALU.mult), reads=[rph2, rs1], writes=[rs2])
                    k.op("pool", lambda e: e.tensor_tensor(out=hb[:, jc, :n], in0=s2[:, :n], in1=gbt[:, :n], op=ALU.mult), reads=[rs2, rgb], writes=[rhb])
                for jc in range(8):
                    hchunk(jc)
                def dchunk(j):
                    wb, rwb = wst.get(Wd[e_i], j * 128, 8)
                    p, rp = po[j % 2], rpo[j % 2]
                    for kc in range(8):
                        k.op("pe", lambda e, kc=kc: e.matmul(p[:, :n], lhsT=wb[:, kc, :], rhs=hb[:, kc, :n], start=(kc == 0), stop=(kc == 7)), reads=[rwb, rhb], writes=[rp])
                    if e_i == 0:
                        k.op("dve", lambda e: e.tensor_copy(out=acc[:, j, :n], in_=p[:, :n]), reads=[rp], writes=[racc])
                    else:
                        k.op("dve", lambda e: e.tensor_tensor(out=acc[:, j, :n], in0=acc[:, j, :n], in1=p[:, :n], op=ALU.add), reads=[rp, racc], writes=[racc])
                for j in range(KC):
                    dchunk(j)
            for e_i in range(NE):
                expert(e_i)
            k.op("act", lambda e: e.mul(out=x1[:, :, :n], in_=x1[:, :, :n], mul=ALPHA), reads=[rx1], writes=[rx1])
            for j in range(KC):
                k.op("dve", lambda e, j=j: e.scalar_tensor_tensor(out=acc[:, j, :n], in0=acc[:, j, :n], scalar=mod[:, 5 * KC + j, which:which + 1], in1=x1[:, j, :n], op0=ALU.mult, op1=ALU.add),
                     reads=[racc, rmod, rx1], writes=[racc])
            ln_block(k, acc, racc, n, ones, rones, epst, reps, psm, rpsm, pse, rpse, uf, ruf, mean, rmean, rstd, rrstd)
            affine_block(k, acc, racc, acc, racc, n, lambda kc: lg[:, kc:kc + 1], lambda kc: lb[:, kc:kc + 1], [rl])
            k.dma("sp", out_fn(t0, n).rearrange("(c p) t -> p c t", p=128), acc[:, :, :n], reads=[racc])
        for (t0, n, which) in blocks:
            do_block(t0, n, which)
        k.barrier()


D = 2048; KC = 16; CTXL = 256; D_IN = 14736

LAYER_INPUTS = [
    ("ada_w", [96, 128, 16, 128]), ("ada_b", [128, 96]), ("w_in", [116, 128, 16, 128]), ("cw", [128, 16, 9]), ("cb", [128, 16]), ("igb", [64, 8]), ("fgb", [64, 8]),
    ("mlng", [128, 8]), ("mlnb", [128, 8]), ("ml_proj", [16, 128, 8, 128]),
    ("mu_pc", [128, 27]), ("w0_pc", [128, 2, 8]), ("a0_pc", [128, 2, 8]), ("w_up", [128, 1024]), ("a_up", [128, 1024]), ("g_up", [128, 1024]),
    ("kk_pc", [128, 8]), ("ka_pc", [128, 8]), ("rk_pc", [128, 8]), ("rwng", [128, 8]), ("rwnb", [128, 8]), ("rw_proj", [16, 128, 8, 128]),
    ("lamN", [64, 3, 2, 64]), ("lamC", [16, 3, 2, 64, 64]), ("Bc", [16, 2, 2, 64, 64]), ("Cn", [64, 2, 2, 64, 16]), ("s5d", [128, 8]),
    ("s5_w_val", [16, 128, 8, 128]), ("s5_w_gate", [16, 128, 8, 128]), ("w_out", [16, 128, 16, 128]),
    ("ln1g", [128, 16]), ("ln1b", [128, 16]), ("ln2g", [128, 16]), ("ln2b", [128, 16]),
    ("exp_wg", [16, 8, 128, 16, 128]), ("exp_wu", [16, 8, 128, 16, 128]), ("exp_wd", [16, 16, 128, 8, 128]),
]
SHARED_INPUTS = [("c2", [128, KC, 2]), ("rw_lay", [128, 16, 16]), ("rb_bc", [128, 16]), ("mlc", [128, 4, 64]), ("ident", [128, 128]),
                 ("rwc", [64, 2, 384]), ("s5c", [64, 2, 512]), ("sel", [16, 16, 128])]

def pc(v):
    return np.ascontiguousarray(np.asarray(v, np.float32).reshape(-1, 128).T)

def blockify(w):
    w = np.asarray(w, np.float32)
    K, N = w.shape
    NJ = (N + 127) // 128
    if NJ * 128 != N:
        w = np.concatenate([w, np.zeros((K, NJ * 128 - N), np.float32)], 1)
    return np.ascontiguousarray(w.reshape(K // 128, 128, NJ, 128).transpose(2, 1, 0, 3))

def host_layer_inputs(inp, l):
    g = lambda n: np.asarray(inp[n][l], np.float32)
    o = {
        "ada_w": blockify(g("ada_w")), "ada_b": pc(g("ada_b")), "w_in": blockify(g("w_in")),
        "cw": np.ascontiguousarray(g("ml_conv_w").reshape(9, 16, 128).transpose(2, 1, 0)), "cb": pc(g("ml_conv_b")),
        "igb": np.ascontiguousarray(np.broadcast_to(g("ml_ig_b").reshape(1, 8), (64, 8))), "fgb": np.ascontiguousarray(np.broadcast_to(g("ml_fg_b").reshape(1, 8), (64, 8))),
        "mlng": pc(g("ml_norm_g")), "mlnb": pc(g("ml_norm_b")), "ml_proj": blockify(g("ml_proj")),
        "mu_pc": pc(g("rw_mu")), "w0_pc": np.ascontiguousarray(g("rw_w0").reshape(2, 8, 128).transpose(2, 0, 1)),
        "a0_pc": np.ascontiguousarray(g("rw_a0").reshape(2, 8, 128).transpose(2, 0, 1)),
        "w_up": np.ascontiguousarray(g("rw_w_up").reshape(128, 1024)), "a_up": np.ascontiguousarray(g("rw_a_up").reshape(128, 1024)), "g_up": g("rw_g_up"),
        "kk_pc": pc(g("rw_k_k")), "ka_pc": pc(g("rw_k_a")), "rk_pc": pc(g("rw_r_k").reshape(-1)), "rwng": pc(g("rw_norm_g")), "rwnb": pc(g("rw_norm_b")),
        "rw_proj": blockify(g("rw_proj")), "s5d": pc(g("s5_d")), "s5_w_val": blockify(g("s5_w_val")), "s5_w_gate": blockify(g("s5_w_gate")), "w_out": blockify(g("w_out")),
        "ln1g": pc(g("ln1_g")), "ln1b": pc(g("ln1_b")), "ln2g": pc(g("ln2_g")), "ln2b": pc(g("ln2_b")),
        "exp_wg": np.stack([blockify(w) for w in g("exp_w_gate")]), "exp_wu": np.stack([blockify(w) for w in g("exp_w_up")]), "exp_wd": np.stack([blockify(w) for w in g("exp_w_down")]),
    }
    o.update(s5_host_layout(g("s5_lam_re"), g("s5_lam_im"), g("s5_log_dt"), g("s5_b_re"), g("s5_b_im"), g("s5_c_re"), g("s5_c_im")))
    return {k_: np.ascontiguousarray(v, dtype=np.float32) for k_, v in o.items()}

def host_shared_inputs(inp, b):
    cc = np.stack([np.asarray(inp["c"][b], np.float32), np.asarray(inp["c_ctx"], np.float32)], 0)
    return {
        "c2": np.ascontiguousarray(cc.reshape(2, KC, 128).transpose(2, 1, 0)),
        "rw_lay": np.ascontiguousarray(np.asarray(inp["router_w"], np.float32).reshape(KC, 128, 16).transpose(1, 0, 2)),
        "rb_bc": np.ascontiguousarray(np.broadcast_to(np.asarray(inp["router_b"], np.float32).reshape(1, 16), (128, 16))),
        "mlc": make_ml_consts(), "ident": np.eye(128, dtype=np.float32), "rwc": make_rw_consts(), "s5c": make_s5_consts(), "sel": make_sel_const(),
    }

def build_program(T, n_layers, out_ctx=False):
    nc = bass.Bass("TRN2", target_bir_lowering=False)
    def din(n, s): return nc.dram_tensor(n, list(s), F32, kind="ExternalInput").ap()
    def dsc(n, s): return nc.dram_tensor(n, list(s), F32).ap()
    NL = T - CTXL
    xT = din("xT", [D, T])
    sh = {n: din(n, s) for n, s in SHARED_INPUTS}
    lay = [{n: din(f"L{l}_{n}", s) for n, s in LAYER_INPUTS} for l in range(n_layers)]
    outT = nc.dram_tensor("outT", [D, NL], F32, kind="ExternalOutput").ap()
    outC = nc.dram_tensor("outC", [D, CTXL], F32, kind="ExternalOutput").ap() if out_ctx else None
    PTA_ROWS = 7568
    PT = dsc("PTa", [PTA_ROWS, T]); PTb = dsc("PTb", [D_IN - PTA_ROWS, T]); PTs = SplitRows([(0, PTA_ROWS, PT), (PTA_ROWS, D_IN, PTb)])
    QK = dsc("QK", [2048, T]); HD = dsc("HD", [2, 1024, T]); YD = dsc("YD", [2, 1024, T]); YSD = dsc("YSD", [2, 1024, T])
    YML = dsc("YML", [1024, T]); YRW = dsc("YRW", [1024, T]); YS5 = dsc("YS5", [1024, T])
    rwo = {n: dsc("s_" + n, [1024, T]) for n in ("R", "KK", "V", "BV", "G")}
    rwo.update({n: dsc("s_" + n, [2, 1024, T]) for n in ("LW", "KD", "A")})
    X1 = dsc("X1", [D, T]); X2 = dsc("X2", [D, T])
    k = KB(nc)
    mod = k.sbuf("mod", [128, 96, 2], F32); rmod = k.res()
    ones = k.sbuf("ones", [128, 128], F32); rones = k.res()
    k.op("pool", lambda e: e.memset(ones[:], 1.0 / D), writes=[rones])
    blocks_all = [(0, CTXL, 1)] + [(t, min(512, T - t), 0) for t in range(CTXL, T, 512)]
    blocks_lat = blocks_all[1:]
    xin = xT
    for l in range(n_layers):
        W = lay[l]; last = (l == n_layers - 1)
        phase_adaln(k, W["ada_w"], W["ada_b"], sh["c2"], mod, rmod, name=f"ad{l}_") if False else phase_adaln(k, W["ada_w"], W["ada_b"], sh["c2"], mod, rmod)
        phase_ln_gemm(k, xin, W["w_in"], PTs, D_IN, T, blocks_all, mod, rmod, 0, 1, ones, rones)
        phase_conv(k, PT, QK, W["cw"], W["cb"], T)
        phase_mlstm(k, PT, QK, HD, W["igb"], W["fgb"], sh["mlc"], sh["ident"], T)
        phase_ml_finish(k, HD, PT, YML, W["mlng"], W["mlnb"], T)
        phase_rw_prep(k, PT, rwo, W, T)
        phase_rwkv_scan(k, rwo["R"], rwo["KK"], rwo["V"], rwo["LW"], rwo["KD"], rwo["A"], YD, sh["rwc"], sh["ident"], T)
        phase_rw_finish(k, YD, rwo["BV"], rwo["G"], YRW, W["rwng"], W["rwnb"], T)
        phase_s5(k, PTb, YSD, W["lamN"], W["lamC"], W["Bc"], W["Cn"], sh["s5c"], T)
        phase_s5_finish(k, PTb, YSD, YS5, W["s5d"], T)
        blk = blocks_all if (not last or out_ctx) else blocks_lat
        phase_merge(k, xin, X1, PTb, YML, YRW, YS5, W, mod, rmod, W["ln1g"], W["ln1b"], blk)
        if last:
            def out_fn(t0, n):
                return outC[:, t0:t0 + n] if t0 < CTXL else outT[:, t0 - CTXL:t0 - CTXL + n]
        else:
            def out_fn(t0, n):
                return X2[:, t0:t0 + n]
        phase_moe(k, X1, out_fn, W["exp_wg"], W["exp_wu"], W["exp_wd"], sh["rw_lay"], sh["rb_bc"], sh["sel"], sh["ident"], mod, rmod, W["ln2g"], W["ln2b"], blk)
        xin = X2
    k.emit()
    return nc


SEQ = 8192; BATCH = 4; DEPTH = 2
N_CORES = 4


def kernel(**inp):
    T = CTXL + SEQ
    nc = build_program(T, DEPTH, out_ctx=False)
    lay = [host_layer_inputs(inp, l) for l in range(DEPTH)]
    x = np.asarray(inp["x"], np.float32); ctx = np.asarray(inp["ctx"], np.float32)
    in_maps = []
    for b in range(N_CORES):
        im = {"xT": np.ascontiguousarray(np.concatenate([ctx[b], x[b]], 0).T)}
        im.update(host_shared_inputs(inp, b))
        for l in range(DEPTH):
            im.update({f"L{l}_{n}": v for n, v in lay[l].items()})
        in_maps.append(im)
    res = run_bass_kernel_spmd(nc, in_maps, core_ids=list(range(N_CORES)))
    out = np.empty((BATCH, SEQ, D), np.float32)
    for b in range(N_CORES):
        out[b] = res.results[b]["outT"].T
    return out
```

```python
import contextlib
import numpy as np
import concourse.bass as bass
import concourse.mybir as mybir
from concourse.bass_utils import run_bass_kernel_spmd
import math

F32 = mybir.dt.float32
BF16 = mybir.dt.bfloat16
ALU = mybir.AluOpType
AF = mybir.ActivationFunctionType
AX = mybir.AxisListType

class Res:
    __slots__ = ("name", "writer", "readers", "excl")
    def __init__(self, name, excl=False):
        self.name = name
        self.excl = excl
        self.writer = None
        self.readers = []

class KB:
    ENGS = ("pe", "act", "dve", "pool", "sp")
    def __init__(self, nc, n_dma_sems=10):
        self.nc = nc
        self.stack = contextlib.ExitStack()
        self.prog = {e: [] for e in self.ENGS}
        self.sem = {e: self.stack.enter_context(nc.semaphore("s_" + e)) for e in self.ENGS}
        self.cnt = {e: 0 for e in self.ENGS}
        self.dsem = {}
        self.dcnt = {}
        self.dnext = {}
        for q in ("sp", "act", "pool"):
            self.dsem[q] = [self.stack.enter_context(nc.semaphore(f"d_{q}{i}")) for i in range(n_dma_sems)]
            self.dcnt[q] = [0] * n_dma_sems
            self.dnext[q] = 0
        self.known = {e: {} for e in self.ENGS}
        self.hist = {e: [] for e in self.ENGS}
        self.pruned = 0
        self.nres = 0
        self.nalloc = 0

    def res(self, name=None, excl=False):
        self.nres += 1
        return Res(name or f"r{self.nres}", excl)
    def pres(self):
        return self.res(excl=True)

    def sbuf(self, name, shape, dtype, st=None):
        self.nalloc += 1
        t = (st or self.stack).enter_context(self.nc.sbuf_tensor(f"{name}_{self.nalloc}", list(shape), dtype))
        return t
    def psum(self, name, shape, dtype, st=None):
        assert dtype == F32
        self.nalloc += 1
        t = (st or self.stack).enter_context(self.nc.psum_tensor(f"{name}_{self.nalloc}", [128, 512], F32))
        n = 1
        for d in shape[1:]:
            n *= d
        assert n <= 512
        v = t[:shape[0], :n]
        if len(shape) == 3:
            v = v.rearrange("p (a b) -> p a b", b=shape[2])
        return v

    def _collect(self, e, reads, writes):
        waits = {}
        def add(sv):
            if sv is None:
                return
            s, v = sv
            k = id(s)
            if k not in waits or waits[k][1] < v:
                waits[k] = (s, v)
        for r in reads:
            add(r.writer)
        for w in writes:
            add(w.writer)
            for rd in w.readers:
                add(rd)
        out = []
        semeng = {id(self.sem[x]): x for x in self.ENGS}
        import bisect
        changed = False
        for k, (s, v) in sorted(waits.items(), key=lambda kv: -kv[1][1]):
            if e == "pe" and s is self.sem["pe"]:
                continue
            if self.known[e].get(k, 0) >= v:
                self.pruned += 1
                continue
            self.known[e][k] = v
            changed = True
            out.append((s, v))
            f = semeng.get(k)
            if f is not None and f != e and self.hist[f]:
                h = self.hist[f]
                i = bisect.bisect_right([c for c, _ in h], v) - 1 if len(h) < 64 else None
                if i is None:
                    lo, hi = 0, len(h)
                    while lo < hi:
                        mid = (lo + hi) // 2
                        if h[mid][0] <= v: lo = mid + 1
                        else: hi = mid
                    i = lo - 1
                if i >= 0:
                    for kk, vv in h[i][1].items():
                        if self.known[e].get(kk, 0) < vv:
                            self.known[e][kk] = vv
        if changed and e in self.hist:
            self.hist[e].append((self.cnt[e] + 1, dict(self.known[e])))
        return out

    def op(self, e, fn, reads=(), writes=()):
        writes = list(writes) + [r for r in reads if r.excl]
        reads = [r for r in reads if not r.excl]
        waits = self._collect(e, reads, writes)
        self.cnt[e] += 1
        n = self.cnt[e]
        s = self.sem[e]
        self.prog[e].append((waits, fn, (s, 1)))
        for r in reads:
            r.readers.append((s, n))
            if len(r.readers) > 8:
                r.readers = self._compact(r.readers)
        for w in writes:
            w.writer = (s, n)
            w.readers = []
        return n

    @staticmethod
    def _compact(lst):
        best = {}
        for s, v in lst:
            k = id(s)
            if k not in best or best[k][1] < v:
                best[k] = (s, v)
        return list(best.values())

    def dma(self, q, out, in_, reads=(), writes=(), **kw):
        waits = self._collect(q, reads, writes)
        i = self.dnext[q]
        self.dnext[q] = (i + 1) % len(self.dsem[q])
        S = self.dsem[q][i]
        prev = self.dcnt[q][i]
        if prev > 0 and self.known[q].get(id(S), 0) < prev:
            self.known[q][id(S)] = prev
            waits.append((S, prev))
        val = prev + 16
        self.dcnt[q][i] = val
        self.prog[q].append((waits, lambda eng: eng.dma_start(out=out, in_=in_, **kw), (S, 16)))
        for r in reads:
            r.readers.append((S, val))
            if len(r.readers) > 8:
                r.readers = self._compact(r.readers)
        for w in writes:
            w.writer = (S, val)
            w.readers = []
        return (S, val)

    def barrier(self):
        targets = []
        for e in self.ENGS:
            if self.cnt[e] > 0:
                targets.append((self.sem[e], self.cnt[e]))
        for q in self.dsem:
            for S, v in zip(self.dsem[q], self.dcnt[q]):
                if v > 0:
                    targets.append((S, v))
        for e in self.ENGS:
            waits = []
            for s, v in targets:
                if s is self.sem[e]:
                    continue
                if self.known[e].get(id(s), 0) >= v:
                    continue
                self.known[e][id(s)] = v
                waits.append((s, v))
            if waits:
                self.prog[e].append((waits, None, None))

    def emit(self):
        self.barrier()
        nc = self.nc
        engmap = {"pe": "tensor", "act": "scalar", "dve": "vector", "pool": "gpsimd", "sp": "sync"}
        with nc.Block() as block:
            for e in self.ENGS:
                prog = self.prog[e]
                def body(eng, prog=prog):
                    for waits, fn, inc in prog:
                        for s, v in waits:
                            eng.wait_ge(s, v)
                        if fn is not None:
                            ins = fn(eng)
                            ins.then_inc(inc[0], inc[1])
                getattr(block, engmap[e])(body)
        self.stack.close()


D = 2048; KC = 16; NMOD = 6
LN_EPS = 1e-5

class SplitRows:
    def __init__(self, parts):
        self.parts = parts
    def pieces(self, r0, r1):
        out = []
        for (a, b, ap) in self.parts:
            lo, hi = max(a, r0), min(b, r1)
            if lo < hi:
                out.append((ap[lo - a:hi - a, :], lo - r0, hi - r0))
        return out

def phase_adaln(k, ada_w, ada_b_pc, c2, mod, rmod, ones_unused=None):
    nc = k.nc
    with contextlib.ExitStack() as st:
        ct = k.sbuf("ad_c", [128, KC, 2], F32, st); rc = k.res()
        cs = k.sbuf("ad_cs", [128, KC, 2], F32, st); rcs = k.res()
        bt = k.sbuf("ad_b", [128, 96], F32, st); rb = k.res()
        NB = 3
        wts = [k.sbuf(f"ad_w{i}", [128, KC, 128], F32, st) for i in range(NB)]
        rws = [k.res() for _ in range(NB)]
        pss = [k.psum(f"ad_ps{i}", [128, 2], F32, st) for i in range(2)]
        rps = [k.pres() for _ in range(2)]
        k.dma("sp", ct[:], c2, writes=[rc])
        k.dma("sp", bt[:], ada_b_pc, writes=[rb])
        k.op("act", lambda e: e.activation(out=cs[:], in_=ct[:], func=AF.Silu), reads=[rc], writes=[rcs])
        for j in range(96):
            wt, rw = wts[j % NB], rws[j % NB]
            ps, rp = pss[j % 2], rps[j % 2]
            k.dma(("sp", "pool")[j % 2], wt[:], ada_w[j], writes=[rw])
            for kc in range(KC):
                k.op("pe", lambda e, wt=wt, ps=ps, kc=kc: e.matmul(ps[:], lhsT=wt[:, kc, :], rhs=cs[:, kc, :],
                                                                    start=(kc == 0), stop=(kc == KC - 1)),
                     reads=[rw, rcs], writes=[rp])
            k.op("dve", lambda e, ps=ps, j=j: e.tensor_scalar(out=mod[:, j, :], in0=ps[:], scalar1=bt[:, j:j + 1],
                                                              scalar2=None, op0=ALU.add),
                 reads=[rp, rb], writes=[rmod])
        k.barrier()

def phase_ln_gemm(k, xT, w, outT, n_cols, T, blocks, mod, rmod, shift_idx, scale_idx, ones, rones, name="g1"):
    nc = k.nc
    TB = 512
    ncc = (n_cols + 127) // 128
    with contextlib.ExitStack() as st:
        xt = k.sbuf(name + "x", [128, KC, TB], F32, st); rx = k.res()
        sq = k.sbuf(name + "sq", [128, KC, TB], F32, st); rsq = k.res()
        u = k.sbuf(name + "u", [128, KC, TB], BF16, st); ru = k.res()
        mean = k.sbuf(name + "mean", [128, TB], F32, st); rmean = k.res()
        rstd = k.sbuf(name + "rstd", [128, TB], F32, st); rrstd = k.res()
        tmp = k.sbuf(name + "tmp", [128, KC, TB], F32, st); rtmp = k.res()
        sc1 = k.sbuf(name + "sc1", [128, KC, 2], F32, st); rsc1 = k.res()
        psm = k.psum(name + "psm", [128, TB], F32, st); rpsm = k.pres()
        pse = k.psum(name + "pse", [128, TB], F32, st); rpse = k.pres()
        NW = 3
        wf = [k.sbuf(f"{name}wf{i}", [128, KC, 128], F32, st) for i in range(NW)]; rwf = [k.res() for _ in range(NW)]
        wb = [k.sbuf(f"{name}wb{i}", [128, KC, 128], BF16, st) for i in range(NW)]; rwb = [k.res() for _ in range(NW)]
        NP = 3
        pso = [k.psum(f"{name}pso{i}", [128, TB], F32, st) for i in range(NP)]; rpso = [k.pres() for _ in range(NP)]
        ot = [k.sbuf(f"{name}ot{i}", [128, TB], F32, st) for i in range(NP)]; rot = [k.res() for _ in range(NP)]
        epst = k.sbuf(name + "eps", [128, 1], F32, st); reps = k.res()
        k.op("pool", lambda e: e.memset(epst[:], LN_EPS), writes=[reps])
        k.op("dve", lambda e: e.tensor_scalar(out=sc1[:], in0=mod[:, scale_idx * KC:(scale_idx + 1) * KC, :], scalar1=1.0,
                                              scalar2=None, op0=ALU.add), reads=[rmod], writes=[rsc1])
        xv = xT.rearrange("(kc p) t -> p kc t", p=128)
        it = 0
        for (t0, n, which) in blocks:
            k.dma("sp", xt[:, :, :n], xv[:, :, t0:t0 + n], writes=[rx])
            k.op("act", lambda e, n=n: e.activation(out=sq[:, :, :n], in_=xt[:, :, :n], func=AF.Square), reads=[rx], writes=[rsq])
            for kc in range(KC):
                k.op("pe", lambda e, kc=kc, n=n: e.matmul(psm[:, :n], lhsT=ones[:], rhs=xt[:, kc, :n], start=(kc == 0), stop=(kc == KC - 1)),
                     reads=[rx, rones], writes=[rpsm])
            for kc in range(KC):
                k.op("pe", lambda e, kc=kc, n=n: e.matmul(pse[:, :n], lhsT=ones[:], rhs=sq[:, kc, :n], start=(kc == 0), stop=(kc == KC - 1)),
                     reads=[rsq, rones], writes=[rpse])
            k.op("dve", lambda e, n=n: e.tensor_copy(out=mean[:, :n], in_=psm[:, :n]), reads=[rpsm], writes=[rmean])
            k.op("dve", lambda e, n=n: e.tensor_tensor(out=rstd[:, :n], in0=mean[:, :n], in1=mean[:, :n], op=ALU.mult), reads=[rmean], writes=[rrstd])
            k.op("dve", lambda e, n=n: e.tensor_tensor(out=rstd[:, :n], in0=pse[:, :n], in1=rstd[:, :n], op=ALU.subtract), reads=[rpse, rrstd], writes=[rrstd])
            k.op("act", lambda e, n=n: e.activation(out=rstd[:, :n], in_=rstd[:, :n], func=AF.Sqrt, bias=epst[:, 0:1]),
                 reads=[rrstd, reps], writes=[rrstd])
            k.op("dve", lambda e, n=n: e.reciprocal(out=rstd[:, :n], in_=rstd[:, :n]), reads=[rrstd], writes=[rrstd])
            k.op("dve", lambda e, n=n: e.tensor_tensor(out=tmp[:, :, :n], in0=xt[:, :, :n],
                                                       in1=mean[:, :n].unsqueeze(1).broadcast_to([128, KC, n]), op=ALU.subtract),
                 reads=[rx, rmean], writes=[rtmp])
            k.op("pool", lambda e, n=n: e.tensor_tensor(out=tmp[:, :, :n], in0=tmp[:, :, :n],
                                                        in1=rstd[:, :n].unsqueeze(1).broadcast_to([128, KC, n]), op=ALU.mult),
                 reads=[rtmp, rrstd], writes=[rtmp])
            for kc in range(KC):
                k.op(("dve", "act")[0], lambda e, kc=kc, n=n, which=which: e.tensor_scalar(
                    out=u[:, kc, :n], in0=tmp[:, kc, :n], scalar1=sc1[:, kc, which:which + 1],
                    scalar2=mod[:, shift_idx * KC + kc, which:which + 1], op0=ALU.mult, op1=ALU.add),
                    reads=[rtmp, rsc1, rmod], writes=[ru])
            for j in range(ncc):
                cw = min(128, n_cols - j * 128)
                b = it % NW
                k.dma(("act", "pool")[it % 2], wf[b][:, :, :cw], w[j][:, :, :cw], writes=[rwf[b]])
                k.op(("act", "pool")[it % 2], (lambda e, b=b, cw=cw: e.activation(out=wb[b][:, :, :cw], in_=wf[b][:, :, :cw], func=AF.Copy)) if it % 2 == 0
                     else (lambda e, b=b, cw=cw: e.tensor_copy(out=wb[b][:, :, :cw], in_=wf[b][:, :, :cw])),
                     reads=[rwf[b]], writes=[rwb[b]])
                p = it % NP
                for kc in range(KC):
                    k.op("pe", lambda e, kc=kc, b=b, p=p, cw=cw, n=n: e.matmul(pso[p][:cw, :n], lhsT=wb[b][:, kc, :cw], rhs=u[:, kc, :n],
                                                                            start=(kc == 0), stop=(kc == KC - 1)),
                         reads=[rwb[b], ru], writes=[rpso[p]])
                k.op("dve", lambda e, p=p, cw=cw, n=n: e.tensor_copy(out=ot[p][:cw, :n], in_=pso[p][:cw, :n]), reads=[rpso[p]], writes=[rot[p]])
                for (oap, a, b_) in outT.pieces(j * 128, j * 128 + cw):
                    k.dma("sp", oap[:, t0:t0 + n], ot[p][a:b_, :n], reads=[rot[p]])
                it += 1
        k.barrier()


GRID_W = 64; CTXL = 256

def phase_conv(k, PT, QK, cw_pc, cb_pc, T, name="cv"):
    R = (T - CTXL) // GRID_W
    with contextlib.ExitStack() as st:
        cw = k.sbuf(name + "w", [128, 16, 9], F32, st); rcw = k.res()
        cb = k.sbuf(name + "b", [128, 16], F32, st); rcb = k.res()
        k.dma("sp", cw[:], cw_pc, writes=[rcw]); k.dma("sp", cb[:], cb_pc, writes=[rcb])
        NB = 2
        zs = [k.sbuf(f"{name}z{i}", [128, T], F32, st) for i in range(NB)]; rz = [k.res() for _ in range(NB)]
        accs = [k.sbuf(f"{name}a{i}", [128, T], F32, st) for i in range(NB)]; ra = [k.res() for _ in range(NB)]
        for j in range(16):
            z, a = zs[j % NB], accs[j % NB]; rzj, raj = rz[j % NB], ra[j % NB]
            k.dma(("sp", "pool")[j % 2], z[:], PT[j * 128:(j + 1) * 128, :], writes=[rzj])
            k.op("act", lambda e, z=z, a=a, j=j: e.activation(out=a[:], in_=z[:], func=AF.Identity, scale=cw[:, j, 4:5], bias=cb[:, j:j + 1]),
                 reads=[rzj, rcw, rcb], writes=[raj])
            zl = z[:, CTXL:].rearrange("p (r c) -> p r c", c=GRID_W); al = a[:, CTXL:].rearrange("p (r c) -> p r c", c=GRID_W)
            for dy in (-1, 0, 1):
                for dx in (-1, 0, 1):
                    if dy == 0 and dx == 0:
                        continue
                    tap = (dy + 1) * 3 + (dx + 1)
                    r0, r1 = max(0, -dy), R - max(0, dy)
                    c0, c1 = max(0, -dx), GRID_W - max(0, dx)
                    if r1 > r0:
                        k.op("dve", lambda e, al=al, zl=zl, r0=r0, r1=r1, c0=c0, c1=c1, dy=dy, dx=dx, j=j, tap=tap: e.scalar_tensor_tensor(
                            out=al[:, r0:r1, c0:c1], in0=zl[:, r0 + dy:r1 + dy, c0 + dx:c1 + dx], scalar=cw[:, j, tap:tap + 1],
                            in1=al[:, r0:r1, c0:c1], op0=ALU.mult, op1=ALU.add), reads=[rzj, raj, rcw], writes=[raj])
                    if dy == 0:
                        k.op("dve", lambda e, a=a, z=z, c0=c0, dx=dx, j=j, tap=tap: e.scalar_tensor_tensor(
                            out=a[:, c0:CTXL - max(0, dx)], in0=z[:, c0 + dx:CTXL - max(0, dx) + dx], scalar=cw[:, j, tap:tap + 1],
                            in1=a[:, c0:CTXL - max(0, dx)], op0=ALU.mult, op1=ALU.add), reads=[rzj, raj, rcw], writes=[raj])
            k.op("act", lambda e, a=a: e.activation(out=a[:], in_=a[:], func=AF.Silu), reads=[raj], writes=[raj])
            k.dma(("sp", "pool")[(j + 1) % 2], QK[j * 128:(j + 1) * 128, :], a[:], reads=[raj])
        k.barrier()


L = 64; ML_H = 4; ML_DH = 256

def make_ml_consts():
    t = np.arange(64)
    tri_f = (t[:, None] <= t[None, :]).astype(np.float32)
    tri_b = (t[:, None] >= t[None, :]).astype(np.float32)
    neg_f = np.where(t[:, None] <= t[None, :], 0.0, -30000.0).astype(np.float32)
    neg_b = np.where(t[:, None] >= t[None, :], 0.0, -30000.0).astype(np.float32)
    c = np.zeros((128, 4, 64), np.float32)
    c[:64, 0] = tri_f; c[:64, 1] = tri_b; c[:64, 2] = neg_f; c[:64, 3] = neg_b
    return c

def phase_mlstm(k, PT, QK, HD, igb_bc, fgb_bc, mlc, ident, T, name="mls_"):
    NCH = T // L
    order = {0: list(range(NCH)), 1: [3, 2, 1, 0] + list(range(NCH - 1, 3, -1))}
    with contextlib.ExitStack() as st:
        cst = k.sbuf(name + "c", [128, 4, 64], F32, st); rcst = k.res()
        idt = k.sbuf(name + "id", [128, 128], F32, st); rid = k.res()
        ones = k.sbuf(name + "ones", [64, 128], F32, st); rones = k.res()
        igb = k.sbuf(name + "igb", [64, 8], F32, st); fgb = k.sbuf(name + "fgb", [64, 8], F32, st); rgb = k.res()
        gT = k.sbuf(name + "gT", [16, T], F32, st); rgT = k.res()
        LI = k.sbuf(name + "LI", [64, NCH, 8], F32, st); LF = k.sbuf(name + "LF", [64, NCH, 8], F32, st); rLI = k.res(); rLF = k.res()
        one1 = k.sbuf(name + "one1", [64, 1], F32, st); rone1 = k.res()
        k.dma("sp", cst[:], mlc, writes=[rcst]); k.dma("sp", idt[:], ident, writes=[rid])
        k.dma("sp", igb[:], igb_bc, writes=[rgb]); k.dma("sp", fgb[:], fgb_bc, writes=[rgb])
        k.dma("sp", gT[:], PT[4096:4112, :], writes=[rgT])
        k.op("pool", lambda e: e.memset(ones[:], 1.0), writes=[rones])
        k.op("pool", lambda e: e.memset(one1[:], 1.0), writes=[rone1])
        pst = k.psum(name + "pst", [64, 512], F32, st); rpst = k.pres()
        for c0 in range(0, NCH, 32):
            nb = min(32, NCH - c0)
            for c in range(c0, c0 + nb):
                k.op("pe", lambda e, c=c, c0=c0: e.transpose(out=pst[:, (c - c0) * 16:(c - c0 + 1) * 16], in_=gT[:, c * L:(c + 1) * L], identity=idt[:16, :16]),
                     reads=[rgT, rid], writes=[rpst])
            pv = pst[:, :nb * 16].rearrange("p (c g) -> p c g", g=16)
            k.op("dve", lambda e, pv=pv, c0=c0, nb=nb: e.tensor_tensor(out=LI[:, c0:c0 + nb, :], in0=pv[:, :, 0:8],
                                                                       in1=igb[:].unsqueeze(1).broadcast_to([64, nb, 8]), op=ALU.add),
                 reads=[rpst, rgb], writes=[rLI])
            k.op("dve", lambda e, pv=pv, c0=c0, nb=nb: e.tensor_tensor(out=LF[:, c0:c0 + nb, :], in0=pv[:, :, 8:16],
                                                                       in1=fgb[:].unsqueeze(1).broadcast_to([64, nb, 8]), op=ALU.add),
                 reads=[rpst, rgb], writes=[rLF])
        k.op("act", lambda e: e.activation(out=LF[:], in_=LF[:], func=AF.Exp, scale=-1.0), reads=[rLF], writes=[rLF])
        k.op("act", lambda e: e.activation(out=LF[:], in_=LF[:], func=AF.Ln, bias=one1[:, 0:1]), reads=[rLF, rone1], writes=[rLF])
        k.op("dve", lambda e: e.tensor_scalar(out=LF[:], in0=LF[:], scalar1=-1.0, scalar2=None, op0=ALU.mult), reads=[rLF], writes=[rLF])

        STOP = 99
        if STOP == 0:
            k.barrier(); return
        BLK = 8
        NB = 2
        qb = [k.sbuf(f"{name}q{i}", [128, 2, BLK * L], F32, st) for i in range(NB)]
        kb_ = [k.sbuf(f"{name}k{i}", [128, 2, BLK * L], F32, st) for i in range(NB)]
        vb = [k.sbuf(f"{name}v{i}", [128, 2, BLK * L], F32, st) for i in range(NB)]
        hb = [k.sbuf(f"{name}h{i}", [128, 2, BLK * L], F32, st) for i in range(NB)]
        rq = [k.res() for _ in range(NB)]; rk = [k.res() for _ in range(NB)]; rv = [k.res() for _ in range(NB)]; rh = [k.res() for _ in range(NB)]
        CT = k.sbuf(name + "CT", [128, 2, 257], F32, st); rCT = k.res()
        ktm = k.sbuf(name + "ktm", [64, 256], F32, st); rktm = k.res()
        vtm = k.sbuf(name + "vtm", [64, 257], F32, st); rvtm = k.res()
        vw = k.sbuf(name + "vw", [64, 257], F32, st); rvw = k.res()
        col = k.sbuf(name + "col", [64, 1], F32, st); rcol = k.res()
        wts = k.sbuf(name + "wts", [64, 1], F32, st); rwts = k.res()
        bl = k.sbuf(name + "bl", [128, 1], F32, st); rbl = k.res()
        ebl = k.sbuf(name + "ebl", [128, 1], F32, st); rebl = k.res()
        tmp = k.sbuf(name + "tmp", [64, 64], F32, st); rtmp = k.res()
        DT = k.sbuf(name + "DT", [64, 64], F32, st); rDT = k.res()
        ScT = k.sbuf(name + "ScT", [64, 64], F32, st); rScT = k.res()
        ebc = k.sbuf(name + "ebc", [128, 64], F32, st); rebc = k.res()
        qs = k.sbuf(name + "qs", [128, 2, 64], F32, st); rqs = k.res()
        rden = k.sbuf(name + "rden", [128, 64], F32, st); rrden = k.res()
        p_bc = k.psum(name + "pbc", [128, 64], F32, st); rpbc = k.pres()
        p_col = k.psum(name + "pcol", [64, 1], F32, st); rpcol = k.pres()
        p_sc = k.psum(name + "psc", [64, 64], F32, st); rpsc = k.pres()
        p_nm = k.psum(name + "pnm", [128, 3, 64], F32, st); rpnm = k.pres()
        p_dc = [k.psum(f"{name}pdc{i}", [128, 257], F32, st) for i in range(2)]; rpdc = [k.pres() for _ in range(2)]
        p_tr = k.psum(name + "ptr", [64, 512], F32, st); rptr = k.pres()
        k.op("pool", lambda e: e.memset(vtm[:, 256:257], 1.0), writes=[rvtm])
        it = 0
        for h in range(ML_H):
            for d in range(2):
                tri = cst[:64, d, :]; neg = cst[:64, 2 + d, :]
                last = L - 1 if d == 0 else 0
                g = d * 4 + h
                k.op("pool", lambda e: e.memset(CT[:], 0.0), writes=[rCT])
                chunks = order[d]
                groups = []
                i = 0
                while i < len(chunks):
                    grp = [chunks[i]]
                    while len(grp) < BLK and i + len(grp) < len(chunks) and abs(chunks[i + len(grp)] - grp[-1]) == 1 \
                            and (chunks[i + len(grp)] // BLK == grp[0] // BLK):
                        grp.append(chunks[i + len(grp)])
                    groups.append(grp); i += len(grp)
                for gi, grp in enumerate(groups):
                    if STOP == 1 and (h, d, gi) != (0, 0, 0): continue
                    lo = min(grp); n = len(grp)
                    b = it % NB; it += 1
                    t0 = lo * L; tn = n * L
                    qv = QK[h * 256:(h + 1) * 256, t0:t0 + tn].rearrange("(dc p) t -> p dc t", p=128)
                    kv = QK[1024 + h * 256:1024 + (h + 1) * 256, t0:t0 + tn].rearrange("(dc p) t -> p dc t", p=128)
                    vv = PT[2048 + h * 256:2048 + (h + 1) * 256, t0:t0 + tn].rearrange("(dc p) t -> p dc t", p=128)
                    k.dma("sp", qb[b][:, :, :tn], qv, writes=[rq[b]])
                    k.dma("act", kb_[b][:, :, :tn], kv, writes=[rk[b]])
                    k.dma("pool", vb[b][:, :, :tn], vv, writes=[rv[b]])
                    k.op("act", lambda e, b=b, tn=tn: e.mul(out=kb_[b][:, :, :tn], in_=kb_[b][:, :, :tn], mul=1.0 / 16.0), reads=[rk[b]], writes=[rk[b]])
                    for c in grp:
                        if STOP < 50:
                            break
                        o = (c - lo) * L
                        lfc = LF[:, c, g:g + 1]; lic = LI[:, c, g:g + 1]
                        for dc in range(2):
                            k.op("pe", lambda e, b=b, dc=dc, o=o: e.transpose(out=p_tr[:, dc * 128:(dc + 1) * 128], in_=kb_[b][:, dc, o:o + L], identity=idt[:]),
                                 reads=[rk[b], rid], writes=[rptr])
                            k.op("pe", lambda e, b=b, dc=dc, o=o: e.transpose(out=p_tr[:, 256 + dc * 128:256 + (dc + 1) * 128], in_=vb[b][:, dc, o:o + L], identity=idt[:]),
                                 reads=[rv[b], rid], writes=[rptr])
                        k.op("dve", lambda e: e.tensor_copy(out=ktm[:], in_=p_tr[:, 0:256]), reads=[rptr], writes=[rktm])
                        k.op("act", lambda e: e.copy(out=vtm[:, 0:256], in_=p_tr[:, 256:512]), reads=[rptr], writes=[rvtm])
                        if STOP <= 51: continue
                        k.op("pe", lambda e, lfc=lfc, tri=tri: e.matmul(p_bc[:], lhsT=lfc.broadcast_to([64, 128]), rhs=tri, start=True, stop=True),
                             reads=[rLF, rcst], writes=[rpbc])
                        k.op("pe", lambda e, lfc=lfc, tri=tri: e.matmul(p_col[:], lhsT=tri, rhs=lfc, start=True, stop=True),
                             reads=[rLF, rcst], writes=[rpcol])
                        k.op("dve", lambda e, lic=lic: e.tensor_tensor(out=col[:], in0=lic, in1=p_col[:], op=ALU.subtract), reads=[rLI, rpcol], writes=[rcol])
                        k.op("dve", lambda e, neg=neg: e.tensor_tensor(out=tmp[:], in0=p_bc[:64, :], in1=neg, op=ALU.add), reads=[rpbc, rcst], writes=[rtmp])
                        k.op("act", lambda e: e.activation(out=DT[:], in_=tmp[:], func=AF.Exp, bias=col[:, 0:1]), reads=[rtmp, rcol], writes=[rDT])
                        k.op("act", lambda e: e.activation(out=ebc[:], in_=p_bc[:], func=AF.Exp), reads=[rpbc], writes=[rebc])
                        k.op("dve", lambda e, last=last: e.tensor_copy(out=bl[:], in_=p_bc[:, last:last + 1]), reads=[rpbc], writes=[rbl])
                        if STOP <= 53: continue
                        for dc in range(2):
                            k.op("pe", lambda e, b=b, dc=dc, o=o: e.matmul(p_sc[:], lhsT=kb_[b][:, dc, o:o + L], rhs=qb[b][:, dc, o:o + L], start=(dc == 0), stop=(dc == 1)),
                                 reads=[rk[b], rq[b]], writes=[rpsc])
                        k.op("dve", lambda e: e.tensor_tensor(out=ScT[:], in0=p_sc[:], in1=DT[:], op=ALU.mult), reads=[rpsc, rDT], writes=[rScT])
                        k.op("pool", lambda e, b=b, o=o: e.tensor_tensor(out=qs[:], in0=qb[b][:, :, o:o + L], in1=ebc[:].unsqueeze(1).broadcast_to([128, 2, 64]), op=ALU.mult),
                             reads=[rq[b], rebc], writes=[rqs])
                        if STOP <= 54: continue
                        for m in range(3):
                            lhs_i = vtm[:, m * 128:(m + 1) * 128] if m < 2 else vtm[:, 256:257].broadcast_to([64, 128])
                            k.op("pe", lambda e, m=m, lhs_i=lhs_i: e.matmul(p_nm[:, m, :], lhsT=lhs_i, rhs=ScT[:], start=True, stop=False),
                                 reads=[rvtm, rScT], writes=[rpnm])
                            for dc in range(2):
                                lhs_c = CT[:, dc, m * 128:(m + 1) * 128] if m < 2 else CT[:, dc, 256:257].broadcast_to([128, 128])
                                k.op("pe", lambda e, m=m, dc=dc, lhs_c=lhs_c: e.matmul(p_nm[:, m, :], lhsT=lhs_c, rhs=qs[:, dc, :], start=False, stop=(dc == 1)),
                                     reads=[rCT, rqs], writes=[rpnm])
                        if STOP <= 55: continue
                        k.op("act", lambda e: e.activation(out=rden[:], in_=p_nm[:, 2, :], func=AF.Abs), reads=[rpnm], writes=[rrden])
                        k.op("dve", lambda e: e.tensor_scalar(out=rden[:], in0=rden[:], scalar1=1.0, scalar2=None, op0=ALU.max), reads=[rrden], writes=[rrden])
                        k.op("dve", lambda e: e.reciprocal(out=rden[:], in_=rden[:]), reads=[rrden], writes=[rrden])
                        k.op("dve", lambda e, b=b, o=o: e.tensor_tensor(out=hb[b][:, :, o:o + L], in0=p_nm[:, 0:2, :],
                                                                          in1=rden[:].unsqueeze(1).broadcast_to([128, 2, 64]), op=ALU.mult),
                             reads=[rpnm, rrden], writes=[rh[b]])
                        if STOP <= 56: continue
                        k.op("act", lambda e: e.activation(out=wts[:], in_=col[:], func=AF.Exp, bias=bl[:64, 0:1]), reads=[rcol, rbl], writes=[rwts])
                        k.op("act", lambda e: e.activation(out=ebl[:], in_=bl[:], func=AF.Exp), reads=[rbl], writes=[rebl])
                        k.op("dve", lambda e: e.tensor_scalar(out=vw[:], in0=vtm[:], scalar1=wts[:, 0:1], scalar2=None, op0=ALU.mult), reads=[rvtm, rwts], writes=[rvw])
                        for dc in range(2):
                            k.op("pe", lambda e, dc=dc: e.matmul(p_dc[dc][:], lhsT=ktm[:, dc * 128:(dc + 1) * 128], rhs=vw[:], start=True, stop=True),
                                 reads=[rktm, rvw], writes=[rpdc[dc]])
                            k.op("dve", lambda e, dc=dc: e.scalar_tensor_tensor(out=CT[:, dc, :], in0=CT[:, dc, :], scalar=ebl[:, 0:1], in1=p_dc[dc][:],
                                                                                 op0=ALU.mult, op1=ALU.add), reads=[rCT, rebl, rpdc[dc]], writes=[rCT])
                    hv = HD[d, h * 256:(h + 1) * 256, t0:t0 + tn].rearrange("(dc p) t -> p dc t", p=128)
                    k.dma("sp", hv, hb[b][:, :, :tn], reads=[rh[b]])
        k.barrier()


L = 64; RW_H = 16

def make_rw_consts():
    t = np.arange(64)
    c = np.zeros((64, 2, 384), np.float32)
    for d in range(2):
        before = (t[:, None] < t[None, :]) if d == 0 else (t[:, None] > t[None, :])
        beq = (t[:, None] <= t[None, :]) if d == 0 else (t[:, None] >= t[None, :])
        c[:, d, 0:64] = before; c[:, d, 64:128] = beq; c[:, d, 128:192] = before; c[:, d, 192:256] = beq
        c[:, d, 256:320] = before.T
        c[:, d, 320:384] = beq
    return c

def phase_rwkv_scan(k, R, KK, V, LW, KD, A, YD, rwc, ident, T, name="rws_"):
    NCH = T // L
    order = {0: list(range(NCH)), 1: [3, 2, 1, 0] + list(range(NCH - 1, 3, -1))}
    H16 = RW_H
    with contextlib.ExitStack() as st:
        cst = k.sbuf(name + "c", [64, 2, 384], F32, st); rcst = k.res()
        idt = k.sbuf(name + "id", [64, 64], F32, st); rid = k.res()
        k.dma("sp", cst[:], rwc, writes=[rcst]); k.dma("sp", idt[:], ident[0:64, 0:64], writes=[rid])
        NB = 2
        def tl(nm, shape):
            return [k.sbuf(f"{name}{nm}{i}", shape, F32, st) for i in range(NB)], [k.res() for _ in range(NB)]
        lw_, rlw = tl("lw", [64, H16, L]); kd_, rkd = tl("kd", [64, H16, L]); a_, ra = tl("a", [64, H16, L])
        r_, rr = tl("r", [64, H16, L]); kk_, rkk = tl("kk", [64, H16, L]); v_, rv = tl("v", [64, H16, L])
        KR, rKR = tl("KR", [64, H16, 2, L]); KBt, rKB = tl("KB", [64, H16, 2, L])
        Wt, rW = tl("W", [64, H16, L]); Wi, rWi = tl("Wi", [64, H16, L]); Wp, rWp = tl("Wp", [64, H16, L])
        lwtm, rlwtm = tl("lwtm", [64, H16 * 64]); khtm, rkhtm = tl("khtm", [64, H16 * 64])
        nbtm, rnbtm = tl("nbtm", [64, H16 * 64]); vtm, rvtm = tl("vtm", [64, H16 * 64])
        yb, ryb = tl("yb", [64, H16, L])
        HS = k.sbuf(name + "HS", [64, 2, H16, 64], F32, st); rHS = [[k.res() for _ in range(H16)] for _ in range(2)]
        k.op("pool", lambda e: e.memset(HS[:], 0.0), writes=[rHS[d][h] for d in range(2) for h in range(H16)])
        NH = 2
        GT = [k.sbuf(f"{name}GT{i}", [64, 320], F32, st) for i in range(NH)]; rGT = [k.res() for _ in range(NH)]
        GTr = [k.sbuf(f"{name}GTr{i}", [64, 320], F32, st) for i in range(NH)]; rGTr = [k.res() for _ in range(NH)]
        MN = [k.sbuf(f"{name}MN{i}", [64, 128], F32, st) for i in range(NH)]; rMN = [k.res() for _ in range(NH)]
        MN2 = [k.sbuf(f"{name}MNb{i}", [64, 128], F32, st) for i in range(NH)]; rMN2 = [k.res() for _ in range(NH)]
        Pm = [k.sbuf(f"{name}P{i}", [64, 64], F32, st) for i in range(NH)]; rPm = [k.res() for _ in range(NH)]
        RHS = [k.sbuf(f"{name}RHS{i}", [64, 64], F32, st) for i in range(NH)]; rRHS = [k.res() for _ in range(NH)]
        Ut = [k.sbuf(f"{name}U{i}", [64, 64], F32, st) for i in range(NH)]; rUt = [k.res() for _ in range(NH)]
        Htmp = [k.sbuf(f"{name}Ht{i}", [64, 64], F32, st) for i in range(NH)]; rHt = [k.res() for _ in range(NH)]
        ptA = k.psum(name + "ptA", [64, 512], F32, st); rptA = k.pres()
        ptB = k.psum(name + "ptB", [64, 512], F32, st); rptB = k.pres()
        pG = [k.psum(f"{name}pG{i}", [64, 320], F32, st) for i in range(2)]; rpG = [k.pres() for _ in range(2)]
        pI = [k.psum(f"{name}pI{i}", [64, 192], F32, st) for i in range(2)]; rpI = [k.pres() for _ in range(2)]
        pS = [k.psum(f"{name}pS{i}", [64, 256], F32, st) for i in range(2)]; rpS = [k.pres() for _ in range(2)]
        pt = [ptA, ptB]; rpt = [rptA, rptB]

        def transpose16(src_fn, dst, rsrc, rdst):
            for half in range(2):
                p, rp = pt[half], rpt[half]
                for hh in range(8):
                    h = half * 8 + hh
                    k.op("pe", lambda e, p=p, hh=hh, h=h: e.transpose(out=p[:, hh * 64:(hh + 1) * 64], in_=src_fn(h), identity=idt[:]),
                         reads=[rsrc, rid], writes=[rp])
                k.op(("act", "dve")[half], (lambda e, p=p, half=half: e.copy(out=dst[:, half * 512:(half + 1) * 512], in_=p[:])) if half == 0 else
                     (lambda e, p=p, half=half: e.tensor_copy(out=dst[:, half * 512:(half + 1) * 512], in_=p[:])), reads=[rp], writes=[rdst])

        it = 0
        for s in range(NCH):
            for d in range(2):
                c = order[d][s]; t0 = c * L
                b = it % NB; it += 1
                last = L - 1 if d == 0 else 0
                def ld(q, dst, src, rdst):
                    k.dma(q, dst[:], src.rearrange("(h p) t -> p h t", p=64)[:, :, t0:t0 + L], writes=[rdst])
                ld("sp", lw_[b], LW[d], rlw[b]); ld("act", kd_[b], KD[d], rkd[b]); ld("pool", a_[b], A[d], ra[b])
                ld("sp", r_[b], R, rr[b]); ld("act", kk_[b], KK, rkk[b]); ld("pool", v_[b], V, rv[b])
                transpose16(lambda h, b=b: lw_[b][:, h, :], lwtm[b], rlw[b], rlwtm[b])
                tri = cst[:, d, 320:384]
                for half in range(2):
                    p, rp = pt[half], rpt[half]
                    for hh in range(8):
                        h = half * 8 + hh
                        k.op("pe", lambda e, p=p, hh=hh, h=h, b=b, tri=tri: e.matmul(p[:, hh * 64:(hh + 1) * 64], lhsT=lwtm[b][:, h * 64:(h + 1) * 64], rhs=tri, start=True, stop=True),
                             reads=[rlwtm[b], rcst], writes=[rp])
                    hs = slice(half * 8, half * 8 + 8)
                    pv = p.rearrange("p (h t) -> p h t", t=L)
                    k.op("act", lambda e, pv=pv, hs=hs, b=b: e.activation(out=Wt[b][:, hs, :], in_=pv, func=AF.Exp), reads=[rp], writes=[rW[b]])
                    k.op("act", lambda e, pv=pv, hs=hs, b=b: e.activation(out=Wi[b][:, hs, :], in_=pv, func=AF.Exp, scale=-1.0), reads=[rp], writes=[rWi[b]])
                    k.op("dve", lambda e, pv=pv, hs=hs, b=b: e.tensor_tensor(out=Wp[b][:, hs, :], in0=pv, in1=lw_[b][:, hs, :], op=ALU.subtract), reads=[rp, rlw[b]], writes=[rWp[b]])
                k.op("act", lambda e, b=b: e.activation(out=Wp[b][:], in_=Wp[b][:], func=AF.Exp), reads=[rWp[b]], writes=[rWp[b]])
                k.op("pool", lambda e, b=b: e.tensor_tensor(out=KR[b][:, :, 0, :], in0=kk_[b][:], in1=Wp[b][:], op=ALU.mult), reads=[rkk[b], rWp[b]], writes=[rKR[b]])
                k.op("pool", lambda e, b=b: e.tensor_tensor(out=KR[b][:, :, 1, :], in0=r_[b][:], in1=Wt[b][:], op=ALU.mult), reads=[rr[b], rW[b]], writes=[rKR[b]])
                k.op("dve", lambda e, b=b: e.tensor_tensor(out=KBt[b][:, :, 0, :], in0=kd_[b][:], in1=Wi[b][:], op=ALU.mult), reads=[rkd[b], rWi[b]], writes=[rKB[b]])
                k.op("pool", lambda e, b=b: e.tensor_tensor(out=a_[b][:], in0=a_[b][:], in1=kk_[b][:], op=ALU.mult), reads=[ra[b], rkk[b]], writes=[ra[b]])
                k.op("dve", lambda e, b=b: e.scalar_tensor_tensor(out=KBt[b][:, :, 1, :], in0=a_[b][:], scalar=-1.0, in1=Wi[b][:], op0=ALU.mult, op1=ALU.mult),
                     reads=[ra[b], rWi[b]], writes=[rKB[b]])
                transpose16(lambda h, b=b: KBt[b][:, h, 0, :], khtm[b], rKB[b], rkhtm[b])
                transpose16(lambda h, b=b: KBt[b][:, h, 1, :], nbtm[b], rKB[b], rnbtm[b])
                transpose16(lambda h, b=b: v_[b][:, h, :], vtm[b], rv[b], rvtm[b])
                for h in range(H16):
                    q = h % NH
                    hcs = slice(h * 64, (h + 1) * 64)
                    Hst = HS[:, d, h, :]; rH = rHS[d][h]
                    krf = KR[b][:, h, :, :].rearrange("p a t -> p (a t)")
                    g = h % 2
                    k.op("pe", lambda e, g=g, b=b, h=h, krf=krf: e.matmul(pG[g][:, 0:128], lhsT=KBt[b][:, h, 0, :], rhs=krf, start=True, stop=True),
                         reads=[rKB[b], rKR[b]], writes=[rpG[g]])
                    k.op("pe", lambda e, g=g, b=b, h=h, krf=krf: e.matmul(pG[g][:, 128:256], lhsT=KBt[b][:, h, 1, :], rhs=krf, start=True, stop=True),
                         reads=[rKB[b], rKR[b]], writes=[rpG[g]])
                    k.op("pe", lambda e, g=g, b=b, h=h: e.matmul(pG[g][:, 256:320], lhsT=KR[b][:, h, 0, :], rhs=KBt[b][:, h, 1, :], start=True, stop=True),
                         reads=[rKB[b], rKR[b]], writes=[rpG[g]])
                    k.op("act", lambda e, g=g, q=q: e.copy(out=GTr[q][:], in_=pG[g][:]), reads=[rpG[g]], writes=[rGTr[q]])
                    k.op("pool", lambda e, q=q, d=d: e.tensor_tensor(out=GT[q][:], in0=GTr[q][:], in1=cst[:, d, 0:320], op=ALU.mult), reads=[rGTr[q], rcst], writes=[rGT[q]])
                    k.op("pool", lambda e, q=q: e.tensor_tensor(out=Pm[q][:], in0=GT[q][:, 128:192], in1=idt[:], op=ALU.add), reads=[rGT[q], rid], writes=[rPm[q]])
                    Mc, Nc, rMc = GT[q][:, 128:192], GT[q][:, 256:320], rGT[q]
                    for lev in range(5):
                        dstt, rdst = (MN, rMN) if lev % 2 == 0 else (MN2, rMN2)
                        pi = pI[lev % 2]; rpi = rpI[lev % 2]
                        k.op("pe", lambda e, pi=pi, Mc=Mc, Nc=Nc: e.matmul(pi[:, 0:64], lhsT=Nc, rhs=Mc, start=True, stop=True), reads=[rMc], writes=[rpi])
                        k.op("pe", lambda e, pi=pi, Mc=Mc, Nc=Nc: e.matmul(pi[:, 64:128], lhsT=Mc, rhs=Nc, start=True, stop=True), reads=[rMc], writes=[rpi])
                        k.op("act", lambda e, pi=pi, dstt=dstt, q=q: e.copy(out=dstt[q][:], in_=pi[:, 0:128]), reads=[rpi], writes=[rdst[q]])
                        Mc, Nc, rMc = dstt[q][:, 0:64], dstt[q][:, 64:128], rdst[q]
                        k.op("pe", lambda e, pi=pi, Nc=Nc, q=q: e.matmul(pi[:, 128:192], lhsT=Nc, rhs=Pm[q][:], start=True, stop=True), reads=[rMc, rPm[q]], writes=[rpi])
                        k.op("dve", lambda e, pi=pi, q=q: e.tensor_tensor(out=Pm[q][:], in0=Pm[q][:], in1=pi[:, 128:192], op=ALU.add), reads=[rpi, rPm[q]], writes=[rPm[q]])
                    ps = pS[h % 2]; rps = rpS[h % 2]
                    k.op("pe", lambda e, ps=ps, b=b, h=h, Hst=Hst: e.matmul(ps[:, 0:64], lhsT=KR[b][:, h, 0, :], rhs=Hst, start=True, stop=False), reads=[rKR[b], rH], writes=[rps])
                    k.op("pe", lambda e, ps=ps, b=b, q=q, hcs=hcs: e.matmul(ps[:, 0:64], lhsT=GT[q][:, 0:64], rhs=vtm[b][:, hcs], start=False, stop=True), reads=[rGT[q], rvtm[b]], writes=[rps])
                    k.op("act", lambda e, ps=ps, q=q: e.copy(out=RHS[q][:], in_=ps[:, 0:64]), reads=[rps], writes=[rRHS[q]])
                    k.op("pe", lambda e, ps=ps, q=q: e.matmul(ps[:, 64:128], lhsT=Pm[q][:], rhs=RHS[q][:], start=True, stop=True), reads=[rPm[q], rRHS[q]], writes=[rps])
                    k.op("dve", lambda e, ps=ps, q=q: e.tensor_copy(out=Ut[q][:], in_=ps[:, 64:128]), reads=[rps], writes=[rUt[q]])
                    k.op("pe", lambda e, ps=ps, b=b, h=h, Hst=Hst: e.matmul(ps[:, 128:192], lhsT=Hst, rhs=KR[b][:, h, 1, :], start=True, stop=False), reads=[rH, rKR[b]], writes=[rps])
                    k.op("pe", lambda e, ps=ps, b=b, q=q, hcs=hcs: e.matmul(ps[:, 128:192], lhsT=vtm[b][:, hcs], rhs=GT[q][:, 64:128], start=False, stop=False), reads=[rvtm[b], rGT[q]], writes=[rps])
                    k.op("pe", lambda e, ps=ps, q=q: e.matmul(ps[:, 128:192], lhsT=Ut[q][:], rhs=GT[q][:, 192:256], start=False, stop=True), reads=[rUt[q], rGT[q]], writes=[rps])
                    k.op("act", lambda e, ps=ps, b=b, h=h: e.copy(out=yb[b][:, h, :], in_=ps[:, 128:192]), reads=[rps], writes=[ryb[b]])
                    k.op("pe", lambda e, ps=ps, b=b, hcs=hcs: e.matmul(ps[:, 192:256], lhsT=khtm[b][:, hcs], rhs=vtm[b][:, hcs], start=True, stop=False), reads=[rkhtm[b], rvtm[b]], writes=[rps])
                    k.op("pe", lambda e, ps=ps, b=b, q=q, hcs=hcs: e.matmul(ps[:, 192:256], lhsT=nbtm[b][:, hcs], rhs=Ut[q][:], start=False, stop=True), reads=[rnbtm[b], rUt[q]], writes=[rps])
                    k.op("dve", lambda e, ps=ps, q=q, Hst=Hst: e.tensor_tensor(out=Htmp[q][:], in0=Hst, in1=ps[:, 192:256], op=ALU.add), reads=[rps, rH], writes=[rHt[q]])
                    k.op("pool", lambda e, q=q, Hst=Hst, b=b, h=h, last=last: e.tensor_scalar(out=Hst, in0=Htmp[q][:], scalar1=Wt[b][:, h, last:last + 1], scalar2=None, op0=ALU.mult),
                         reads=[rHt[q], rW[b]], writes=[rH])
                k.dma("sp", YD[d].rearrange("(h p) t -> p h t", p=64)[:, :, t0:t0 + L], yb[b][:], reads=[ryb[b]])
        k.barrier()


CTXL = 256
RW0 = 4112

def _blocks(T, TB=512):
    return [(0, CTXL)] + [(t, min(TB, T - t)) for t in range(CTXL, T, TB)]

def head_ln(k, x, rx, n, nch, onesb, rones, eps_ap, reps, psm, rpsm, pse, rpse, sq, rsq, mean, rmean, rstd, rrstd, groups):
    k.op("act", lambda e: e.activation(out=sq[:, :nch, :n], in_=x[:, :nch, :n], func=AF.Square), reads=[rx], writes=[rsq])
    for grp in groups:
        for i, ch in enumerate(grp):
            k.op("pe", lambda e, ch=ch, i=i, grp=grp: e.matmul(psm[:, :n], lhsT=onesb[:], rhs=x[:, ch, :n], start=(i == 0), stop=(i == len(grp) - 1)),
                 reads=[rx, rones], writes=[rpsm])
        for i, ch in enumerate(grp):
            k.op("pe", lambda e, ch=ch, i=i, grp=grp: e.matmul(pse[:, :n], lhsT=onesb[:], rhs=sq[:, ch, :n], start=(i == 0), stop=(i == len(grp) - 1)),
                 reads=[rsq, rones], writes=[rpse])
        k.op("dve", lambda e: e.tensor_copy(out=mean[:, :n], in_=psm[:, :n]), reads=[rpsm], writes=[rmean])
        k.op("dve", lambda e: e.tensor_tensor(out=rstd[:, :n], in0=mean[:, :n], in1=mean[:, :n], op=ALU.mult), reads=[rmean], writes=[rrstd])
        k.op("dve", lambda e: e.tensor_tensor(out=rstd[:, :n], in0=pse[:, :n], in1=rstd[:, :n], op=ALU.subtract), reads=[rpse, rrstd], writes=[rrstd])
        k.op("act", lambda e: e.activation(out=rstd[:, :n], in_=rstd[:, :n], func=AF.Sqrt, bias=eps_ap), reads=[rrstd, reps], writes=[rrstd])
        k.op("dve", lambda e: e.reciprocal(out=rstd[:, :n], in_=rstd[:, :n]), reads=[rrstd], writes=[rrstd])
        for ch in grp:
            k.op("dve", lambda e, ch=ch: e.tensor_tensor(out=x[:, ch, :n], in0=x[:, ch, :n], in1=mean[:, :n], op=ALU.subtract), reads=[rx, rmean], writes=[rx])
            k.op("pool", lambda e, ch=ch: e.tensor_tensor(out=x[:, ch, :n], in0=x[:, ch, :n], in1=rstd[:, :n], op=ALU.mult), reads=[rx, rrstd], writes=[rx])

def phase_ml_finish(k, HD, PT, YML, ng_pc, nb_pc, T, name="mlf_"):
    with contextlib.ExitStack() as st:
        TB = 512
        onesb = k.sbuf(name + "ones", [128, 128], F32, st); rones = k.res()
        k.op("pool", lambda e: e.memset(onesb[:], 1.0 / 256.0), writes=[rones])
        eps = k.sbuf(name + "eps", [128, 1], F32, st); reps = k.res()
        k.op("pool", lambda e: e.memset(eps[:], 1e-6), writes=[reps])
        ng = k.sbuf(name + "ng", [128, 8], F32, st); nb = k.sbuf(name + "nb", [128, 8], F32, st); rpar = k.res()
        k.dma("sp", ng[:], ng_pc, writes=[rpar]); k.dma("sp", nb[:], nb_pc, writes=[rpar])
        x = k.sbuf(name + "x", [128, 8, TB], F32, st); rx = k.res()
        x2 = k.sbuf(name + "x2", [128, 8, TB], F32, st); rx2 = k.res()
        o = k.sbuf(name + "o", [128, 8, TB], F32, st); ro = k.res()
        sq = k.sbuf(name + "sq", [128, 8, TB], F32, st); rsq = k.res()
        mean = k.sbuf(name + "mean", [128, TB], F32, st); rmean = k.res()
        rstd = k.sbuf(name + "rstd", [128, TB], F32, st); rrstd = k.res()
        psm = k.psum(name + "psm", [128, TB], F32, st); rpsm = k.pres()
        pse = k.psum(name + "pse", [128, TB], F32, st); rpse = k.pres()
        for (t0, n) in _blocks(T):
            k.dma("sp", x[:, :, :n], HD[0, :, t0:t0 + n].rearrange("(c p) t -> p c t", p=128), writes=[rx])
            k.dma("act", x2[:, :, :n], HD[1, :, t0:t0 + n].rearrange("(c p) t -> p c t", p=128), writes=[rx2])
            k.dma("pool", o[:, :, :n], PT[3072:4096, t0:t0 + n].rearrange("(c p) t -> p c t", p=128), writes=[ro])
            k.op("dve", lambda e, n=n: e.tensor_tensor(out=x[:, :, :n], in0=x[:, :, :n], in1=x2[:, :, :n], op=ALU.add), reads=[rx2], writes=[rx])
            head_ln(k, x, rx, n, 8, onesb, rones, eps[:, 0:1], reps, psm, rpsm, pse, rpse, sq, rsq, mean, rmean, rstd, rrstd,
                    [[0, 1], [2, 3], [4, 5], [6, 7]])
            k.op("act", lambda e, n=n: e.activation(out=o[:, :, :n], in_=o[:, :, :n], func=AF.Sigmoid), reads=[ro], writes=[ro])
            for ch in range(8):
                k.op("dve", lambda e, ch=ch, n=n: e.tensor_scalar(out=x[:, ch, :n], in0=x[:, ch, :n], scalar1=ng[:, ch:ch + 1], scalar2=nb[:, ch:ch + 1],
                                                               op0=ALU.mult, op1=ALU.add), reads=[rx, rpar], writes=[rx])
            k.op("pool", lambda e, n=n: e.tensor_tensor(out=x[:, :, :n], in0=x[:, :, :n], in1=o[:, :, :n], op=ALU.mult), reads=[rx, ro], writes=[rx])
            k.dma("sp", YML[:, t0:t0 + n].rearrange("(c p) t -> p c t", p=128), x[:, :, :n], reads=[rx])
        k.barrier()

def phase_rw_prep(k, PT, outs, prm, T, name="rwp_"):
    with contextlib.ExitStack() as st:
        TB = 512
        def ld(nm, shape, src):
            t = k.sbuf(name + nm, shape, F32, st); r = k.res(); k.dma("sp", t[:], src, writes=[r]); return t, r
        mu, rmu = ld("mu", [128, 27], prm["mu_pc"]); w0, rw0 = ld("w0", [128, 2, 8], prm["w0_pc"]); a0, ra0 = ld("a0", [128, 2, 8], prm["a0_pc"])
        wup, rwup = ld("wup", [128, 1024], prm["w_up"]); aup, raup = ld("aup", [128, 1024], prm["a_up"]); gup, rgup = ld("gup", [128, 1024], prm["g_up"])
        kkp, rkkp = ld("kkp", [128, 8], prm["kk_pc"]); kap, rkap = ld("kap", [128, 8], prm["ka_pc"]); rkp, rrkp = ld("rkp", [128, 8], prm["rk_pc"])
        mu1 = k.sbuf(name + "mu1", [128, 27], F32, st); muh = k.sbuf(name + "muh", [128, 27], F32, st); rmu2 = k.res()
        ka1 = k.sbuf(name + "ka1", [128, 8], F32, st); rka1 = k.res()
        k.op("dve", lambda e: e.tensor_scalar(out=mu1[:], in0=mu[:], scalar1=-1.0, scalar2=1.0, op0=ALU.mult, op1=ALU.add), reads=[rmu], writes=[rmu2])
        k.op("dve", lambda e: e.tensor_scalar(out=muh[:], in0=mu[:], scalar1=0.5, scalar2=None, op0=ALU.mult), reads=[rmu], writes=[rmu2])
        k.op("dve", lambda e: e.tensor_scalar(out=ka1[:], in0=kap[:], scalar1=-1.0, scalar2=1.0, op0=ALU.mult, op1=ALU.add), reads=[rkap], writes=[rka1])
        bones = k.sbuf(name + "bones", [128, 128], F32, st); rbones = k.res()
        k.op("pool", lambda e: e.memset(bones[:], 0.0), writes=[rbones])
        k.op("pool", lambda e: e.memset(bones[0:64, 0:64], 1.0), writes=[rbones])
        k.op("pool", lambda e: e.memset(bones[64:128, 64:128], 1.0), writes=[rbones])
        tiny = k.sbuf(name + "tiny", [128, 1], F32, st); rtiny = k.res()
        k.op("pool", lambda e: e.memset(tiny[:], 0.0), writes=[rtiny])
        z = [k.sbuf(f"{name}z{i}", [128, TB + 2], F32, st) for i in range(3)]; rz = [k.res() for _ in range(3)]
        s_ = [k.sbuf(f"{name}s{i}", [128, TB], F32, st) for i in range(2)]; rs = [k.res() for _ in range(2)]
        ZS = k.sbuf(name + "ZS", [128, 27, TB], F32, st); rZS = [k.res() for _ in range(27)]
        Ab = k.sbuf(name + "Ab", [128, 2, 8, TB], F32, st); rAb = [[k.res() for _ in range(8)] for _ in range(2)]
        KDb = k.sbuf(name + "KDb", [128, 2, TB], F32, st); rKDb = k.res()
        t1 = [k.sbuf(f"{name}t1{i}", [128, TB], F32, st) for i in range(3)]; rt1 = [k.res() for _ in range(3)]
        t2 = [k.sbuf(f"{name}t2{i}", [128, TB], F32, st) for i in range(3)]; rt2 = [k.res() for _ in range(3)]
        ps = [k.psum(f"{name}ps{i}", [128, TB], F32, st) for i in range(4)]; rps = [k.pres() for _ in range(4)]
        qi = 0; pi = 0; ti = 0
        for (t0, n) in _blocks(T):
            seg_lo, seg_hi = (0, CTXL) if t0 < CTXL else (CTXL, T)
            for c in range(27):
                zz, rzz = z[c % 3], rz[c % 3]
                rows = PT[RW0 + c * 128:RW0 + (c + 1) * 128, :]
                lo = max(seg_lo, t0 - 1); hi = min(seg_hi, t0 + n + 1)
                if lo > t0 - 1:
                    k.op("pool", lambda e, zz=zz: e.memset(zz[:, 0:1], 0.0), writes=[rzz])
                if hi < t0 + n + 1:
                    k.op("pool", lambda e, zz=zz, n=n: e.memset(zz[:, n + 1:n + 2], 0.0), writes=[rzz])
                k.dma(("sp", "act", "pool")[c % 3], zz[:, lo - (t0 - 1):hi - (t0 - 1)], rows[:, lo:hi], writes=[rzz])
                ss, rss = s_[c % 2], rs[c % 2]
                k.op("pool", lambda e, zz=zz, ss=ss, n=n: e.tensor_tensor(out=ss[:, :n], in0=zz[:, 0:n], in1=zz[:, 2:n + 2], op=ALU.add), reads=[rzz], writes=[rss])
                k.op("dve", lambda e, ss=ss, c=c, n=n: e.tensor_scalar(out=ss[:, :n], in0=ss[:, :n], scalar1=muh[:, c:c + 1], scalar2=None, op0=ALU.mult), reads=[rss, rmu2], writes=[rss])
                k.op("dve", lambda e, zz=zz, ss=ss, c=c, n=n: e.scalar_tensor_tensor(out=ZS[:, c, :n], in0=zz[:, 1:n + 1], scalar=mu1[:, c:c + 1], in1=ss[:, :n], op0=ALU.mult, op1=ALU.add),
                     reads=[rzz, rss, rmu2], writes=[rZS[c]])
            k.dma("sp", outs["R"][:, t0:t0 + n].rearrange("(c p) t -> p c t", p=128), ZS[:, 0:8, :n], reads=rZS[0:8])
            k.dma("act", outs["V"][:, t0:t0 + n].rearrange("(c p) t -> p c t", p=128), ZS[:, 16:24, :n], reads=rZS[16:24])
            k.op("act", lambda e, n=n: e.activation(out=ZS[:, 24, :n], in_=ZS[:, 24, :n], func=AF.Tanh), reads=[rZS[24]], writes=[rZS[24]])
            k.op("act", lambda e, n=n: e.activation(out=ZS[:, 26, :n], in_=ZS[:, 26, :n], func=AF.Sigmoid), reads=[rZS[26]], writes=[rZS[26]])
            for d in range(2):
                for j in range(8):
                    p, rp = ps[pi % 4], rps[pi % 4]; pi += 1
                    tt, rtt = t1[ti % 3], rt1[ti % 3]; ti += 1
                    k.op("pe", lambda e, p=p, d=d, j=j, n=n: e.matmul(p[:, :n], lhsT=wup[d * 64:(d + 1) * 64, j * 128:(j + 1) * 128], rhs=ZS[d * 64:(d + 1) * 64, 24, :n], start=True, stop=True),
                         reads=[rwup, rZS[24]], writes=[rp])
                    k.op("act", lambda e, p=p, tt=tt, d=d, j=j, n=n: e.activation(out=tt[:, :n], in_=p[:, :n], func=AF.Sigmoid, bias=w0[:, d, j:j + 1]), reads=[rp, rw0], writes=[rtt])
                    k.op("pool", lambda e, tt=tt, n=n: e.tensor_scalar(out=tt[:, :n], in0=tt[:, :n], scalar1=-0.6065306597126334, scalar2=None, op0=ALU.mult), reads=[rtt], writes=[rtt])
                    k.dma("sp", outs["LW"][d, j * 128:(j + 1) * 128, t0:t0 + n], tt[:, :n], reads=[rtt])
                    p, rp = ps[pi % 4], rps[pi % 4]; pi += 1
                    k.op("pe", lambda e, p=p, d=d, j=j, n=n: e.matmul(p[:, :n], lhsT=aup[d * 64:(d + 1) * 64, j * 128:(j + 1) * 128], rhs=ZS[d * 64:(d + 1) * 64, 25, :n], start=True, stop=True),
                         reads=[raup, rZS[25]], writes=[rp])
                    k.op("act", lambda e, p=p, d=d, j=j, n=n: e.activation(out=Ab[:, d, j, :n], in_=p[:, :n], func=AF.Sigmoid, bias=a0[:, d, j:j + 1]), reads=[rp, ra0], writes=[rAb[d][j]])
                    k.dma("act", outs["A"][d, j * 128:(j + 1) * 128, t0:t0 + n], Ab[:, d, j, :n], reads=[rAb[d][j]])
            for j in range(8):
                p, rp = ps[pi % 4], rps[pi % 4]; pi += 1
                tt, rtt = t1[ti % 3], rt1[ti % 3]; ti += 1
                k.op("pe", lambda e, p=p, j=j, n=n: e.matmul(p[:, :n], lhsT=gup[:, j * 128:(j + 1) * 128], rhs=ZS[:, 26, :n], start=True, stop=True), reads=[rgup, rZS[26]], writes=[rp])
                k.op("act", lambda e, p=p, tt=tt, n=n: e.copy(out=tt[:, :n], in_=p[:, :n]), reads=[rp], writes=[rtt])
                k.dma("pool", outs["G"][j * 128:(j + 1) * 128, t0:t0 + n], tt[:, :n], reads=[rtt])
            for j in range(8):
                kc = 8 + j; rc = j; vc = 16 + j
                tt, rtt = t1[ti % 3], rt1[ti % 3]; tu, rtu = t2[ti % 3], rt2[ti % 3]; ti += 1
                p, rp = ps[pi % 4], rps[pi % 4]; pi += 1
                k.op("act", lambda e, tt=tt, kc=kc, j=j, n=n: e.activation(out=tt[:, :n], in_=ZS[:, kc, :n], func=AF.Square, scale=kkp[:, j:j + 1]), reads=[rZS[kc], rkkp], writes=[rtt])
                k.op("pe", lambda e, p=p, tt=tt, n=n: e.matmul(p[:, :n], lhsT=bones[:], rhs=tt[:, :n], start=True, stop=True), reads=[rtt, rbones], writes=[rp])
                k.op("act", lambda e, p=p, tt=tt, n=n: e.activation(out=tt[:, :n], in_=p[:, :n], func=AF.Sqrt, bias=tiny[:, 0:1]), reads=[rp, rtiny], writes=[rtt])
                k.op("dve", lambda e, tt=tt, n=n: e.tensor_scalar(out=tt[:, :n], in0=tt[:, :n], scalar1=1e-12, scalar2=None, op0=ALU.max), reads=[rtt], writes=[rtt])
                k.op("dve", lambda e, tt=tt, n=n: e.reciprocal(out=tt[:, :n], in_=tt[:, :n]), reads=[rtt], writes=[rtt])
                k.op("dve", lambda e, tt=tt, kc=kc, j=j, n=n: e.scalar_tensor_tensor(out=tt[:, :n], in0=ZS[:, kc, :n], scalar=kkp[:, j:j + 1], in1=tt[:, :n], op0=ALU.mult, op1=ALU.mult),
                     reads=[rZS[kc], rkkp, rtt], writes=[rtt])
                k.dma("sp", outs["KK"][j * 128:(j + 1) * 128, t0:t0 + n], tt[:, :n], reads=[rtt])
                for d in range(2):
                    k.op("dve", lambda e, d=d, j=j, n=n: e.tensor_scalar(out=KDb[:, d, :n], in0=Ab[:, d, j, :n], scalar1=kap[:, j:j + 1], scalar2=ka1[:, j:j + 1], op0=ALU.mult, op1=ALU.add),
                         reads=[rAb[d][j], rkap, rka1], writes=[rKDb])
                    k.op("pool", lambda e, d=d, kc=kc, n=n: e.tensor_tensor(out=KDb[:, d, :n], in0=KDb[:, d, :n], in1=ZS[:, kc, :n], op=ALU.mult), reads=[rKDb, rZS[kc]], writes=[rKDb])
                    k.dma(("act", "pool")[d], outs["KD"][d, j * 128:(j + 1) * 128, t0:t0 + n], KDb[:, d, :n], reads=[rKDb])
                p, rp = ps[pi % 4], rps[pi % 4]; pi += 1
                k.op("pool", lambda e, tu=tu, n=n: e.tensor_tensor(out=tu[:, :n], in0=KDb[:, 0, :n], in1=KDb[:, 1, :n], op=ALU.add), reads=[rKDb], writes=[rtu])
                k.op("dve", lambda e, tu=tu, rc=rc, j=j, n=n: e.scalar_tensor_tensor(out=tu[:, :n], in0=ZS[:, rc, :n], scalar=rkp[:, j:j + 1], in1=tu[:, :n], op0=ALU.mult, op1=ALU.mult),
                     reads=[rZS[rc], rrkp, rtu], writes=[rtu])
                k.op("pe", lambda e, p=p, tu=tu, n=n: e.matmul(p[:, :n], lhsT=bones[:], rhs=tu[:, :n], start=True, stop=True), reads=[rtu, rbones], writes=[rp])
                k.op("dve", lambda e, p=p, tu=tu, vc=vc, n=n: e.tensor_tensor(out=tu[:, :n], in0=p[:, :n], in1=ZS[:, vc, :n], op=ALU.mult), reads=[rp, rZS[vc]], writes=[rtu])
                k.dma("sp", outs["BV"][j * 128:(j + 1) * 128, t0:t0 + n], tu[:, :n], reads=[rtu])
        k.barrier()

def phase_rw_finish(k, YD, BV, G, YRW, ng_pc, nb_pc, T, name="rwf_"):
    with contextlib.ExitStack() as st:
        TB = 512
        onesb = k.sbuf(name + "ones", [128, 128], F32, st); rones = k.res()
        k.op("pool", lambda e: e.memset(onesb[:], 0.0), writes=[rones])
        k.op("pool", lambda e: e.memset(onesb[0:64, 0:64], 1.0 / 64.0), writes=[rones])
        k.op("pool", lambda e: e.memset(onesb[64:128, 64:128], 1.0 / 64.0), writes=[rones])
        eps = k.sbuf(name + "eps", [128, 1], F32, st); reps = k.res()
        k.op("pool", lambda e: e.memset(eps[:], 64e-5), writes=[reps])
        ng = k.sbuf(name + "ng", [128, 8], F32, st); nb = k.sbuf(name + "nb", [128, 8], F32, st); rpar = k.res()
        k.dma("sp", ng[:], ng_pc, writes=[rpar]); k.dma("sp", nb[:], nb_pc, writes=[rpar])
        x = k.sbuf(name + "x", [128, 8, TB], F32, st); rx = k.res()
        x2 = k.sbuf(name + "x2", [128, 8, TB], F32, st); rx2 = k.res()
        bv = k.sbuf(name + "bv", [128, 8, TB], F32, st); rbv = k.res()
        g = k.sbuf(name + "g", [128, 8, TB], F32, st); rg = k.res()
        sq = k.sbuf(name + "sq", [128, 8, TB], F32, st); rsq = k.res()
        mean = k.sbuf(name + "mean", [128, TB], F32, st); rmean = k.res()
        rstd = k.sbuf(name + "rstd", [128, TB], F32, st); rrstd = k.res()
        psm = k.psum(name + "psm", [128, TB], F32, st); rpsm = k.pres()
        pse = k.psum(name + "pse", [128, TB], F32, st); rpse = k.pres()
        for (t0, n) in _blocks(T):
            v3 = lambda ap: ap[:, t0:t0 + n].rearrange("(c p) t -> p c t", p=128)
            k.dma("sp", x[:, :, :n], v3(YD[0]), writes=[rx]); k.dma("act", x2[:, :, :n], v3(YD[1]), writes=[rx2])
            k.dma("pool", bv[:, :, :n], v3(BV), writes=[rbv]); k.dma("sp", g[:, :, :n], v3(G), writes=[rg])
            k.op("dve", lambda e, n=n: e.tensor_tensor(out=x[:, :, :n], in0=x[:, :, :n], in1=x2[:, :, :n], op=ALU.add), reads=[rx2], writes=[rx])
            head_ln(k, x, rx, n, 8, onesb, rones, eps[:, 0:1], reps, psm, rpsm, pse, rpse, sq, rsq, mean, rmean, rstd, rrstd, [[c] for c in range(8)])
            for ch in range(8):
                k.op("dve", lambda e, ch=ch, n=n: e.tensor_scalar(out=x[:, ch, :n], in0=x[:, ch, :n], scalar1=ng[:, ch:ch + 1], scalar2=nb[:, ch:ch + 1],
                                                               op0=ALU.mult, op1=ALU.add), reads=[rx, rpar], writes=[rx])
            k.op("pool", lambda e, n=n: e.tensor_tensor(out=x[:, :, :n], in0=x[:, :, :n], in1=bv[:, :, :n], op=ALU.add), reads=[rx, rbv], writes=[rx])
            k.op("dve", lambda e, n=n: e.tensor_tensor(out=x[:, :, :n], in0=x[:, :, :n], in1=g[:, :, :n], op=ALU.mult), reads=[rx, rg], writes=[rx])
            k.dma("sp", v3(YRW), x[:, :, :n], reads=[rx])
        k.barrier()


L = 64; S5G = 64; S5N = 64; S5C = 16; S5_ROW0 = 0
GB = 8

def s5_host_layout(lam_re, lam_im, log_dt, b_re, b_im, c_re, c_im):
    lamN = np.stack([np.transpose(lam_re, (2, 0, 1)), np.transpose(lam_im, (2, 0, 1)),
                     np.broadcast_to(log_dt[None], (64, 2, 64))], 1).astype(np.float32)
    lamC = np.stack([lam_re, lam_im, np.broadcast_to(log_dt[..., None], (2, 64, 64))], 0)
    lamC = np.broadcast_to(lamC[None], (16, 3, 2, 64, 64)).astype(np.float32)
    Bc = np.stack([np.transpose(b_re, (3, 0, 1, 2)), np.transpose(b_im, (3, 0, 1, 2))], 1).astype(np.float32)
    Cn = np.stack([np.transpose(c_re, (3, 0, 1, 2)), np.transpose(c_im, (3, 0, 1, 2))], 1).astype(np.float32)
    return {"lamN": np.ascontiguousarray(lamN), "lamC": np.ascontiguousarray(lamC), "Bc": np.ascontiguousarray(Bc), "Cn": np.ascontiguousarray(Cn)}

def make_s5_consts():
    c = np.zeros((64, 2, 512), np.float32)
    c[:, 0, :64] = np.arange(1, 65, dtype=np.float32)[None]
    rst = np.ones((GB, 64), np.float32); rst[:, 0] = 0.0
    c[:, 1, :] = rst.reshape(-1)[None]
    return c

def cmul(k, eng2, outr, outi, ar, ai, br, bi, tmp, rds, wr, rtmp):
    e1, e2 = eng2
    k.op(e1, lambda e: e.tensor_tensor(out=outr, in0=ar, in1=br, op=ALU.mult), reads=rds, writes=[wr])
    k.op(e2, lambda e: e.tensor_tensor(out=tmp, in0=ai, in1=bi, op=ALU.mult), reads=rds, writes=[rtmp])
    k.op(e1, lambda e: e.tensor_tensor(out=outr, in0=outr, in1=tmp, op=ALU.subtract), reads=[wr, rtmp], writes=[wr])
    k.op(e2, lambda e: e.tensor_tensor(out=outi, in0=ar, in1=bi, op=ALU.mult), reads=rds, writes=[wr])
    k.op(e1, lambda e: e.tensor_tensor(out=tmp, in0=ai, in1=br, op=ALU.mult), reads=rds + [wr], writes=[rtmp])
    k.op(e2, lambda e: e.tensor_tensor(out=outi, in0=outi, in1=tmp, op=ALU.add), reads=[wr, rtmp], writes=[wr])

def phase_s5(k, PT, YSD, lamN_d, lamC_d, Bc_d, Cn_d, s5c_d, T, name="s5_", dbg=None):
    NCH = T // L
    order = {0: list(range(NCH)), 1: [3, 2, 1, 0] + list(range(NCH - 1, 3, -1))}
    with contextlib.ExitStack() as st:
        def sb(nm, shape): return k.sbuf(name + nm, shape, F32, st)
        cst = sb("cst", [64, 2, 512]); rcst = k.res(); k.dma("sp", cst[:], s5c_d, writes=[rcst])
        lamN = sb("lamN", [64, 3, 2, 64]); rlamN = k.res(); k.dma("sp", lamN[:], lamN_d, writes=[rlamN])
        Cn = sb("Cn", [64, 2, 2, 64, 16]); rCn = k.res(); k.dma("act", Cn[:], Cn_d, writes=[rCn])
        halfpi = sb("hpi", [64, 1]); rhp = k.res(); k.op("pool", lambda e: e.memset(halfpi[:], math.pi / 2), writes=[rhp])
        k.op("pool", lambda e: e.tensor_scalar(out=Cn[:, 1], in0=Cn[:, 1], scalar1=-1.0, scalar2=None, op0=ALU.mult), reads=[rCn], writes=[rCn])
        dtN = sb("dtN", [64, 2, 64]); aN = sb("aN", [64, 2, 64]); thN = sb("thN", [64, 2, 64]); rN = k.res()
        c1 = sb("c1", [64, 2, 64]); s1 = sb("s1", [64, 2, 64]); tN = sb("tN", [64, 2, 64]); tN2 = sb("tN2", [64, 2, 64]); rcs = k.res(); rtN = k.res()
        k.op("act", lambda e: e.activation(out=dtN[:], in_=lamN[:, 2], func=AF.Exp), reads=[rlamN], writes=[rN])
        k.op("dve", lambda e: e.tensor_tensor(out=aN[:], in0=lamN[:, 0], in1=dtN[:], op=ALU.mult), reads=[rlamN, rN], writes=[rN])
        k.op("dve", lambda e: e.tensor_tensor(out=thN[:], in0=lamN[:, 1], in1=dtN[:], op=ALU.mult), reads=[rlamN, rN], writes=[rN])
        def unit_phasor(th, cc, ss, t1_, t2_, shape_p, rth, rout, rt, hp):
            k.op("act", lambda e: e.activation(out=ss, in_=th, func=AF.Sin, scale=1.0 / 16.0), reads=[rth], writes=[rout])
            k.op("act", lambda e: e.activation(out=cc, in_=th, func=AF.Sin, scale=1.0 / 16.0, bias=hp), reads=[rth, rhp], writes=[rout])
            for _ in range(4):
                k.op("dve", lambda e: e.tensor_tensor(out=t1_, in0=cc, in1=cc, op=ALU.mult), reads=[rout], writes=[rt])
                k.op("dve", lambda e: e.tensor_tensor(out=t2_, in0=ss, in1=ss, op=ALU.mult), reads=[rout], writes=[rt])
                k.op("dve", lambda e: e.scalar_tensor_tensor(out=ss, in0=ss, scalar=2.0, in1=cc, op0=ALU.mult, op1=ALU.mult), reads=[rout], writes=[rout])
                k.op("dve", lambda e: e.tensor_tensor(out=cc, in0=t1_, in1=t2_, op=ALU.subtract), reads=[rt], writes=[rout])
        unit_phasor(thN[:], c1[:], s1[:], tN[:], tN2[:], None, rN, rcs, rtN, halfpi[:, 0:1])
        Bbr = sb("Bbr", [16, 2, 64, 64]); Bbi = sb("Bbi", [16, 2, 64, 64]); rBb = k.res()
        with contextlib.ExitStack() as st2:
            def s2(nm): return k.sbuf(name + "b_" + nm, [16, 16, 64], F32, st2)
            lre, lim, ldt, br_, bi_ = s2("lre"), s2("lim"), s2("ldt"), s2("br"), s2("bi")
            ea, cc, ss, u1, u2, fr, fi = s2("ea"), s2("cc"), s2("ss"), s2("u1"), s2("u2"), s2("fr"), s2("fi")
            rl = k.res(); rw = k.res(); rt = k.res(); rf = k.res()
            def bbar_block(d, qq):
                qs = slice(qq * 16, (qq + 1) * 16)
                k.dma("sp", lre[:], lamC_d[:, 0, d, qs], writes=[rl]); k.dma("act", lim[:], lamC_d[:, 1, d, qs], writes=[rl]); k.dma("pool", ldt[:], lamC_d[:, 2, d, qs], writes=[rl])
                k.dma("sp", br_[:], Bc_d[:, 0, d, qs], writes=[rl]); k.dma("act", bi_[:], Bc_d[:, 1, d, qs], writes=[rl])
                k.op("act", lambda e: e.activation(out=ldt[:], in_=ldt[:], func=AF.Exp), reads=[rl], writes=[rl])
                k.op("dve", lambda e: e.tensor_tensor(out=u1[:], in0=lre[:], in1=ldt[:], op=ALU.mult), reads=[rl], writes=[rw])
                k.op("dve", lambda e: e.tensor_tensor(out=u2[:], in0=lim[:], in1=ldt[:], op=ALU.mult), reads=[rl], writes=[rw])
                k.op("act", lambda e: e.activation(out=ea[:], in_=u1[:], func=AF.Exp), reads=[rw], writes=[rw])
                unit_phasor(u2[:], cc[:], ss[:], fr[:], fi[:], None, rw, rw, rf, halfpi[0:16, 0:1])
                k.op("dve", lambda e: e.tensor_tensor(out=cc[:], in0=cc[:], in1=ea[:], op=ALU.mult), reads=[rw], writes=[rw])
                k.op("dve", lambda e: e.tensor_scalar(out=cc[:], in0=cc[:], scalar1=-1.0, scalar2=None, op0=ALU.add), reads=[rw], writes=[rw])
                k.op("dve", lambda e: e.tensor_tensor(out=ss[:], in0=ss[:], in1=ea[:], op=ALU.mult), reads=[rw], writes=[rw])
                k.op("dve", lambda e: e.tensor_tensor(out=u1[:], in0=lre[:], in1=lre[:], op=ALU.mult), reads=[rl, rw], writes=[rw])
                k.op("dve", lambda e: e.tensor_tensor(out=u2[:], in0=lim[:], in1=lim[:], op=ALU.mult), reads=[rl, rw], writes=[rw])
                k.op("dve", lambda e: e.tensor_tensor(out=ea[:], in0=u1[:], in1=u2[:], op=ALU.add), reads=[rw], writes=[rw])
                k.op("dve", lambda e: e.reciprocal(out=ea[:], in_=ea[:]), reads=[rw], writes=[rw])
                k.op("dve", lambda e: e.tensor_tensor(out=fr[:], in0=cc[:], in1=lre[:], op=ALU.mult), reads=[rw, rl], writes=[rf])
                k.op("dve", lambda e: e.tensor_tensor(out=u1[:], in0=ss[:], in1=lim[:], op=ALU.mult), reads=[rw, rl], writes=[rw])
                k.op("dve", lambda e: e.tensor_tensor(out=fr[:], in0=fr[:], in1=u1[:], op=ALU.add), reads=[rw, rf], writes=[rf])
                k.op("dve", lambda e: e.tensor_tensor(out=fr[:], in0=fr[:], in1=ea[:], op=ALU.mult), reads=[rw, rf], writes=[rf])
                k.op("dve", lambda e: e.tensor_tensor(out=fi[:], in0=ss[:], in1=lre[:], op=ALU.mult), reads=[rw, rl], writes=[rf])
                k.op("dve", lambda e: e.tensor_tensor(out=u1[:], in0=cc[:], in1=lim[:], op=ALU.mult), reads=[rw, rl], writes=[rw])
                k.op("dve", lambda e: e.tensor_tensor(out=fi[:], in0=fi[:], in1=u1[:], op=ALU.subtract), reads=[rw, rf], writes=[rf])
                k.op("dve", lambda e: e.tensor_tensor(out=fi[:], in0=fi[:], in1=ea[:], op=ALU.mult), reads=[rw, rf], writes=[rf])
                cmul(k, ("dve", "pool"), Bbr[:, d, qs], Bbi[:, d, qs], fr[:], fi[:], br_[:], bi_[:], u1[:], [rf, rl], rBb, rw)
            for d in range(2):
                for qq in range(4):
                    bbar_block(d, qq)
            k.barrier()
        if dbg is not None:
            k.dma("sp", dbg["c1"], c1[:], reads=[rcs]); k.dma("sp", dbg["s1"], s1[:], reads=[rcs]); k.dma("sp", dbg["aN"], aN[:], reads=[rN])
            k.dma("sp", dbg["Bbr"], Bbr[:], reads=[rBb]); k.dma("sp", dbg["Bbi"], Bbi[:], reads=[rBb])
        u_ = [sb(f"u{i}", [16, GB, L]) for i in range(2)]; ru = [k.res() for _ in range(2)]
        yb = [sb(f"yb{i}", [16, GB, L]) for i in range(2)]; ryb = [k.res() for _ in range(2)]
        shp = [64, GB, L]
        EMp, EMm, CM, SM = sb("EMp", shp), sb("EMm", shp), sb("CM", shp), sb("SM", shp); rtab0 = k.res()
        Pr, Pi, Qr, Qi = sb("Pr", shp), sb("Pi", shp), sb("Qr", shp), sb("Qi", shp); rtab = k.res()
        Zr, Zi, Sr, Si, Xr, Xi, tm = sb("Zr", shp), sb("Zi", shp), sb("Sr", shp), sb("Si", shp), sb("Xr", shp), sb("Xi", shp), sb("tm", shp)
        rZ, rS, rX, rtm = k.res(), k.res(), k.res(), k.res()
        xsr, xsi, l65r, l65i, c0r, c0i, ts = sb("xsr", [64, GB, 1]), sb("xsi", [64, GB, 1]), sb("l65r", [64, GB, 1]), sb("l65i", [64, GB, 1]), sb("c0r", [64, GB, 1]), sb("c0i", [64, GB, 1]), sb("ts", [64, GB, 1])
        rxs, rl65, rc0, rts = k.res(), k.res(), k.res(), k.res()
        pBr = k.psum(name + "pBr", [64, GB, L], F32, st); rpBr = k.pres()
        pBi = k.psum(name + "pBi", [64, GB, L], F32, st); rpBi = k.pres()
        pY = [k.psum(f"{name}pY{i}", [16, GB, L], F32, st) for i in range(2)]; rpY = [k.pres() for _ in range(2)]
        mr = cst[:, 0, 0:64]; rst = cst[:, 1, :].rearrange("p (g t) -> p g t", t=L)
        itc = [0]
        def do_block(d, gb):
                gs = slice(gb * GB, (gb + 1) * GB)
                a_bc = aN[:, d, gs].unsqueeze(2).broadcast_to(shp); mr_bc = mr.unsqueeze(1).broadcast_to(shp)
                k.op("dve", lambda e: e.tensor_tensor(out=EMp[:], in0=a_bc, in1=mr_bc, op=ALU.mult), reads=[rN, rcst, rtab], writes=[rtab0])
                k.op("act", lambda e: e.activation(out=EMm[:], in_=EMp[:], func=AF.Exp, scale=-1.0), reads=[rtab0], writes=[rtab0])
                k.op("act", lambda e: e.activation(out=EMp[:], in_=EMp[:], func=AF.Exp), reads=[rtab0], writes=[rtab0])
                k.op("dve", lambda e: e.tensor_copy(out=CM[:, :, 0:1], in_=c1[:, d, gs].unsqueeze(2)), reads=[rcs], writes=[rtab0])
                k.op("dve", lambda e: e.tensor_copy(out=SM[:, :, 0:1], in_=s1[:, d, gs].unsqueeze(2)), reads=[rcs], writes=[rtab0])
                kk_ = 1
                while kk_ < L:
                    ck = CM[:, :, kk_ - 1:kk_].broadcast_to([64, GB, kk_]); sk = SM[:, :, kk_ - 1:kk_].broadcast_to([64, GB, kk_])
                    cmul(k, ("dve", "pool"), CM[:, :, kk_:2 * kk_], SM[:, :, kk_:2 * kk_], CM[:, :, 0:kk_], SM[:, :, 0:kk_], ck, sk, tm[:, :, 0:kk_], [rtab0], rtab0, rtm)
                    kk_ *= 2
                k.op("dve", lambda e: e.tensor_tensor(out=Pr[:], in0=EMm[:], in1=CM[:], op=ALU.mult), reads=[rtab0], writes=[rtab])
                k.op("dve", lambda e: e.scalar_tensor_tensor(out=Pi[:], in0=EMm[:], scalar=-1.0, in1=SM[:], op0=ALU.mult, op1=ALU.mult), reads=[rtab0], writes=[rtab])
                k.op("pool", lambda e: e.tensor_tensor(out=Qr[:], in0=EMp[:], in1=CM[:], op=ALU.mult), reads=[rtab0], writes=[rtab])
                k.op("pool", lambda e: e.tensor_tensor(out=Qi[:], in0=EMp[:], in1=SM[:], op=ALU.mult), reads=[rtab0], writes=[rtab])
                if d == 1:
                    cmul(k, ("dve", "pool"), l65r[:], l65i[:], Qr[:, :, 63:64], Qi[:, :, 63:64], Qr[:, :, 0:1], Qi[:, :, 0:1], ts[:], [rtab], rl65, rts)
                k.op("pool", lambda e: e.memset(xsr[:], 0.0), writes=[rxs]); k.op("pool", lambda e: e.memset(xsi[:], 0.0), writes=[rxs])
                Tin = (Pr, Pi) if d == 0 else (Qr, Qi); Tout = (Qr, Qi) if d == 0 else (Pr, Pi)
                last = L - 1 if d == 0 else 0
                for c in order[d]:
                    do_chunk(d, gb, c, Tin, Tout, last)
        def do_chunk(d, gb, c, Tin, Tout, last):
                    b = itc[0] % 2; itc[0] += 1
                    t0 = c * L
                    k.dma("sp", u_[b][:], PT[S5_ROW0 + gb * GB * 16:S5_ROW0 + (gb + 1) * GB * 16, t0:t0 + L].rearrange("(g c) t -> c g t", c=16), writes=[ru[b]])
                    for g in range(GB):
                        k.op("pe", lambda e, g=g, b=b: e.matmul(pBr[:, g, :], lhsT=Bbr[:, d, gb * GB + g, :], rhs=u_[b][:, g, :], start=True, stop=True), reads=[rBb, ru[b]], writes=[rpBr])
                        k.op("pe", lambda e, g=g, b=b: e.matmul(pBi[:, g, :], lhsT=Bbi[:, d, gb * GB + g, :], rhs=u_[b][:, g, :], start=True, stop=True), reads=[rBb, ru[b]], writes=[rpBi])
                    k.op("dve", lambda e: e.tensor_tensor(out=Zr[:], in0=pBr[:], in1=Tin[0][:], op=ALU.mult), reads=[rpBr, rtab], writes=[rZ])
                    k.op("dve", lambda e: e.tensor_tensor(out=tm[:], in0=pBi[:], in1=Tin[1][:], op=ALU.mult), reads=[rpBi, rtab], writes=[rtm])
                    k.op("pool", lambda e: e.tensor_tensor(out=Zr[:], in0=Zr[:], in1=tm[:], op=ALU.subtract), reads=[rtm, rZ], writes=[rZ])
                    k.op("dve", lambda e: e.tensor_tensor(out=Zi[:], in0=pBr[:], in1=Tin[1][:], op=ALU.mult), reads=[rpBr, rtab], writes=[rZ])
                    k.op("dve", lambda e: e.tensor_tensor(out=tm[:], in0=pBi[:], in1=Tin[0][:], op=ALU.mult), reads=[rpBi, rtab, rZ], writes=[rtm])
                    k.op("pool", lambda e: e.tensor_tensor(out=Zi[:], in0=Zi[:], in1=tm[:], op=ALU.add), reads=[rtm, rZ], writes=[rZ])
                    fl = lambda ap: ap.rearrange("p g t -> p (g t)")
                    k.op("dve", lambda e: e.tensor_tensor_scan(out=fl(Sr[:]), data0=fl(rst), data1=fl(Zr[:]), initial=0.0, op0=ALU.mult, op1=ALU.add), reads=[rZ, rcst], writes=[rS])
                    k.op("dve", lambda e: e.tensor_tensor_scan(out=fl(Si[:]), data0=fl(rst), data1=fl(Zi[:]), initial=0.0, op0=ALU.mult, op1=ALU.add), reads=[rZ, rcst], writes=[rS])
                    if d == 0:
                        k.op("pool", lambda e: e.tensor_tensor(out=Sr[:], in0=Sr[:], in1=xsr[:].broadcast_to(shp), op=ALU.add), reads=[rS, rxs], writes=[rS])
                        k.op("pool", lambda e: e.tensor_tensor(out=Si[:], in0=Si[:], in1=xsi[:].broadcast_to(shp), op=ALU.add), reads=[rS, rxs], writes=[rS])
                    else:
                        cmul(k, ("dve", "pool"), c0r[:], c0i[:], l65r[:], l65i[:], xsr[:], xsi[:], ts[:], [rl65, rxs], rc0, rts)
                        k.op("dve", lambda e: e.tensor_tensor(out=c0r[:], in0=c0r[:], in1=Sr[:, :, 63:64], op=ALU.add), reads=[rS, rc0], writes=[rc0])
                        k.op("dve", lambda e: e.tensor_tensor(out=c0i[:], in0=c0i[:], in1=Si[:, :, 63:64], op=ALU.add), reads=[rS, rc0], writes=[rc0])
                        k.op("pool", lambda e: e.tensor_tensor(out=Sr[:], in0=Zr[:], in1=Sr[:], op=ALU.subtract), reads=[rS, rZ, rc0], writes=[rS])
                        k.op("pool", lambda e: e.tensor_tensor(out=Si[:], in0=Zi[:], in1=Si[:], op=ALU.subtract), reads=[rS, rZ, rc0], writes=[rS])
                        k.op("pool", lambda e: e.tensor_tensor(out=Sr[:], in0=Sr[:], in1=c0r[:].broadcast_to(shp), op=ALU.add), reads=[rS, rc0], writes=[rS])
                        k.op("pool", lambda e: e.tensor_tensor(out=Si[:], in0=Si[:], in1=c0i[:].broadcast_to(shp), op=ALU.add), reads=[rS, rc0], writes=[rS])
                    cmul(k, ("dve", "pool"), Xr[:], Xi[:], Sr[:], Si[:], Tout[0][:], Tout[1][:], tm[:], [rS, rtab], rX, rtm)
                    if dbg is not None and d == 0 and gb == 0 and c == 0:
                        for nm, tl_, rr_ in (("Zi", CM, rtab0), ("Si", EMp, rtab0), ("Pr", Pr, rtab), ("Pi", Pi, rtab), ("Qr", Qr, rtab), ("Qi", Qi, rtab), ("Zr", Zr, rZ), ("Sr", Sr, rS), ("Xr", Xr, rX), ("Xi", Xi, rX)):
                            k.dma("sp", dbg[nm], tl_[:], reads=[rr_])
                    py, rpy = pY[b], rpY[b]
                    for g in range(GB):
                        k.op("pe", lambda e, g=g, py=py: e.matmul(py[:, g, :], lhsT=Cn[:, 0, d, gb * GB + g, :], rhs=Xr[:, g, :], start=True, stop=False), reads=[rCn, rX], writes=[rpy])
                        k.op("pe", lambda e, g=g, py=py: e.matmul(py[:, g, :], lhsT=Cn[:, 1, d, gb * GB + g, :], rhs=Xi[:, g, :], start=False, stop=True), reads=[rCn, rX], writes=[rpy])
                    k.op("act", lambda e, py=py, b=b: e.copy(out=yb[b][:], in_=py[:]), reads=[rpy], writes=[ryb[b]])
                    k.dma("act", YSD[d, gb * GB * 16:(gb + 1) * GB * 16, t0:t0 + L].rearrange("(g c) t -> c g t", c=16), yb[b][:], reads=[ryb[b]])
                    k.op("dve", lambda e: e.tensor_copy(out=xsr[:], in_=Xr[:, :, last:last + 1]), reads=[rX], writes=[rxs])
                    k.op("dve", lambda e: e.tensor_copy(out=xsi[:], in_=Xi[:, :, last:last + 1]), reads=[rX], writes=[rxs])
        for d in range(2):
            for gb in range(S5G // GB):
                do_block(d, gb)
        k.barrier()

def phase_s5_finish(k, PT, YSD, YS5, d_pc, T, name="s5f_"):
    with contextlib.ExitStack() as st:
        TB = 512
        dsk = k.sbuf(name + "d", [128, 8], F32, st); rd = k.res(); k.dma("sp", dsk[:], d_pc, writes=[rd])
        x = k.sbuf(name + "x", [128, 8, TB], F32, st); rx = k.res()
        y0 = k.sbuf(name + "y0", [128, 8, TB], F32, st); ry0 = k.res()
        y1 = k.sbuf(name + "y1", [128, 8, TB], F32, st); ry1 = k.res()
        t = k.sbuf(name + "t", [128, 8, TB], F32, st); rt = k.res()
        blocks = [(t0, min(TB, T - t0)) for t0 in range(0, T, TB)]
        for (t0, n) in blocks:
            v3 = lambda ap: ap[:, t0:t0 + n].rearrange("(c p) t -> p c t", p=128)
            k.dma("sp", x[:, :, :n], v3(PT[S5_ROW0:S5_ROW0 + 1024]), writes=[rx]); k.dma("act", y0[:, :, :n], v3(YSD[0]), writes=[ry0]); k.dma("pool", y1[:, :, :n], v3(YSD[1]), writes=[ry1])
            k.op("pool", lambda e, n=n: e.tensor_tensor(out=y0[:, :, :n], in0=y0[:, :, :n], in1=y1[:, :, :n], op=ALU.add), reads=[ry1], writes=[ry0])
            for ch in range(8):
                k.op("dve", lambda e, ch=ch, n=n: e.scalar_tensor_tensor(out=x[:, ch, :n], in0=x[:, ch, :n], scalar=dsk[:, ch:ch + 1], in1=y0[:, ch, :n], op0=ALU.mult, op1=ALU.add),
                     reads=[rx, ry0, rd], writes=[rx])
            k.op("pool", lambda e, n=n: e.tensor_tensor(out=t[:, :, :n], in0=x[:, :, :n], in1=x[:, :, :n], op=ALU.mult), reads=[rx], writes=[rt])
            k.op("dve", lambda e, n=n: e.tensor_scalar(out=t[:, :, :n], in0=t[:, :, :n], scalar1=0.044715, scalar2=1.0, op0=ALU.mult, op1=ALU.add), reads=[rt], writes=[rt])
            k.op("pool", lambda e, n=n: e.tensor_tensor(out=t[:, :, :n], in0=t[:, :, :n], in1=x[:, :, :n], op=ALU.mult), reads=[rt, rx], writes=[rt])
            k.op("act", lambda e, n=n: e.activation(out=t[:, :, :n], in_=t[:, :, :n], func=AF.Tanh, scale=0.7978845608028654), reads=[rt], writes=[rt])
            k.op("dve", lambda e, n=n: e.tensor_scalar(out=t[:, :, :n], in0=t[:, :, :n], scalar1=0.5, scalar2=0.5, op0=ALU.mult, op1=ALU.add), reads=[rt], writes=[rt])
            k.op("pool", lambda e, n=n: e.tensor_tensor(out=t[:, :, :n], in0=t[:, :, :n], in1=x[:, :, :n], op=ALU.mult), reads=[rt, rx], writes=[rt])
            k.dma("sp", v3(YS5), t[:, :, :n], reads=[rt])
        k.barrier()


KC = 16; D = 2048; GATE_ROW0 = 1024
ALPHA = (2.0 * 2) ** 0.25
LN_EPS = 1e-5

def ln_block(k, x, rx, n, ones, rones, epst, reps, psm, rpsm, pse, rpse, sq, rsq, mean, rmean, rstd, rrstd):
    k.op("act", lambda e: e.activation(out=sq[:, :, :n], in_=x[:, :, :n], func=AF.Square), reads=[rx], writes=[rsq])
    for kc in range(KC):
        k.op("pe", lambda e, kc=kc: e.matmul(psm[:, :n], lhsT=ones[:], rhs=x[:, kc, :n], start=(kc == 0), stop=(kc == KC - 1)), reads=[rx, rones], writes=[rpsm])
    for kc in range(KC):
        k.op("pe", lambda e, kc=kc: e.matmul(pse[:, :n], lhsT=ones[:], rhs=sq[:, kc, :n], start=(kc == 0), stop=(kc == KC - 1)), reads=[rsq, rones], writes=[rpse])
    k.op("dve", lambda e: e.tensor_copy(out=mean[:, :n], in_=psm[:, :n]), reads=[rpsm], writes=[rmean])
    k.op("dve", lambda e: e.tensor_tensor(out=rstd[:, :n], in0=mean[:, :n], in1=mean[:, :n], op=ALU.mult), reads=[rmean], writes=[rrstd])
    k.op("dve", lambda e: e.tensor_tensor(out=rstd[:, :n], in0=pse[:, :n], in1=rstd[:, :n], op=ALU.subtract), reads=[rpse, rrstd], writes=[rrstd])
    k.op("act", lambda e: e.activation(out=rstd[:, :n], in_=rstd[:, :n], func=AF.Sqrt, bias=epst[:, 0:1]), reads=[rrstd, reps], writes=[rrstd])
    k.op("dve", lambda e: e.reciprocal(out=rstd[:, :n], in_=rstd[:, :n]), reads=[rrstd], writes=[rrstd])
    k.op("dve", lambda e: e.tensor_tensor(out=x[:, :, :n], in0=x[:, :, :n], in1=mean[:, :n].unsqueeze(1).broadcast_to([128, KC, n]), op=ALU.subtract), reads=[rx, rmean], writes=[rx])
    k.op("pool", lambda e: e.tensor_tensor(out=x[:, :, :n], in0=x[:, :, :n], in1=rstd[:, :n].unsqueeze(1).broadcast_to([128, KC, n]), op=ALU.mult), reads=[rx, rrstd], writes=[rx])

def affine_block(k, dst, rdst, src, rsrc, n, sc, sh, rpar, sc_idx=None):
    for kc in range(KC):
        k.op("dve", lambda e, kc=kc: e.tensor_scalar(out=dst[:, kc, :n], in0=src[:, kc, :n], scalar1=sc(kc), scalar2=sh(kc), op0=ALU.mult, op1=ALU.add),
             reads=[rsrc] + rpar, writes=[rdst])

class WStream:
    def __init__(self, k, st, name, kcmax, nbuf=3):
        self.k = k; self.n = nbuf; self.i = 0
        self.wf = [k.sbuf(f"{name}wf{i}", [128, kcmax, 128], F32, st) for i in range(nbuf)]; self.rwf = [k.res() for _ in range(nbuf)]
        self.wb = [k.sbuf(f"{name}wb{i}", [128, kcmax, 128], BF16, st) for i in range(nbuf)]; self.rwb = [k.res() for _ in range(nbuf)]
    def get(self, w2d, col0, kcn, ncol=128):
        k = self.k; b = self.i % self.n; it = self.i; self.i += 1
        wf, wb, rwf, rwb = self.wf[b], self.wb[b], self.rwf[b], self.rwb[b]
        src = w2d[col0 // 128]
        k.dma(("sp", "act", "pool")[it % 3], wf[:, :kcn, :ncol], src, writes=[rwf])
        if it % 2 == 0:
            k.op("act", lambda e: e.copy(out=wb[:, :kcn, :ncol], in_=wf[:, :kcn, :ncol]), reads=[rwf], writes=[rwb])
        else:
            k.op("pool", lambda e: e.tensor_copy(out=wb[:, :kcn, :ncol], in_=wf[:, :kcn, :ncol]), reads=[rwf], writes=[rwb])
        return wb, rwb

def phase_merge(k, xin, X1, PT, YML, YRW, YS5, W, mod, rmod, ln_g_pc, ln_b_pc, blocks, name="mg_"):
    with contextlib.ExitStack() as st:
        TB = 512
        def sb(nm, shape, dt=F32): return k.sbuf(name + nm, shape, dt, st)
        ones = sb("ones", [128, 128]); rones = k.res(); k.op("pool", lambda e: e.memset(ones[:], 1.0 / D), writes=[rones])
        epst = sb("eps", [128, 1]); reps = k.res(); k.op("pool", lambda e: e.memset(epst[:], LN_EPS), writes=[reps])
        lg = sb("lg", [128, KC]); lb = sb("lb", [128, KC]); rl = k.res()
        k.dma("sp", lg[:], ln_g_pc, writes=[rl]); k.dma("sp", lb[:], ln_b_pc, writes=[rl])
        yf = sb("yf", [128, 8, TB]); ryf = k.res()
        ybf = [sb(f"yb{i}", [128, 8, TB], BF16) for i in range(3)]; rybf = [k.res() for _ in range(3)]
        z = sb("z", [128, KC, TB], BF16); rz = k.res()
        xb = sb("x", [128, KC, TB]); rxb = k.res()
        pre = sb("pre", [128, KC, TB]); rpre = k.res()
        sq, rsq = xb, rxb
        gt = sb("gt", [128, 3, TB]); rgt = k.res()
        sgg = sb("sgg", [128, TB]); rsgg = k.res()
        t1 = sb("t1", [128, TB]); rt1 = k.res(); t2 = sb("t2", [128, TB]); rt2 = k.res(); t3 = sb("t3", [128, TB]); rt3 = k.res()
        mean = sb("mean", [128, TB]); rmean = k.res(); rstd = sb("rstd", [128, TB]); rrstd = k.res()
        ws = WStream(k, st, name, KC, nbuf=3)
        pp = [k.psum(f"{name}p{i}", [128, TB], F32, st) for i in range(4)]; rpp = [k.pres() for _ in range(4)]
        psm = k.psum(name + "psm", [128, TB], F32, st); rpsm = k.pres()
        pse = k.psum(name + "pse", [128, TB], F32, st); rpse = k.pres()
        po = [k.psum(f"{name}po{i}", [128, TB], F32, st) for i in range(2)]; rpo = [k.pres() for _ in range(2)]
        def do_block(t0, n, which):
            v8 = lambda ap: ap[:, t0:t0 + n].rearrange("(c p) t -> p c t", p=128)
            for i, src in enumerate((YML, YRW, YS5)):
                k.dma(("sp", "act", "pool")[i], yf[:, :, :n], v8(src), writes=[ryf])
                k.op(("dve", "act", "pool")[i], (lambda e, i=i: e.tensor_copy(out=ybf[i][:, :, :n], in_=yf[:, :, :n])) if i != 1 else
                     (lambda e, i=i: e.copy(out=ybf[i][:, :, :n], in_=yf[:, :, :n])), reads=[ryf], writes=[rybf[i]])
            k.dma("sp", xb[:, :, :n], xin[:, t0:t0 + n].rearrange("(c p) t -> p c t", p=128), writes=[rxb])
            k.op("act", lambda e: e.mul(out=xb[:, :, :n], in_=xb[:, :, :n], mul=ALPHA), reads=[rxb], writes=[rxb])
            def zchunk(j):
                for wi, (wn, src_i) in enumerate((("ml_proj", 0), ("rw_proj", 1), ("s5_w_val", 2), ("s5_w_gate", 2))):
                    wb, rwb = ws.get(W[wn], j * 128, 8)
                    for kc in range(8):
                        k.op("pe", lambda e, kc=kc, wb=wb, wi=wi, src_i=src_i: e.matmul(pp[wi][:, :n], lhsT=wb[:, kc, :], rhs=ybf[src_i][:, kc, :n], start=(kc == 0), stop=(kc == 7)),
                             reads=[rwb, rybf[src_i]], writes=[rpp[wi]])
                k.dma("sp", gt[:, :, :n], PT[GATE_ROW0:GATE_ROW0 + 3 * D, t0:t0 + n].rearrange("(b c p) t -> p b c t", b=3, p=128)[:, :, j, :], writes=[rgt])
                k.op("act", lambda e: e.activation(out=gt[:, :, :n], in_=gt[:, :, :n], func=AF.Sigmoid), reads=[rgt], writes=[rgt])
                k.op("act", lambda e: e.activation(out=sgg[:, :n], in_=pp[3][:, :n], func=AF.Sigmoid), reads=[rpp[3]], writes=[rsgg])
                k.op("dve", lambda e: e.tensor_tensor(out=t1[:, :n], in0=pp[2][:, :n], in1=sgg[:, :n], op=ALU.mult), reads=[rpp[2], rsgg], writes=[rt1])
                k.op("pool", lambda e: e.tensor_tensor(out=t1[:, :n], in0=t1[:, :n], in1=gt[:, 2, :n], op=ALU.mult), reads=[rt1, rgt], writes=[rt1])
                k.op("dve", lambda e: e.tensor_tensor(out=t2[:, :n], in0=pp[0][:, :n], in1=gt[:, 0, :n], op=ALU.mult), reads=[rpp[0], rgt], writes=[rt2])
                k.op("dve", lambda e: e.tensor_tensor(out=t3[:, :n], in0=pp[1][:, :n], in1=gt[:, 1, :n], op=ALU.mult), reads=[rpp[1], rgt], writes=[rt3])
                k.op("pool", lambda e: e.tensor_tensor(out=t2[:, :n], in0=t2[:, :n], in1=t3[:, :n], op=ALU.add), reads=[rt2, rt3], writes=[rt2])
                k.op("pool", lambda e: e.tensor_tensor(out=z[:, j, :n], in0=t2[:, :n], in1=t1[:, :n], op=ALU.add), reads=[rt2, rt1], writes=[rz])
            for j in range(KC):
                zchunk(j)
            def ochunk(j):
                wb, rwb = ws.get(W["w_out"], j * 128, KC)
                p, rp = po[j % 2], rpo[j % 2]
                for kc in range(KC):
                    k.op("pe", lambda e, kc=kc: e.matmul(p[:, :n], lhsT=wb[:, kc, :], rhs=z[:, kc, :n], start=(kc == 0), stop=(kc == KC - 1)), reads=[rwb, rz], writes=[rp])
                k.op("dve", lambda e: e.scalar_tensor_tensor(out=pre[:, j, :n], in0=p[:, :n], scalar=mod[:, 2 * KC + j, which:which + 1], in1=xb[:, j, :n], op0=ALU.mult, op1=ALU.add),
                     reads=[rp, rmod, rxb], writes=[rpre])
            for j in range(KC):
                ochunk(j)
            ln_block(k, pre, rpre, n, ones, rones, epst, reps, psm, rpsm, pse, rpse, sq, rsq, mean, rmean, rstd, rrstd)
            affine_block(k, pre, rpre, pre, rpre, n, lambda kc: lg[:, kc:kc + 1], lambda kc: lb[:, kc:kc + 1], [rl])
            k.dma("sp", X1[:, t0:t0 + n].rearrange("(c p) t -> p c t", p=128), pre[:, :, :n], reads=[rpre])
        for (t0, n, which) in blocks:
            do_block(t0, n, which)
        k.barrier()

def make_sel_const():
    s = np.zeros((16, 16, 128), np.float32)
    for e in range(16):
        s[e, e, :] = 1.0
    return s

def phase_moe(k, X1, out_fn, Wg, Wu, Wd, rw_lay, rb_bc, sel_d, ident_d, mod, rmod, ln_g_pc, ln_b_pc, blocks, name="moe_"):
    with contextlib.ExitStack() as st:
        TB = 512; NE = 16
        def sb(nm, shape, dt=F32): return k.sbuf(name + nm, shape, dt, st)
        ones = sb("ones", [128, 128]); rones = k.res(); k.op("pool", lambda e: e.memset(ones[:], 1.0 / D), writes=[rones])
        epst = sb("eps", [128, 1]); reps = k.res(); k.op("pool", lambda e: e.memset(epst[:], LN_EPS), writes=[reps])
        lg = sb("lg", [128, KC]); lb = sb("lb", [128, KC]); rl = k.res()
        k.dma("sp", lg[:], ln_g_pc, writes=[rl]); k.dma("sp", lb[:], ln_b_pc, writes=[rl])
        rwt = sb("rwt", [128, KC, NE]); rbt = sb("rbt", [128, NE]); selt = sb("sel", [16, NE, 128]); idt = sb("idt", [128, 128]); rcn = k.res()
        k.dma("sp", rwt[:], rw_lay, writes=[rcn]); k.dma("sp", rbt[:], rb_bc, writes=[rcn]); k.dma("sp", selt[:], sel_d, writes=[rcn]); k.dma("sp", idt[:], ident_d, writes=[rcn])
        sc1 = sb("sc1", [128, KC, 2]); rsc1 = k.res()
        k.op("dve", lambda e: e.tensor_scalar(out=sc1[:], in0=mod[:, 4 * KC:5 * KC, :], scalar1=1.0, scalar2=None, op0=ALU.add), reads=[rmod], writes=[rsc1])
        x1 = sb("x1", [128, KC, TB]); rx1 = k.res()
        uf = sb("uf", [128, KC, TB]); ruf = k.res()
        ub = sb("ub", [128, KC, TB], BF16); rub = k.res()
        acc = sb("acc", [128, KC, TB]); racc = k.res()
        hb = sb("h", [128, 8, TB], BF16); rhb = k.res()
        gbt = sb("gb", [128, TB]); rgb = k.res()
        s1 = sb("s1", [128, TB]); rs1 = k.res(); s2 = sb("s2", [128, TB]); rs2 = k.res()
        mean = sb("mean", [128, TB]); rmean = k.res(); rstd = sb("rstd", [128, TB]); rrstd = k.res()
        affT = sb("affT", [16, TB]); raffT = k.res(); gT = sb("gT", [16, TB]); rgT = k.res()
        NBm = TB // 128
        def rt(nm, last): return sb(nm, [128, NBm, last])
        aff = rt("aff", 16); scr = rt("scr", 16); eq = rt("eq", 16); ms = rt("ms", 16); ws_ = rt("ws", 16); rr = k.res()
        m1 = rt("m1", 4); m2 = rt("m2", 4); gs = rt("gs", 4); keep = rt("keep", 4); gmx = rt("gmx", 1); tt1 = rt("tt1", 1); tt2 = rt("tt2", 1); wsum = rt("wsum", 1)
        wst = WStream(k, st, name, KC, nbuf=3)
        psm = k.psum(name + "psm", [128, TB], F32, st); rpsm = k.pres()
        pse = k.psum(name + "pse", [128, TB], F32, st); rpse = k.pres()
        ph1 = k.psum(name + "ph1", [128, TB], F32, st); rph1 = k.pres()
        ph2 = k.psum(name + "ph2", [128, TB], F32, st); rph2 = k.pres()
        po = [k.psum(f"{name}po{i}", [128, TB], F32, st) for i in range(2)]; rpo = [k.pres() for _ in range(2)]
        pgb = k.psum(name + "pgb", [128, TB], F32, st); rpgb = k.pres()
        pms = k.psum(name + "pms", [128, TB], F32, st); rpms = k.pres()
        def do_block(t0, n, which):
            nb = n // 128
            k.dma("sp", x1[:, :, :n], X1[:, t0:t0 + n].rearrange("(c p) t -> p c t", p=128), writes=[rx1])
            k.op("pool", lambda e: e.tensor_copy(out=uf[:, :, :n], in_=x1[:, :, :n]), reads=[rx1], writes=[ruf])
            ln_block(k, uf, ruf, n, ones, rones, epst, reps, psm, rpsm, pse, rpse, acc, racc, mean, rmean, rstd, rrstd)
            affine_block(k, uf, ruf, uf, ruf, n, lambda kc: sc1[:, kc, which:which + 1], lambda kc: mod[:, 3 * KC + kc, which:which + 1], [rsc1, rmod])
            k.op("act", lambda e: e.copy(out=ub[:, :, :n], in_=uf[:, :, :n]), reads=[ruf], writes=[rub])
            for kc in range(KC):
                k.op("pe", lambda e, kc=kc: e.matmul(pms[:16, :n], lhsT=rwt[:, kc, :], rhs=uf[:, kc, :n], start=(kc == 0), stop=(kc == KC - 1)), reads=[rcn, ruf], writes=[rpms])
            k.op("act", lambda e: e.activation(out=affT[:, :n], in_=pms[:16, :n], func=AF.Sigmoid), reads=[rpms], writes=[raffT])
            for tb in range(nb):
                k.op("pe", lambda e, tb=tb: e.transpose(out=pms[:, tb * 16:(tb + 1) * 16], in_=affT[:, tb * 128:(tb + 1) * 128], identity=idt[:16, :16]), reads=[raffT, rcn], writes=[rpms])
            A3 = lambda t_: t_[:, :nb, :]
            k.op("dve", lambda e: e.tensor_copy(out=A3(aff), in_=pms[:, :nb * 16].rearrange("p (b e) -> p b e", e=16)), reads=[rpms], writes=[rr])
            k.op("dve", lambda e: e.tensor_tensor(out=A3(scr), in0=A3(aff), in1=rbt[:].unsqueeze(1).broadcast_to([128, nb, 16]), op=ALU.add), reads=[rr, rcn], writes=[rr])
            g4 = lambda t_: t_[:, :nb, :].rearrange("p b (g e) -> p (b g) e", e=4)
            f4 = lambda t_: t_[:, :nb, :].rearrange("p b g -> p (b g)")
            k.op("dve", lambda e: e.tensor_reduce(out=f4(m1), in_=g4(scr), axis=AX.X, op=ALU.max), reads=[rr], writes=[rr])
            k.op("dve", lambda e: e.tensor_tensor(out=g4(eq), in0=g4(scr), in1=f4(m1).unsqueeze(2).broadcast_to([128, nb * 4, 4]), op=ALU.is_equal), reads=[rr], writes=[rr])
            k.op("dve", lambda e: e.scalar_tensor_tensor(out=A3(ms), in0=A3(eq), scalar=-1e9, in1=A3(scr), op0=ALU.mult, op1=ALU.add), reads=[rr], writes=[rr])
            k.op("dve", lambda e: e.tensor_reduce(out=f4(m2), in_=g4(ms), axis=AX.X, op=ALU.max), reads=[rr], writes=[rr])
            k.op("dve", lambda e: e.tensor_tensor(out=A3(gs), in0=A3(m1), in1=A3(m2), op=ALU.add), reads=[rr], writes=[rr])
            k.op("dve", lambda e: e.tensor_reduce(out=gmx[:, :nb, 0], in_=A3(gs), axis=AX.X, op=ALU.max), reads=[rr], writes=[rr])
            k.op("dve", lambda e: e.tensor_tensor(out=A3(keep), in0=A3(gs), in1=gmx[:, :nb, :].broadcast_to([128, nb, 4]), op=ALU.is_equal), reads=[rr], writes=[rr])
            k.op("dve", lambda e: e.tensor_scalar(out=A3(keep), in0=A3(keep), scalar1=1e9, scalar2=-1e9, op0=ALU.mult, op1=ALU.add), reads=[rr], writes=[rr])
            k.op("dve", lambda e: e.tensor_tensor(out=g4(ms), in0=g4(scr), in1=f4(keep).unsqueeze(2).broadcast_to([128, nb * 4, 4]), op=ALU.add), reads=[rr], writes=[rr])
            k.op("dve", lambda e: e.tensor_reduce(out=tt1[:, :nb, 0], in_=A3(ms), axis=AX.X, op=ALU.max), reads=[rr], writes=[rr])
            k.op("dve", lambda e: e.tensor_tensor(out=A3(eq), in0=A3(ms), in1=tt1[:, :nb, :].broadcast_to([128, nb, 16]), op=ALU.is_equal), reads=[rr], writes=[rr])
            k.op("dve", lambda e: e.scalar_tensor_tensor(out=A3(scr), in0=A3(eq), scalar=-1e9, in1=A3(ms), op0=ALU.mult, op1=ALU.add), reads=[rr], writes=[rr])
            k.op("dve", lambda e: e.tensor_reduce(out=tt2[:, :nb, 0], in_=A3(scr), axis=AX.X, op=ALU.max), reads=[rr], writes=[rr])
            k.op("dve", lambda e: e.tensor_tensor(out=A3(eq), in0=A3(ms), in1=tt2[:, :nb, :].broadcast_to([128, nb, 16]), op=ALU.is_ge), reads=[rr], writes=[rr])
            k.op("dve", lambda e: e.tensor_tensor(out=A3(ws_), in0=A3(aff), in1=A3(eq), op=ALU.mult), reads=[rr], writes=[rr])
            k.op("dve", lambda e: e.tensor_reduce(out=wsum[:, :nb, 0], in_=A3(ws_), axis=AX.X, op=ALU.add), reads=[rr], writes=[rr])
            k.op("dve", lambda e: e.reciprocal(out=wsum[:, :nb, :], in_=wsum[:, :nb, :]), reads=[rr], writes=[rr])
            k.op("dve", lambda e: e.tensor_tensor(out=A3(ws_), in0=A3(ws_), in1=wsum[:, :nb, :].broadcast_to([128, nb, 16]), op=ALU.mult), reads=[rr], writes=[rr])
            for tb in range(nb):
                k.op("pe", lambda e, tb=tb: e.transpose(out=pms[:16, tb * 128:(tb + 1) * 128], in_=ws_[:, tb, :], identity=idt[:]), reads=[rr, rcn], writes=[rpms])
            k.op("act", lambda e: e.copy(out=gT[:, :n], in_=pms[:16, :n]), reads=[rpms], writes=[rgT])
            def expert(e_i):
                k.op("pe", lambda e: e.matmul(pgb[:, :n], lhsT=selt[:, e_i, :], rhs=gT[:, :n], start=True, stop=True), reads=[rcn, rgT], writes=[rpgb])
                k.op("act", lambda e: e.copy(out=gbt[:, :n], in_=pgb[:, :n]), reads=[rpgb], writes=[rgb])
                def hchunk(jc):
                    wb, rwb = wst.get(Wg[e_i], jc * 128, KC)
                    for kc in range(KC):
                        k.op("pe", lambda e, kc=kc: e.matmul(ph1[:, :n], lhsT=wb[:, kc, :], rhs=ub[:, kc, :n], start=(kc == 0), stop=(kc == KC - 1)), reads=[rwb, rub], writes=[rph1])
                    wb2, rwb2 = wst.get(Wu[e_i], jc * 128, KC)
                    for kc in range(KC):
                        k.op("pe", lambda e, kc=kc: e.matmul(ph2[:, :n], lhsT=wb2[:, kc, :], rhs=ub[:, kc, :n], start=(kc == 0), stop=(kc == KC - 1)), reads=[rwb2, rub], writes=[rph2])
                    k.op("act", lambda e: e.activation(out=s1[:, :n], in_=ph1[:, :n], func=AF.Silu), reads=[rph1], writes=[rs1])
                    k.op("dve", lambda e: e.tensor_tensor(out=s2[:, :n], in0=ph2[:, :n], in1=s1[:, :n], op=ALU.mult), reads=[rph2, rs1], writes=[rs2])
                    k.op("pool", lambda e: e.tensor_tensor(out=hb[:, jc, :n], in0=s2[:, :n], in1=gbt[:, :n], op=ALU.mult), reads=[rs2, rgb], writes=[rhb])
                for jc in range(8):
                    hchunk(jc)
                def dchunk(j):
                    wb, rwb = wst.get(Wd[e_i], j * 128, 8)
                    p, rp = po[j % 2], rpo[j % 2]
                    for kc in range(8):
                        k.op("pe", lambda e, kc=kc: e.matmul(p[:, :n], lhsT=wb[:, kc, :], rhs=hb[:, kc, :n], start=(kc == 0), stop=(kc == 7)), reads=[rwb, rhb], writes=[rp])
                    if e_i == 0:
                        k.op("dve", lambda e: e.tensor_copy(out=acc[:, j, :n], in_=p[:, :n]), reads=[rp], writes=[racc])
                    else:
                        k.op("dve", lambda e: e.tensor_tensor(out=acc[:, j, :n], in0=acc[:, j, :n], in1=p[:, :n], op=ALU.add), reads=[rp, racc], writes=[racc])
                for j in range(KC):
                    dchunk(j)
            for e_i in range(NE):
                expert(e_i)
            k.op("act", lambda e: e.mul(out=x1[:, :, :n], in_=x1[:, :, :n], mul=ALPHA), reads=[rx1], writes=[rx1])
            for j in range(KC):
                k.op("dve", lambda e, j=j: e.scalar_tensor_tensor(out=acc[:, j, :n], in0=acc[:, j, :n], scalar=mod[:, 5 * KC + j, which:which + 1], in1=x1[:, j, :n], op0=ALU.mult, op1=ALU.add),
                     reads=[racc, rmod, rx1], writes=[racc])
            ln_block(k, acc, racc, n, ones, rones, epst, reps, psm, rpsm, pse, rpse, uf, ruf, mean, rmean, rstd, rrstd)
            affine_block(k, acc, racc, acc, racc, n, lambda kc: lg[:, kc:kc + 1], lambda kc: lb[:, kc:kc + 1], [rl])
            k.dma("sp", out_fn(t0, n).rearrange("(c p) t -> p c t", p=128), acc[:, :, :n], reads=[racc])
        for (t0, n, which) in blocks:
            do_block(t0, n, which)
        k.barrier()


D = 2048; KC = 16; CTXL = 256; D_IN = 14736

LAYER_INPUTS = [
    ("ada_w", [96, 128, 16, 128]), ("ada_b", [128, 96]), ("w_in", [116, 128, 16, 128]), ("cw", [128, 16, 9]), ("cb", [128, 16]), ("igb", [64, 8]), ("fgb", [64, 8]),
    ("mlng", [128, 8]), ("mlnb", [128, 8]), ("ml_proj", [16, 128, 8, 128]),
    ("mu_pc", [128, 27]), ("w0_pc", [128, 2, 8]), ("a0_pc", [128, 2, 8]), ("w_up", [128, 1024]), ("a_up", [128, 1024]), ("g_up", [128, 1024]),
    ("kk_pc", [128, 8]), ("ka_pc", [128, 8]), ("rk_pc", [128, 8]), ("rwng", [128, 8]), ("rwnb", [128, 8]), ("rw_proj", [16, 128, 8, 128]),
    ("lamN", [64, 3, 2, 64]), ("lamC", [16, 3, 2, 64, 64]), ("Bc", [16, 2, 2, 64, 64]), ("Cn", [64, 2, 2, 64, 16]), ("s5d", [128, 8]),
    ("s5_w_val", [16, 128, 8, 128]), ("s5_w_gate", [16, 128, 8, 128]), ("w_out", [16, 128, 16, 128]),
    ("ln1g", [128, 16]), ("ln1b", [128, 16]), ("ln2g", [128, 16]), ("ln2b", [128, 16]),
    ("exp_wg", [16, 8, 128, 16, 128]), ("exp_wu", [16, 8, 128, 16, 128]), ("exp_wd", [16, 16, 128, 8, 128]),
]
SHARED_INPUTS = [("c2", [128, KC, 2]), ("rw_lay", [128, 16, 16]), ("rb_bc", [128, 16]), ("mlc", [128, 4, 64]), ("ident", [128, 128]),
                 ("rwc", [64, 2, 384]), ("s5c", [64, 2, 512]), ("sel", [16, 16, 128])]

def pc(v):
    return np.ascontiguousarray(np.asarray(v, np.float32).reshape(-1, 128).T)

def blockify(w):
    w = np.asarray(w, np.float32)
    K, N = w.shape
    NJ = (N + 127) // 128
    if NJ * 128 != N:
        w = np.concatenate([w, np.zeros((K, NJ * 128 - N), np.float32)], 1)
    return np.ascontiguousarray(w.reshape(K // 128, 128, NJ, 128).transpose(2, 1, 0, 3))

def host_layer_inputs(inp, l):
    g = lambda n: np.asarray(inp[n][l], np.float32)
    o = {
        "ada_w": blockify(g("ada_w")), "ada_b": pc(g("ada_b")), "w_in": blockify(g("w_in")),
        "cw": np.ascontiguousarray(g("ml_conv_w").reshape(9, 16, 128).transpose(2, 1, 0)), "cb": pc(g("ml_conv_b")),
        "igb": np.ascontiguousarray(np.broadcast_to(g("ml_ig_b").reshape(1, 8), (64, 8))), "fgb": np.ascontiguousarray(np.broadcast_to(g("ml_fg_b").reshape(1, 8), (64, 8))),
        "mlng": pc(g("ml_norm_g")), "mlnb": pc(g("ml_norm_b")), "ml_proj": blockify(g("ml_proj")),
        "mu_pc": pc(g("rw_mu")), "w0_pc": np.ascontiguousarray(g("rw_w0").reshape(2, 8, 128).transpose(2, 0, 1)),
        "a0_pc": np.ascontiguousarray(g("rw_a0").reshape(2, 8, 128).transpose(2, 0, 1)),
        "w_up": np.ascontiguousarray(g("rw_w_up").reshape(128, 1024)), "a_up": np.ascontiguousarray(g("rw_a_up").reshape(128, 1024)), "g_up": g("rw_g_up"),
        "kk_pc": pc(g("rw_k_k")), "ka_pc": pc(g("rw_k_a")), "rk_pc": pc(g("rw_r_k").reshape(-1)), "rwng": pc(g("rw_norm_g")), "rwnb": pc(g("rw_norm_b")),
        "rw_proj": blockify(g("rw_proj")), "s5d": pc(g("s5_d")), "s5_w_val": blockify(g("s5_w_val")), "s5_w_gate": blockify(g("s5_w_gate")), "w_out": blockify(g("w_out")),
        "ln1g": pc(g("ln1_g")), "ln1b": pc(g("ln1_b")), "ln2g": pc(g("ln2_g")), "ln2b": pc(g("ln2_b")),
        "exp_wg": np.stack([blockify(w) for w in g("exp_w_gate")]), "exp_wu": np.stack([blockify(w) for w in g("exp_w_up")]), "exp_wd": np.stack([blockify(w) for w in g("exp_w_down")]),
    }
    o.update(s5_host_layout(g("s5_lam_re"), g("s5_lam_im"), g("s5_log_dt"), g("s5_b_re"), g("s5_b_im"), g("s5_c_re"), g("s5_c_im")))
    return {k_: np.ascontiguousarray(v, dtype=np.float32) for k_, v in o.items()}

def host_shared_inputs(inp, b):
    cc = np.stack([np.asarray(inp["c"][b], np.float32), np.asarray(inp["c_ctx"], np.float32)], 0)
    return {
        "c2": np.ascontiguousarray(cc.reshape(2, KC, 128).transpose(2, 1, 0)),
        "rw_lay": np.ascontiguousarray(np.asarray(inp["router_w"], np.float32).reshape(KC, 128, 16).transpose(1, 0, 2)),
        "rb_bc": np.ascontiguousarray(np.broadcast_to(np.asarray(inp["router_b"], np.float32).reshape(1, 16), (128, 16))),
        "mlc": make_ml_consts(), "ident": np.eye(128, dtype=np.float32), "rwc": make_rw_consts(), "s5c": make_s5_consts(), "sel": make_sel_const(),
    }

def build_program(T, n_layers, out_ctx=False):
    nc = bass.Bass("TRN2", target_bir_lowering=False)
    def din(n, s): return nc.dram_tensor(n, list(s), F32, kind="ExternalInput").ap()
    def dsc(n, s): return nc.dram_tensor(n, list(s), F32).ap()
    NL = T - CTXL
    xT = din("xT", [D, T])
    sh = {n: din(n, s) for n, s in SHARED_INPUTS}
    lay = [{n: din(f"L{l}_{n}", s) for n, s in LAYER_INPUTS} for l in range(n_layers)]
    outT = nc.dram_tensor("outT", [D, NL], F32, kind="ExternalOutput").ap()
    outC = nc.dram_tensor("outC", [D, CTXL], F32, kind="ExternalOutput").ap() if out_ctx else None
    PTA_ROWS = 7568
    PT = dsc("PTa", [PTA_ROWS, T]); PTb = dsc("PTb", [D_IN - PTA_ROWS, T]); PTs = SplitRows([(0, PTA_ROWS, PT), (PTA_ROWS, D_IN, PTb)])
    QK = dsc("QK", [2048, T]); HD = dsc("HD", [2, 1024, T]); YD = dsc("YD", [2, 1024, T]); YSD = dsc("YSD", [2, 1024, T])
    YML = dsc("YML", [1024, T]); YRW = dsc("YRW", [1024, T]); YS5 = dsc("YS5", [1024, T])
    rwo = {n: dsc("s_" + n, [1024, T]) for n in ("R", "KK", "V", "BV", "G")}
    rwo.update({n: dsc("s_" + n, [2, 1024, T]) for n in ("LW", "KD", "A")})
    X1 = dsc("X1", [D, T]); X2 = dsc("X2", [D, T])
    k = KB(nc)
    mod = k.sbuf("mod", [128, 96, 2], F32); rmod = k.res()
    ones = k.sbuf("ones", [128, 128], F32); rones = k.res()
    k.op("pool", lambda e: e.memset(ones[:], 1.0 / D), writes=[rones])
    blocks_all = [(0, CTXL, 1)] + [(t, min(512, T - t), 0) for t in range(CTXL, T, 512)]
    blocks_lat = blocks_all[1:]
    xin = xT
    for l in range(n_layers):
        W = lay[l]; last = (l == n_layers - 1)
        phase_adaln(k, W["ada_w"], W["ada_b"], sh["c2"], mod, rmod, name=f"ad{l}_") if False else phase_adaln(k, W["ada_w"], W["ada_b"], sh["c2"], mod, rmod)
        phase_ln_gemm(k, xin, W["w_in"], PTs, D_IN, T, blocks_all, mod, rmod, 0, 1, ones, rones)
        phase_conv(k, PT, QK, W["cw"], W["cb"], T)
        phase_mlstm(k, PT, QK, HD, W["igb"], W["fgb"], sh["mlc"], sh["ident"], T)
        phase_ml_finish(k, HD, PT, YML, W["mlng"], W["mlnb"], T)
        phase_rw_prep(k, PT, rwo, W, T)
        phase_rwkv_scan(k, rwo["R"], rwo["KK"], rwo["V"], rwo["LW"], rwo["KD"], rwo["A"], YD, sh["rwc"], sh["ident"], T)
        phase_rw_finish(k, YD, rwo["BV"], rwo["G"], YRW, W["rwng"], W["rwnb"], T)
        phase_s5(k, PTb, YSD, W["lamN"], W["lamC"], W["Bc"], W["Cn"], sh["s5c"], T)
        phase_s5_finish(k, PTb, YSD, YS5, W["s5d"], T)
        blk = blocks_all if (not last or out_ctx) else blocks_lat
        phase_merge(k, xin, X1, PTb, YML, YRW, YS5, W, mod, rmod, W["ln1g"], W["ln1b"], blk)
        if last:
            def out_fn(t0, n):
                return outC[:, t0:t0 + n] if t0 < CTXL else outT[:, t0 - CTXL:t0 - CTXL + n]
        else:
            def out_fn(t0, n):
                return X2[:, t0:t0 + n]
        phase_moe(k, X1, out_fn, W["exp_wg"], W["exp_wu"], W["exp_wd"], sh["rw_lay"], sh["rb_bc"], sh["sel"], sh["ident"], mod, rmod, W["ln2g"], W["ln2b"], blk)
        xin = X2
    k.emit()
    return nc


SEQ = 8192; BATCH = 4; DEPTH = 2
N_CORES = 4


def kernel(**inp):
    T = CTXL + SEQ
    nc = build_program(T, DEPTH, out_ctx=False)
    lay = [host_layer_inputs(inp, l) for l in range(DEPTH)]
    x = np.asarray(inp["x"], np.float32); ctx = np.asarray(inp["ctx"], np.float32)
    in_maps = []
    for b in range(N_CORES):
        im = {"xT": np.ascontiguousarray(np.concatenate([ctx[b], x[b]], 0).T)}
        im.update(host_shared_inputs(inp, b))
        for l in range(DEPTH):
            im.update({f"L{l}_{n}": v for n, v in lay[l].items()})
        in_maps.append(im)
    res = run_bass_kernel_spmd(nc, in_maps, core_ids=list(range(N_CORES)))
    out = np.empty((BATCH, SEQ, D), np.float32)
    for b in range(N_CORES):
        out[b] = res.results[b]["outT"].T
    return out
```
